# Optimizing a Trainium2 kernel written in Bass

```python
import math
import jax, jax.numpy as jnp
from jax import lax
import numpy as np

D_MODEL = 2048
BATCH = 1
SEQ = 8192
DEPTH = 2

N_MIXERS = 2
Q_BLOCK = 128
RMS_EPS = 1e-6
NEG_INF = -1e30

DIFF_HEADS = 8
DIFF_HEAD_DIM = 128
DIFF_V_DIM = 2 * DIFF_HEAD_DIM
DIFF_QK_WIDTH = DIFF_HEADS * 2 * DIFF_HEAD_DIM
DIFF_V_WIDTH = DIFF_HEADS * DIFF_V_DIM

MLA_HEADS = 16
MLA_Q_RANK = 512
MLA_KV_RANK = 512
MLA_NOPE = 128
MLA_ROPE = 64
MLA_V = 128
ROPE_THETA = 10000.0

REL_BUCKETS = 32
REL_MAX_DIST = 128

N_EXPERTS = 32
TOP_K = 4
D_FF = 2048
SWIGLU_LIMIT = 7.0
SWIGLU_ALPHA = 1.702
MOE_BLOCK = 128

N_DIFF_LAYERS = (DEPTH + 1) // 2
N_MLA_LAYERS = DEPTH // 2

kernel_name = "hybrid_diffattn_mla_moe_adaln"


def rmsnorm(x, g):
    xf = x.astype(jnp.float32)
    y = xf * lax.rsqrt(jnp.mean(xf * xf, axis=-1, keepdims=True) + RMS_EPS)
    return (y * g.astype(jnp.float32)).astype(x.dtype)


def t5_bucket(rel):
    n = jnp.maximum(rel, 0)
    max_exact = REL_BUCKETS // 2
    nf = jnp.maximum(n, 1).astype(jnp.float32)
    large = max_exact + (jnp.log(nf / max_exact) / math.log(REL_MAX_DIST / max_exact)
                         * (REL_BUCKETS - max_exact)).astype(jnp.int32)
    large = jnp.minimum(large, REL_BUCKETS - 1)
    return jnp.where(n < max_exact, n, large)


def rope(x, pos):
    half = x.shape[-1] // 2
    inv = ROPE_THETA ** (-jnp.arange(half, dtype=jnp.float32) / half)
    ang = pos.astype(jnp.float32)[:, :, None] * inv
    cos = jnp.cos(ang)[:, :, None, :]
    sin = jnp.sin(ang)[:, :, None, :]
    xf = x.astype(jnp.float32)
    x1, x2 = xf[..., :half], xf[..., half:]
    return jnp.concatenate([x1 * cos - x2 * sin, x1 * sin + x2 * cos], axis=-1).astype(x.dtype)


def block_rel(pos, q0, kend):
    pq = pos[:, q0:q0 + Q_BLOCK]
    pk = pos[:, :kend]
    rel = pq[:, :, None] - pk[:, None, :]
    return rel, rel >= 0


def causal_blocks(fn, seq):
    outs = [fn(b * Q_BLOCK, (b + 1) * Q_BLOCK) for b in range(seq // Q_BLOCK)]
    return jnp.concatenate(outs, axis=1)


def diff_attention(u, pos, w_qkv, lq1, lk1, lq2, lk2, sub_g, w_o, rel_bias, layer_idx):
    B, S, _ = u.shape
    qkv = u @ w_qkv
    q = qkv[..., :DIFF_QK_WIDTH].reshape(B, S, DIFF_HEADS, 2, DIFF_HEAD_DIM)
    k = qkv[..., DIFF_QK_WIDTH:2 * DIFF_QK_WIDTH].reshape(B, S, DIFF_HEADS, 2, DIFF_HEAD_DIM)
    v = qkv[..., 2 * DIFF_QK_WIDTH:].reshape(B, S, DIFF_HEADS, DIFF_V_DIM)
    lam_init = 0.8 - 0.6 * math.exp(-0.3 * (layer_idx - 1))
    lam = (jnp.exp(jnp.sum(lq1.astype(jnp.float32) * lk1.astype(jnp.float32)))
           - jnp.exp(jnp.sum(lq2.astype(jnp.float32) * lk2.astype(jnp.float32))) + lam_init)
    scale = DIFF_HEAD_DIM ** -0.5

    def block(q0, kend):
        qb = q[:, q0:q0 + Q_BLOCK]
        logits = jnp.einsum('bqhmd,bkhmd->bhmqk', qb, k[:, :kend]).astype(jnp.float32) * scale
        rel, mask = block_rel(pos, q0, kend)
        bias = rel_bias[t5_bucket(rel)].astype(jnp.float32)
        logits = logits + jnp.transpose(bias, (0, 3, 1, 2))[:, :, None]
        logits = jnp.where(mask[:, None, None], logits, NEG_INF)
        p = jax.nn.softmax(logits, axis=-1)
        attn = p[:, :, 0] - lam * p[:, :, 1]
        return jnp.einsum('bhqk,bkhe->bqhe', attn.astype(v.dtype), v[:, :kend])

    o = causal_blocks(block, S)
    o = rmsnorm(o, sub_g) * (1.0 - lam_init)
    return o.reshape(B, S, DIFF_V_WIDTH) @ w_o


def mla_attention(u, pos, w_in, q_norm_g, kv_norm_g, w_uq, w_ukv, w_o):
    B, S, _ = u.shape
    z = u @ w_in
    cq = rmsnorm(z[..., :MLA_Q_RANK], q_norm_g)
    ckv = rmsnorm(z[..., MLA_Q_RANK:MLA_Q_RANK + MLA_KV_RANK], kv_norm_g)
    kr = z[..., MLA_Q_RANK + MLA_KV_RANK:]
    q = (cq @ w_uq).reshape(B, S, MLA_HEADS, MLA_NOPE + MLA_ROPE)
    q_nope = q[..., :MLA_NOPE]
    q_rope = rope(q[..., MLA_NOPE:], pos)
    kv = (ckv @ w_ukv).reshape(B, S, MLA_HEADS, MLA_NOPE + MLA_V)
    k_nope = kv[..., :MLA_NOPE]
    v = kv[..., MLA_NOPE:]
    k_rope = rope(kr[:, :, None, :], pos)[:, :, 0]
    scale = (MLA_NOPE + MLA_ROPE) ** -0.5

    def block(q0, kend):
        qn = q_nope[:, q0:q0 + Q_BLOCK]
        qr = q_rope[:, q0:q0 + Q_BLOCK]
        logits = (jnp.einsum('bqhd,bkhd->bhqk', qn, k_nope[:, :kend])
                  + jnp.einsum('bqhr,bkr->bhqk', qr, k_rope[:, :kend])).astype(jnp.float32) * scale
        _, mask = block_rel(pos, q0, kend)
        logits = jnp.where(mask[:, None], logits, NEG_INF)
        p = jax.nn.softmax(logits, axis=-1)
        return jnp.einsum('bhqk,bkhd->bqhd', p.astype(v.dtype), v[:, :kend])

    o = causal_blocks(block, S)
    return o.reshape(B, S, MLA_HEADS * MLA_V) @ w_o


def moe(u, w_router, b_router, w_gu, b_gu, w_down, b_down):
    B, S, D = u.shape
    T = B * S
    xt = u.reshape(T, D)
    logits = (xt @ w_router + b_router).astype(jnp.float32)
    top_val, top_idx = lax.top_k(logits, TOP_K)
    gates = jax.nn.softmax(top_val, axis=-1)
    A = T * TOP_K
    e_flat = top_idx.reshape(A)
    tok_flat = jnp.arange(A, dtype=jnp.int32) // TOP_K
    g_flat = gates.reshape(A)
    order = jnp.argsort(e_flat)
    e_sorted = e_flat[order]
    counts = jnp.bincount(e_flat, length=N_EXPERTS)
    padded = ((counts + MOE_BLOCK - 1) // MOE_BLOCK) * MOE_BLOCK
    pad_end = jnp.cumsum(padded)
    pad_start = pad_end - padded
    start = jnp.cumsum(counts) - counts
    rank = jnp.arange(A, dtype=jnp.int32) - start[e_sorted]
    dest = pad_start[e_sorted] + rank
    n_blocks = -(-A // MOE_BLOCK) + N_EXPERTS
    P = n_blocks * MOE_BLOCK
    src_tok = jnp.full((P,), T, jnp.int32).at[dest].set(tok_flat[order])
    row_gate = jnp.zeros((P,), jnp.float32).at[dest].set(g_flat[order])
    xpad = jnp.concatenate([xt, jnp.zeros((1, D), xt.dtype)], axis=0)
    xbuf = xpad[src_tok].reshape(n_blocks, MOE_BLOCK, D)
    block_start = jnp.arange(n_blocks, dtype=jnp.int32) * MOE_BLOCK
    block_exp = jnp.minimum(jnp.searchsorted(pad_end, block_start, side='right'), N_EXPERTS - 1)

    def expert_block(args):
        xb, e = args
        gu = xb @ w_gu[e] + b_gu[e]
        g = jnp.minimum(gu[:, :D_FF], SWIGLU_LIMIT)
        up = jnp.clip(gu[:, D_FF:], -SWIGLU_LIMIT, SWIGLU_LIMIT)
        act = (up + 1.0) * (g * jax.nn.sigmoid(g * SWIGLU_ALPHA))
        return act @ w_down[e] + b_down[e]

    ybuf = lax.map(expert_block, (xbuf, block_exp)).reshape(P, D)
    y = jax.ops.segment_sum(ybuf * row_gate[:, None].astype(ybuf.dtype), src_tok,
                            num_segments=T + 1)[:T]
    return y.reshape(B, S, D)


def setup_inputs(seed: int = 0) -> dict:
    key = jax.random.key(seed)
    ks = jax.random.split(key, 32)
    f32 = jnp.float32
    D = D_MODEL

    def nrm(k, shape, s):
        return jax.random.normal(k, shape, f32) * s

    return {
        "x": nrm(ks[0], (BATCH, SEQ, D), 1.0),
        "c": nrm(ks[1], (BATCH, D), 1.0),
        "positions": jnp.broadcast_to(jnp.arange(SEQ, dtype=jnp.int32), (BATCH, SEQ)),
        "ada_w": nrm(ks[2], (DEPTH, D, 6 * D), 0.5 * D ** -0.5),
        "ada_b": nrm(ks[3], (DEPTH, 6 * D), 0.02),
        "norm1_g": 1.0 + nrm(ks[4], (DEPTH, D), 0.02),
        "norm2_g": 1.0 + nrm(ks[5], (DEPTH, D), 0.02),
        "final_g": 1.0 + nrm(ks[6], (D,), 0.02),
        "rel_bias": nrm(ks[7], (REL_BUCKETS, DIFF_HEADS), 0.3),
        "diff_w_qkv": nrm(ks[8], (N_DIFF_LAYERS, D, 2 * DIFF_QK_WIDTH + DIFF_V_WIDTH), D ** -0.5),
        "diff_lq1": nrm(ks[9], (N_DIFF_LAYERS, DIFF_HEAD_DIM), 0.1),
        "diff_lk1": nrm(ks[10], (N_DIFF_LAYERS, DIFF_HEAD_DIM), 0.1),
        "diff_lq2": nrm(ks[11], (N_DIFF_LAYERS, DIFF_HEAD_DIM), 0.1),
        "diff_lk2": nrm(ks[12], (N_DIFF_LAYERS, DIFF_HEAD_DIM), 0.1),
        "diff_sub_g": 1.0 + nrm(ks[13], (N_DIFF_LAYERS, DIFF_V_DIM), 0.02),
        "diff_w_o": nrm(ks[14], (N_DIFF_LAYERS, DIFF_V_WIDTH, D), DIFF_V_WIDTH ** -0.5),
        "mla_w_in": nrm(ks[15], (N_MLA_LAYERS, D, MLA_Q_RANK + MLA_KV_RANK + MLA_ROPE), D ** -0.5),
        "mla_q_norm_g": 1.0 + nrm(ks[16], (N_MLA_LAYERS, MLA_Q_RANK), 0.02),
        "mla_kv_norm_g": 1.0 + nrm(ks[17], (N_MLA_LAYERS, MLA_KV_RANK), 0.02),
        "mla_w_uq": nrm(ks[18], (N_MLA_LAYERS, MLA_Q_RANK, MLA_HEADS * (MLA_NOPE + MLA_ROPE)), MLA_Q_RANK ** -0.5),
        "mla_w_ukv": nrm(ks[19], (N_MLA_LAYERS, MLA_KV_RANK, MLA_HEADS * (MLA_NOPE + MLA_V)), MLA_KV_RANK ** -0.5),
        "mla_w_o": nrm(ks[20], (N_MLA_LAYERS, MLA_HEADS * MLA_V, D), (MLA_HEADS * MLA_V) ** -0.5),
        "router_w": nrm(ks[21], (DEPTH, D, N_EXPERTS), D ** -0.5),
        "router_b": nrm(ks[22], (DEPTH, N_EXPERTS), 0.01),
        "exp_w_gu": nrm(ks[23], (DEPTH, N_EXPERTS, D, 2 * D_FF), D ** -0.5),
        "exp_b_gu": nrm(ks[24], (DEPTH, N_EXPERTS, 2 * D_FF), 0.01),
        "exp_w_down": nrm(ks[25], (DEPTH, N_EXPERTS, D_FF, D), D_FF ** -0.5),
        "exp_b_down": nrm(ks[26], (DEPTH, N_EXPERTS, D), 0.01),
    }


def reference(x, c, positions, ada_w, ada_b, norm1_g, norm2_g, final_g, rel_bias,
              diff_w_qkv, diff_lq1, diff_lk1, diff_lq2, diff_lk2, diff_sub_g, diff_w_o,
              mla_w_in, mla_q_norm_g, mla_kv_norm_g, mla_w_uq, mla_w_ukv, mla_w_o,
              router_w, router_b, exp_w_gu, exp_b_gu, exp_w_down, exp_b_down):
    h = x
    cs = jax.nn.silu(c)
    for i in range(DEPTH):
        mod = (cs @ ada_w[i] + ada_b[i])[:, None, :]
        sh1, sc1, g1, sh2, sc2, g2 = jnp.split(mod, 6, axis=-1)
        u = rmsnorm(h, norm1_g[i]) * (1.0 + sc1) + sh1
        j = i // N_MIXERS
        if i % N_MIXERS == 0:
            m = diff_attention(u, positions, diff_w_qkv[j], diff_lq1[j], diff_lk1[j],
                               diff_lq2[j], diff_lk2[j], diff_sub_g[j], diff_w_o[j],
                               rel_bias, i + 1)
        else:
            m = mla_attention(u, positions, mla_w_in[j], mla_q_norm_g[j], mla_kv_norm_g[j],
                              mla_w_uq[j], mla_w_ukv[j], mla_w_o[j])
        h = h + g1 * m
        u = rmsnorm(h, norm2_g[i]) * (1.0 + sc2) + sh2
        h = h + g2 * moe(u, router_w[i], router_b[i], exp_w_gu[i], exp_b_gu[i],
                         exp_w_down[i], exp_b_down[i])
    return rmsnorm(h, final_g)
```

```python
import math
import contextlib
import numpy as np
import ml_dtypes
import concourse.bass as bass
import concourse.mybir as mybir
from concourse.bass_utils import run_bass_kernel_spmd

F32 = mybir.dt.float32
BF16 = mybir.dt.bfloat16
ALU = mybir.AluOpType
AF = mybir.ActivationFunctionType
AX = mybir.AxisListType

NCORES = 8
D = 2048
S = 8192
KC = D // 128
RMS_EPS = 1e-6
NEG = -30000.0

ENGS = ("pe", "act", "dve", "pool", "sp")


class Tok:
    __slots__ = ("name", "w", "r", "dsem")

    def __init__(self, name=""):
        self.name = name
        self.w = None
        self.r = []
        self.dsem = None


class Ev:
    __slots__ = ("sem", "val", "op")

    def __init__(self, sem, val=None, op=None):
        self.sem = sem
        self.val = val
        self.op = op


class Op:
    __slots__ = ("eng", "fn", "needs", "ev", "signal", "dma")

    def __init__(self, eng, fn, needs, dma):
        self.eng = eng
        self.fn = fn
        self.needs = needs
        self.ev = None
        self.signal = False
        self.dma = dma


class Prog:
    N_HW = 36
    N_SW = 20

    def __init__(self, nc):
        self.nc = nc
        self.stack = contextlib.ExitStack()
        self.sems = {}
        for e in ENGS:
            self.sems[e] = self.stack.enter_context(nc.semaphore("s_" + e))
        for i in range(self.N_HW):
            self.sems[("h", i)] = self.stack.enter_context(nc.semaphore("dh%d" % i))
        for i in range(self.N_SW):
            self.sems[("s", i)] = self.stack.enter_context(nc.semaphore("ds%d" % i))
        self.count = {k: 0 for k in self.sems}
        self.waited = {e: {} for e in ENGS}
        self.first_phase = True
        self._reset_phase()

    def _reset_phase(self):
        self.ops = {e: [] for e in ENGS}
        self.used = {"h": 0, "s": 0}

    def _needs(self, reads, writes):
        needs = []
        for t in reads:
            if t.w is not None:
                needs.append(t.w)
        for t in writes:
            if t.w is not None:
                needs.append(t.w)
            needs.extend(t.r)
        return needs

    def _finish(self, o, ev, needs, reads, writes):
        o.ev = ev
        for n in needs:
            if n.op is not None and not (n.op.eng == "pe" and o.eng == "pe"):
                n.op.signal = True
        for t in reads:
            if ev.op is not None:
                t.r = [x for x in t.r if x.sem != ev.sem]
            t.r.append(ev)
        for t in writes:
            t.w = ev
            t.r = []
        self.ops[o.eng].append(o)

    def op(self, eng, fn, reads=(), writes=()):
        needs = self._needs(reads, writes)
        o = Op(eng, fn, needs, None)
        self._finish(o, Ev(eng, None, o), needs, reads, writes)
        return o

    def dma(self, eng, fn, reads=(), writes=(), semtok=None):
        needs = self._needs(reads, writes)
        if semtok is None:
            semtok = writes[0] if writes else reads[0]
        kind = "h" if eng == "sp" else "s"
        if semtok.dsem is None or semtok.dsem[0] != kind or semtok.dsem[2] != id(self.ops):
            lim = self.N_HW if kind == "h" else self.N_SW
            idx = self.used[kind]
            assert idx < lim, "out of DMA semaphores (%s)" % kind
            self.used[kind] += 1
            semtok.dsem = (kind, idx, id(self.ops))
        key = semtok.dsem[:2]
        self.count[key] += 16
        o = Op(eng, fn, needs, key)
        self._finish(o, Ev(key, self.count[key], None), needs, reads, writes)
        return o

    def wait_end(self, eng, evs):
        needs = []
        for ev in evs:
            needs.append(ev)
            if ev.op is not None:
                ev.op.signal = True
        o = Op(eng, None, needs, None)
        o.ev = Ev(eng, None, o)
        self.ops[eng].append(o)

    def emit(self):
        nc = self.nc
        sems = self.sems
        for e in ENGS:
            for o in reversed(self.ops[e]):
                if o.dma is None and o.fn is not None:
                    o.signal = True
                    break
        for e in ENGS:
            c = self.count[e]
            for o in self.ops[e]:
                if o.dma is None and o.signal and o.fn is not None:
                    c += 1
                    o.ev.val = c
            self.count[e] = c
        barrier = None
        if not self.first_phase:
            barrier = dict(self.prev_totals)
        self.first_phase = False
        with nc.Block() as block:
            def run(engname):
                def body(engobj):
                    waited = self.waited[engname]
                    if barrier is not None:
                        for s_, v in barrier.items():
                            if v > 0 and waited.get(s_, 0) < v:
                                engobj.wait_ge(sems[s_], v)
                                waited[s_] = v
                    for o in self.ops[engname]:
                        mx = {}
                        for n in o.needs:
                            if n.val is None:
                                assert engname == "pe" and n.sem == "pe"
                                continue
                            if mx.get(n.sem, 0) < n.val:
                                mx[n.sem] = n.val
                        for s_, v in mx.items():
                            if waited.get(s_, 0) >= v:
                                continue
                            if s_ == engname and engname in ("pe", "sp"):
                                continue
                            engobj.wait_ge(sems[s_], v)
                            waited[s_] = v
                        if o.fn is None:
                            continue
                        ins = o.fn(engobj)
                        if o.dma is not None:
                            ins.then_inc(sems[o.dma], 16)
                        elif o.signal:
                            ins.then_inc(sems[engname], 1)
                return body

            block.tensor(run("pe"))
            block.scalar(run("act"))
            block.vector(run("dve"))
            block.gpsimd(run("pool"))
            block.sync(run("sp"))
        self.prev_totals = dict(self.count)
        self._reset_phase()

    def close(self):
        self.stack.close()


class Bld:
    def __init__(self):
        self.nc = bass.Bass("TRN2", target_bir_lowering=False)
        self.P = Prog(self.nc)
        self.st = contextlib.ExitStack()
        self.nt = 0
        self.phase = 0
        self.dr = {}

    def dram(self, name, shape, dt, kind):
        if name in self.dr:
            return self.dr[name]
        return self.nc.dram_tensor(name, list(shape), dt, kind=kind).ap()

    def sb(self, shape, dt, name=None):
        self.nt += 1
        nm = "sb%d_%s" % (self.phase, name or ("t%d" % self.nt))
        return self.st.enter_context(self.nc.sbuf_tensor(nm, list(shape), dt))

    def ps(self, shape, dt, name=None):
        self.nt += 1
        nm = "ps%d_%s" % (self.phase, name or ("p%d" % self.nt))
        return self.st.enter_context(self.nc.psum_tensor(nm, list(shape), dt))

    def end_phase(self):
        self.P.emit()
        self.st.close()
        self.st = contextlib.ExitStack()
        self.phase += 1
        self.dr = {}

    def finish(self):
        self.end_phase()
        self.P.close()
        return self.nc

    def mm(self, out, lhsT, rhs, start, stop, r, w):
        self.P.op("pe", lambda e: e.matmul(out, lhsT, rhs, start=start, stop=stop), r, w)

    def tr(self, out, in_, ident, r, w):
        self.P.op("pe", lambda e: e.transpose(out, in_, ident), r, w)

    def act(self, out, in_, func, r, w, bias=0.0, scale=1.0, accum=None, eng="act"):
        if accum is None:
            self.P.op(eng, lambda e: e.activation(out=out, in_=in_, func=func, bias=bias, scale=scale), r, w)
        else:
            self.P.op(eng, lambda e: e.activation(out=out, in_=in_, func=func, bias=bias, scale=scale, accum_out=accum), r, w)

    def tt(self, out, in0, in1, op, r, w, eng="dve"):
        self.P.op(eng, lambda e: e.tensor_tensor(out=out, in0=in0, in1=in1, op=op), r, w)

    def ts(self, out, in0, s1, s2, op0, op1, r, w, eng="dve"):
        if op1 is None:
            self.P.op(eng, lambda e: e.tensor_single_scalar(out=out, in_=in0, scalar=s1, op=op0), r, w)
        else:
            self.P.op(eng, lambda e: e.tensor_scalar(out=out, in0=in0, scalar1=s1, scalar2=s2, op0=op0, op1=op1), r, w)

    def stt(self, out, in0, scalar, in1, op0, op1, r, w, eng="dve"):
        self.P.op(eng, lambda e: e.scalar_tensor_tensor(out=out, in0=in0, scalar=scalar, in1=in1, op0=op0, op1=op1), r, w)

    def cp(self, out, in_, r, w, eng="dve"):
        if eng == "act":
            self.P.op(eng, lambda e: e.activation(out=out, in_=in_, func=AF.Copy), r, w)
        else:
            self.P.op(eng, lambda e: e.tensor_copy(out=out, in_=in_), r, w)

    def red(self, out, in_, op, r, w, eng="dve"):
        self.P.op(eng, lambda e: e.tensor_reduce(out=out, in_=in_, axis=AX.X, op=op), r, w)

    def recip(self, out, in_, r, w):
        self.P.op("dve", lambda e: e.reciprocal(out=out, in_=in_), r, w)

    def memset(self, ap, val, w, eng="dve"):
        self.P.op(eng, lambda e: e.memset(ap, val), (), w)

    def dma(self, out, in_, r, w, eng=None, semtok=None):
        if eng is None:
            eng = "sp"
        return self.P.dma(eng, lambda e: e.dma_start(out=out, in_=in_), r, w, semtok)

    def coll(self, kind, in_, out, r, w):
        rg = [list(range(NCORES))]
        return self.P.dma("pool", lambda e: e.collective_compute(kind, ALU.bypass, replica_groups=rg, ins=[in_], outs=[out]), r, w)


def t5_bucket_np(rel):
    n = np.maximum(rel, 0)
    nf = np.maximum(n, 1).astype(np.float32)
    large = 16 + (np.log(nf / np.float32(16)) / np.float32(math.log(128 / 16)) * np.float32(16)).astype(np.int32)
    large = np.minimum(large, 31)
    return np.where(n < 16, n, large)


def load_consts(b, ident_d):
    T = Tok("consts")
    ident = b.sb([128, 128], F32, "ident")
    b.dma(ident[:], ident_d, (), [T])
    ones_bf = b.sb([128, 128], BF16, "ones_bf")
    b.memset(ones_bf[:], 1.0, [T])
    ones_f = b.sb([128, 128], F32, "ones_f")
    b.memset(ones_f[:], 1.0, [T])
    return ident, ones_bf, ones_f, T


def col_from_rows(b, src_d, nrows, ident, Tc, psum_ap, Tps, name):
    raw = b.sb([nrows, 128], F32, name + "_raw")
    Traw = Tok(name + "_raw")
    b.dma(raw[:], src_d, (), [Traw])
    b.tr(psum_ap, raw[:], ident[0:nrows, 0:nrows], [Traw, Tc], [Tps])
    out = b.sb([128, nrows], F32, name)
    To = Tok(name)
    b.cp(out[:], psum_ap, [Tps], [To])
    return out, To


def ada_cols(b, adaw_d, ncols, cs_col, Tcs, abT, Tab, psum_ap, Tps, name, bufs=None):
    npc = ncols // 128
    if bufs is None:
        bufs = [b.sb([128, KC, 128], F32, "%s_w%d" % (name, i))[:] for i in range(2)]
    Tb = [Tok("adaw%d" % i) for i in range(2)]
    src = adaw_d.rearrange("(kc p) n -> p kc n", p=128)
    for p in range(npc):
        bb = p % 2
        b.dma(bufs[bb], src[:, :, p * 128:(p + 1) * 128], (), [Tb[bb]], eng=("sp" if p % 2 == 0 else "pool"))
        for kc in range(KC):
            b.mm(psum_ap[:, p:p + 1], bufs[bb][:, kc, :], cs_col[:, kc:kc + 1], kc == 0, kc == KC - 1,
                 [Tb[bb], Tcs], [Tps])
    modT = b.sb([128, npc], F32, name)
    Tm = Tok(name)
    b.tt(modT[:], psum_ap[:, 0:npc], abT[:, 0:npc], ALU.add, [Tps, Tab], [Tm])
    return modT, Tm


CH = 256
NCH = S // CH
DIFF_LAM_INIT = 0.8 - 0.6 * math.exp(-0.3 * 0.0)


def build_attn_diff(seq=S, b=None):
    NCH = seq // CH
    own = b is None
    if own:
        b = Bld()
    nc = b.nc
    hT_d = b.dram("hT", [D, seq], F32, "ExternalInput")
    c_d = b.dram("cvec", [16, 128], F32, "ExternalInput")
    adaw_d = b.dram("adaw", [D, 4096], F32, "ExternalInput")
    adab_d = b.dram("adab", [32, 128], F32, "ExternalInput")
    n1g_d = b.dram("n1g", [16, 128], F32, "ExternalInput")
    wqkv_d = b.dram("wqkv", [D, 768], F32, "ExternalInput")
    rows_d = b.dram("rows", [1, 800], F32, "ExternalInput")
    idx_d = b.dram("idx", [128, 3, 256], F32, "ExternalInput")
    ident_d = b.dram("ident", [128, 128], F32, "ExternalInput")
    oT_d = b.dram("oT", [256, seq], BF16, "ExternalOutput")

    banks = [b.ps([128, 512], F32, "bank%d" % i) for i in range(8)]
    Tbank = [Tok("bank%d" % i) for i in range(8)]
    hs = [banks[6][:, 0:256], banks[7][:, 0:256], banks[6][:, 256:512], banks[7][:, 256:512]]
    Ths = [Tbank[6], Tbank[7], Tbank[6], Tbank[7]]
    hsi = [0]

    def next_hs():
        i = hsi[0] % 4
        hsi[0] += 1
        return hs[i], Ths[i]

    ident, ones_bf, ones_f, Tc = load_consts(b, ident_d)

    p, Tp = next_hs()
    c_col, Tcc = col_from_rows(b, c_d, 16, ident, Tc, p[:, 0:16], Tp, "c_col")
    sig = b.sb([128, 16], F32, "sig")
    Tsig = Tok()
    b.act(sig[:], c_col[:], AF.Sigmoid, [Tcc], [Tsig])
    cs_col = b.sb([128, 16], F32, "cs_col")
    Tcs = Tok()
    b.tt(cs_col[:], c_col[:], sig[:], ALU.mult, [Tcc, Tsig], [Tcs])
    p, Tp = next_hs()
    abT, Tab = col_from_rows(b, adab_d, 32, ident, Tc, p[:, 0:32], Tp, "abT")
    p, Tp = next_hs()
    g1T, Tg1 = col_from_rows(b, n1g_d, 16, ident, Tc, p[:, 0:16], Tp, "g1T")
    p, Tp = next_hs()
    hbuf0 = b.sb([128, KC, CH], F32, "hbuf0")
    modT, Tm = ada_cols(b, adaw_d, 4096, cs_col, Tcs, abT, Tab, p, Tp, "modT",
                        bufs=[hbuf0[:, :, 0:128], hbuf0[:, :, 128:256]])
    sh1 = modT[:, 0:16]
    sc1 = modT[:, 16:32]
    gsc = b.sb([128, 16], F32, "gsc")
    Tgsc = Tok()
    b.stt(gsc[:], sc1, 1.0, g1T[:], ALU.add, ALU.mult, [Tm, Tg1], [Tgsc])
    sh1_bf = b.sb([128, 16], BF16, "sh1_bf")
    Tsh = Tok()
    b.cp(sh1_bf[:], sh1, [Tm], [Tsh])

    Wb = b.sb([128, KC, 768], BF16, "Wb")
    TW = Tok("Wb")
    wsrc = wqkv_d.rearrange("(kc p) n -> p kc n", p=128)
    for q4 in range(4):
        b.dma(Wb[:, q4 * 4:(q4 + 1) * 4, :], wsrc[:, q4 * 4:(q4 + 1) * 4, :], (), [TW], eng="pool")
    SCL = 128.0 ** -0.25
    p, Tp = next_hs()
    for f in range(4):
        for kc in range(KC):
            b.mm(p[:, f:f + 1], Wb[:, kc, f * 128:(f + 1) * 128], sh1_bf[:, kc:kc + 1], kc == 0, kc == KC - 1,
                 [TW, Tsh], [Tp])
    bqk = b.sb([128, 4], F32, "bqk")
    Tbqk = Tok()
    b.ts(bqk[:], p[:, 0:4], SCL, None, ALU.mult, None, [Tp], [Tbqk])
    p, Tp = next_hs()
    for kc in range(KC):
        b.mm(p[0:1, 0:256], sh1_bf[:, kc:kc + 1], Wb[:, kc, 512:768], kc == 0, kc == KC - 1, [TW, Tsh], [Tp])
    bv_row = b.sb([1, 256], BF16, "bv_row")
    Tbv = Tok()
    b.cp(bv_row[:], p[0:1, 0:256], [Tp], [Tbv])
    ones_row_bf = b.sb([1, 128], BF16, "ones_row_bf")
    b.memset(ones_row_bf[:], 1.0, [Tbv])
    TWj = Tok("Wb_scaled")
    for kc in range(KC):
        b.ts(Wb[:, kc, :], Wb[:, kc, :], gsc[:, kc:kc + 1], None, ALU.mult, None, [Tgsc, TW, Tbqk, Tbv], [TWj])

    rows = b.sb([1, 800], F32, "rows")
    Trows = Tok()
    b.dma(rows[:], rows_d, (), [Trows])
    bc = b.sb([128, 800], F32, "bc")
    Tbc = Tok()
    p0, Tp0 = banks[0], Tbank[0]
    p1, Tp1 = banks[1], Tbank[1]
    b.mm(p0[:, 0:512], ones_f[0:1, :], rows[:, 0:512], True, True, [Trows, Tc], [Tp0])
    b.mm(p1[:, 0:288], ones_f[0:1, :], rows[:, 512:800], True, True, [Trows, Tc], [Tp1])
    b.cp(bc[:, 0:512], p0[:, 0:512], [Tp0], [Tbc])
    b.cp(bc[:, 512:800], p1[:, 0:288], [Tp1], [Tbc])
    rb_b = bc[:, 0:32]
    small = b.sb([128, 16], F32, "small")
    Tsm = Tok()
    lt = b.sb([128, 256], F32, "lt")
    Tlt = Tok()
    b.tt(lt[:, 0:128], bc[:, 32:160], bc[:, 160:288], ALU.mult, [Tbc], [Tlt])
    b.tt(lt[:, 128:256], bc[:, 288:416], bc[:, 416:544], ALU.mult, [Tbc], [Tlt])
    b.red(small[:, 0:1], lt[:, 0:128], ALU.add, [Tlt], [Tsm])
    b.red(small[:, 1:2], lt[:, 128:256], ALU.add, [Tlt], [Tsm])
    b.act(small[:, 2:4], small[:, 0:2], AF.Exp, [Tsm], [Tsm])
    b.stt(small[:, 4:5], small[:, 3:4], -DIFF_LAM_INIT, small[:, 2:3], ALU.add, ALU.subtract, [Tsm], [Tsm])
    neg_lam = small[:, 4:5]
    b.red(small[:, 5:6], rb_b, ALU.max, [Tbc], [Tsm])
    maxbias = small[:, 5:6]
    b.tt(small[:, 6:7], bc[:, 31:32], small[:, 5:6], ALU.subtract, [Tbc, Tsm], [Tsm])
    c31mb = small[:, 6:7]
    subg_s = b.sb([128, 256], F32, "subg_s")
    Tsg = Tok()
    b.ts(subg_s[:], bc[:, 544:800], 1.0 - DIFF_LAM_INIT, None, ALU.mult, None, [Tbc], [Tsg])

    idx = b.sb([128, 3, 256], F32, "idx")
    Tidx = Tok()
    b.dma(idx[:], idx_d, (), [Tidx])
    BTN = b.sb([128, 3, 256], F32, "BTN")
    TBTN = Tok()
    eqt = b.sb([128, 3, 256], F32, "eqt")
    Teq = Tok()
    b.ts(BTN[:], idx[:], 0.0, NEG, ALU.is_lt, ALU.mult, [Tidx], [TBTN])
    for bk in range(32):
        b.ts(eqt[:], idx[:], float(bk), None, ALU.is_equal, None, [Tidx], [Teq])
        b.stt(BTN[:], eqt[:], rb_b[:, bk:bk + 1], BTN[:], ALU.mult, ALU.add, [Teq, Tbc], [TBTN])

    kT = b.sb([128, 2, seq], BF16, "kT")
    Vx = b.sb([128, seq // 128, 257], BF16, "Vx")
    Tk = [Tok("k%d" % c) for c in range(NCH)]
    Tv = [Tok("v%d" % c) for c in range(NCH)]
    TV1 = Tok("vones")
    b.P.op("pool", lambda e: e.memset(Vx[:, :, 256:257], 1.0), [TW], [TV1])
    qbuf = [b.sb([128, 2, CH], BF16, "qbuf%d" % i) for i in range(2)]
    Tq = [Tok("q%d" % i) for i in range(2)]
    hbuf = [hbuf0]
    Th = [Tok("h%d" % i) for i in range(1)]
    sq = b.sb([128, KC, CH], BF16, "sq")
    Tsq = Tok()
    xn = [b.sb([128, KC, CH], BF16, "xn%d" % i) for i in range(2)]
    Txn = [Tok("xn%d" % i) for i in range(2)]
    lnv = b.sb([128, CH], F32, "lnv")
    Tln = Tok()
    rstd = b.sb([128, CH], F32, "rstd")
    Trs = Tok()
    st2 = b.sb([128, 2, CH], BF16, "st2")
    Tst2 = Tok()
    qmax = b.sb([128, NCH], F32, "qmax")
    kmax = b.sb([128, NCH + 1], F32, "kmax")
    negB = b.sb([128, NCH, 2], F32, "negB")
    Tqm = Tok()
    Tkm = Tok()
    TnB = [Tok("nB%d" % c) for c in range(NCH)]
    b.memset(kmax[:, 0:1], 0.0, [Tkm])
    tmpm = b.sb([128, 4], F32, "tmpm")
    Ttm = Tok()

    hsrc = hT_d.rearrange("(kc p) n -> p kc n", p=128)
    hsrc_fn = b.dr.get("hT_src", lambda c: hsrc[:, :, c * CH:(c + 1) * CH])

    def stage1(c):
        t0 = c * CH
        hb, Thb = hbuf[0], Th[0]
        x, Tx = xn[c % 2], Txn[c % 2]
        qb, Tqb = qbuf[c % 2], Tq[c % 2]
        b.dma(hb[:, 0:8, :], hsrc_fn(c)[:, 0:8, :], [Tm], [Thb], eng="sp")
        b.dma(hb[:, 8:16, :], hsrc_fn(c)[:, 8:16, :], [Tm], [Thb], eng="sp")
        b.tt(sq[:], hb[:], hb[:], ALU.mult, [Thb], [Tsq], eng="pool")
        p, Tp = next_hs()
        for kc in range(KC):
            b.mm(p, ones_bf[:], sq[:, kc, :], kc == 0, kc == KC - 1, [Tsq, Tc], [Tp])
        b.act(lnv[:], p, AF.Ln, [Tp], [Tln], bias=RMS_EPS, scale=1.0 / D)
        b.act(rstd[:], lnv[:], AF.Exp, [Tln], [Trs], scale=-0.5)
        b.tt(x[:], hb[:], rstd[:].unsqueeze(1).to_broadcast([128, KC, CH]), ALU.mult, [Thb, Trs], [Tx])
        for f in range(4):
            p, Tp = next_hs()
            for kc in range(KC):
                b.mm(p, Wb[:, kc, f * 128:(f + 1) * 128], x[:, kc, :], kc == 0, kc == KC - 1, [TWj, Tx], [Tp])
            if f < 2:
                dst, Td = qb[:, f, :], Tqb
            else:
                dst, Td = kT[:, f - 2, t0:t0 + CH], Tk[c]
            b.act(dst, p, AF.Identity, [Tp, Tbqk], [Td], bias=bqk[:, f:f + 1], scale=SCL)
        for tb in range(CH // 128):
            p, Tp = next_hs()
            for kc in range(KC):
                b.mm(p, x[:, kc, tb * 128:(tb + 1) * 128], Wb[:, kc, 512:768], kc == 0, False, [TWj, Tx], [Tp])
            b.mm(p, ones_row_bf[:], bv_row[:], False, True, [Tbv], [Tp])
            b.cp(Vx[:, c * 2 + tb, 0:256], p, [Tp, TV1], [Tv[c]])
        b.tt(st2[:], qb[:], qb[:], ALU.mult, [Tqb], [Tst2], eng="pool")
        for m in range(2):
            p, Tp = next_hs()
            b.mm(p, ones_bf[:], st2[:, m, :], True, True, [Tst2, Tc], [Tp])
            b.red(tmpm[:, m:m + 1], p, ALU.max, [Tp], [Ttm])
        b.tt(qmax[:, c:c + 1], tmpm[:, 0:1], tmpm[:, 1:2], ALU.max, [Ttm], [Tqm])
        b.tt(st2[:], kT[:, :, t0:t0 + CH], kT[:, :, t0:t0 + CH], ALU.mult, [Tk[c]], [Tst2], eng="pool")
        for m in range(2):
            p, Tp = next_hs()
            b.mm(p, ones_bf[:], st2[:, m, :], True, True, [Tst2, Tc], [Tp])
            b.red(tmpm[:, 2 + m:3 + m], p, ALU.max, [Tp], [Ttm])
        b.tt(tmpm[:, 2:3], tmpm[:, 2:3], tmpm[:, 3:4], ALU.max, [Ttm], [Ttm])
        b.tt(kmax[:, c + 1:c + 2], kmax[:, c:c + 1], tmpm[:, 2:3], ALU.max, [Ttm, Tkm], [Tkm])
        b.stt(negB[:, c, 0:1], qmax[:, c:c + 1], 1.0, kmax[:, c + 1:c + 2], ALU.mult, ALU.add, [Tqm, Tkm], [TnB[c]])
        b.ts(negB[:, c, 0:1], negB[:, c, 0:1], -0.5, maxbias, ALU.mult, ALU.subtract, [Tsm, TnB[c]], [TnB[c]])
        b.tt(negB[:, c, 1:2], negB[:, c, 0:1], bc[:, 31:32], ALU.add, [TnB[c], Tbc], [TnB[c]])

    Sb = [banks[0], banks[1]]
    TS = [Tbank[0], Tbank[1]]
    ACC = [[banks[2], banks[3]], [banks[4], banks[5]]]
    TACC = [[Tbank[2], Tbank[3]], [Tbank[4], Tbank[5]]]
    Ssb = [b.sb([128, 2, CH], F32, "Ssb%d" % i) for i in range(2)]
    TSsb = [Tok() for _ in range(2)]
    PT = [b.sb([128, 2, CH], BF16, "PT%d" % i) for i in range(3)]
    TPT = [Tok() for _ in range(3)]
    ep = b.sb([128, 8], F32, "ep")
    Tep = Tok()
    t0buf = b.sb([128, 256], F32, "t0buf")
    Tt0 = Tok()
    obuf = b.sb([128, 256], F32, "obuf")
    Tob = Tok()
    osq = b.sb([128, 256], F32, "osq")
    Tosq = Tok()
    onb = b.sb([128, 256], F32, "onb")
    Ton = Tok()
    oTb = [b.sb([128, 2, CH], BF16, "oTb%d" % i) for i in range(2)]
    ToT = [Tok() for _ in range(2)]
    Tout = Tok("out")
    odst = oT_d.rearrange("(j p) n -> p j n", p=128)
    odst_fn = b.dr.get("oT_dst", lambda c: odst[:, :, c * CH:(c + 1) * CH])
    cnt = {"s": 0, "p": 0, "n": 0}
    outevs = []

    def scores(c, j):
        sbi = cnt["s"] % 2
        cnt["s"] += 1
        qb, Tqb = qbuf[c % 2], Tq[c % 2]
        for m in range(2):
            b.mm(Sb[sbi][:, m * CH:(m + 1) * CH], kT[:, m, j * 128:(j + 1) * 128], qb[:, m, :], True, True,
                 [Tk[j // 2], Tqb], [TS[sbi]])
        return sbi

    def stage3(c):
        nj = 2 * c + 2
        sbi_next = scores(c, 0)
        for j in range(nj):
            sbi = sbi_next
            if j + 1 < nj:
                sbi_next = scores(c, j + 1)
            pi = cnt["p"] % 3
            cnt["p"] += 1
            t = j - (2 * c - 1)
            if t >= 0:
                ni = cnt["n"] % 2
                cnt["n"] += 1
                for m in range(2):
                    b.tt(Ssb[ni][:, m, :], Sb[sbi][:, m * CH:(m + 1) * CH], BTN[:, t, :], ALU.add,
                         [TS[sbi], TBTN], [TSsb[ni]])
                b.act(PT[pi][:].rearrange("p m q -> p (m q)"), Ssb[ni][:].rearrange("p m q -> p (m q)"), AF.Exp,
                      [TSsb[ni], TnB[c]], [TPT[pi]], bias=negB[:, c, 0:1])
            else:
                b.act(PT[pi][:].rearrange("p m q -> p (m q)"), Sb[sbi][:, :], AF.Exp,
                      [TS[sbi], TnB[c]], [TPT[pi]], bias=negB[:, c, 1:2])
            for m in range(2):
                for qb_ in range(2):
                    last = (2 * c) if qb_ == 0 else (2 * c + 1)
                    if j > last:
                        continue
                    b.mm(ACC[m][qb_][:, 0:257], PT[pi][:, m, qb_ * 128:(qb_ + 1) * 128], Vx[:, j, :], j == 0, j == last,
                         [TPT[pi], Tv[j // 2], TV1], [TACC[m][qb_]])
        ob, Tobuf = oTb[c % 2], ToT[c % 2]
        for qb_ in range(2):
            A0, A1 = ACC[0][qb_], ACC[1][qb_]
            b.recip(ep[:, 0:1], A0[:, 256:257], [TACC[0][qb_]], [Tep])
            b.recip(ep[:, 1:2], A1[:, 256:257], [TACC[1][qb_]], [Tep])
            b.tt(ep[:, 2:3], ep[:, 1:2], neg_lam, ALU.mult, [Tep, Tsm], [Tep])
            b.ts(t0buf[:], A0[:, 0:256], ep[:, 0:1], None, ALU.mult, None, [TACC[0][qb_], Tep], [Tt0])
            b.stt(obuf[:], A1[:, 0:256], ep[:, 2:3], t0buf[:], ALU.mult, ALU.add, [TACC[1][qb_], Tep, Tt0], [Tob])
            b.tt(osq[:], obuf[:], obuf[:], ALU.mult, [Tob], [Tosq], eng="pool")
            b.red(ep[:, 3:4], osq[:], ALU.add, [Tosq], [Tep])
            b.act(ep[:, 4:5], ep[:, 3:4], AF.Ln, [Tep], [Tep], bias=RMS_EPS, scale=1.0 / 256)
            b.act(ep[:, 5:6], ep[:, 4:5], AF.Exp, [Tep], [Tep], scale=-0.5)
            b.stt(onb[:], obuf[:], ep[:, 5:6], subg_s[:], ALU.mult, ALU.mult, [Tob, Tep, Tsg], [Ton])
            p, Tp = next_hs()
            for jj in range(2):
                b.tr(p[:, jj * 128:(jj + 1) * 128], onb[:, jj * 128:(jj + 1) * 128], ident[:], [Ton, Tc], [Tp])
            b.cp(ob[:, :, qb_ * 128:(qb_ + 1) * 128], p.rearrange("p (j q) -> p j q", j=2), [Tp], [Tobuf])
        outevs.append(b.dma(odst_fn(c), ob[:], [Tobuf], [Tout], eng="sp", semtok=Tobuf).ev)

    stage1(0)
    for c in range(NCH):
        if c + 1 < NCH:
            stage1(c + 1)
        stage3(c)
    b.P.wait_end("sp", outevs)
    if own:
        return b.finish()
    b.end_phase()


def ada_rows(b, adaw_d, adab_d, ncols, cs_col, Tcs, ones_f, Tc, banks, Tbank, name):
    W = 128
    csb = b.sb([128, KC, 128], F32, name + "_csb")
    Tcsb = Tok()
    for kc in range(KC):
        b.ts(csb[:, kc, :], ones_f[:], cs_col[:, kc:kc + 1], None, ALU.mult, None, [Tcs, Tc], [Tcsb])
    abrow = [b.sb([1, W], F32, "%s_abrow%d" % (name, i)) for i in range(2)]
    Tabr = [Tok() for _ in range(2)]
    out = b.sb([128, ncols], F32, name)
    To = Tok(name)
    bufs = [b.sb([128, KC, W], F32, "%s_w%d" % (name, i)) for i in range(2)]
    Tb = [Tok() for _ in range(2)]
    src = adaw_d.rearrange("(kc p) n -> p kc n", p=128)
    for n in range(ncols // W):
        bb = n % 2
        b.dma(bufs[bb][:], src[:, :, n * W:(n + 1) * W], (), [Tb[bb]], eng="sp")
        b.dma(abrow[bb][:], adab_d[:, n * W:(n + 1) * W], (), [Tabr[bb]], eng="sp")
        pb, Tpb = banks[n % 2], Tbank[n % 2]
        for kc in range(KC):
            b.mm(pb[:, 0:W], csb[:, kc, :], bufs[bb][:, kc, :], kc == 0, False, [Tcsb, Tb[bb]], [Tpb])
        b.mm(pb[:, 0:W], ones_f[0:1, :], abrow[bb][:], False, True, [Tabr[bb], Tc], [Tpb])
        b.cp(out[:, n * W:(n + 1) * W], pb[:, 0:W], [Tpb], [To], eng=("dve" if n % 2 == 0 else "act"))
    return out, To


def silu_col(b, c_d, ident, Tc, psum_ap, Tps):
    c_col, Tcc = col_from_rows(b, c_d, 16, ident, Tc, psum_ap, Tps, "c_col")
    sig = b.sb([128, 16], F32, "sig")
    Tsig = Tok()
    b.act(sig[:], c_col[:], AF.Sigmoid, [Tcc], [Tsig])
    cs_col = b.sb([128, 16], F32, "cs_col")
    Tcs = Tok()
    b.tt(cs_col[:], c_col[:], sig[:], ALU.mult, [Tcc, Tsig], [Tcs])
    return cs_col, Tcs


def bcast_row(b, row_d, n, ones_f, Tc, bank, Tb_, name, eng="dve"):
    row = b.sb([1, n], F32, name + "_row")
    Tr = Tok()
    b.dma(row[:], row_d, (), [Tr])
    out = b.sb([128, n], F32, name)
    To = Tok()
    for i in range(0, n, 512):
        w = min(512, n - i)
        b.mm(bank[:, 0:w], ones_f[0:1, :], row[:, i:i + w], True, True, [Tr, Tc], [Tb_])
        b.cp(out[:, i:i + w], bank[:, 0:w], [Tb_], [To], eng=eng)
    return out, To


def build_b1(ntok=1024, dbg=9, b=None):
    own = b is None
    if own:
        b = Bld()
    NT = ntok // 128
    oT_d = b.dram("oT", [D, ntok], BF16, "ExternalInput")
    h_d = b.dram("h", [ntok, D], F32, "ExternalInput")
    c_d = b.dram("cvec", [16, 128], F32, "ExternalInput")
    adaw_d = b.dram("adaw", [D, 6144], F32, "ExternalInput")
    adab_d = b.dram("adab", [1, 6144], F32, "ExternalInput")
    n2g_d = b.dram("n2g", [1, D], F32, "ExternalInput")
    wo_d = b.dram("wo", [D, D], F32, "ExternalInput")
    wr_d = b.dram("wr", [D, 32], F32, "ExternalInput")
    br_d = b.dram("br", [1, 32], F32, "ExternalInput")
    ident_d = b.dram("ident", [128, 128], F32, "ExternalInput")
    h1_d = b.dram("h1", [ntok, D], F32, "ExternalOutput")
    xT_d = b.dram("xT", [ntok // 128, 128, KC * 128], BF16, "ExternalOutput")
    gates_d = b.dram("gates", [128, (ntok // 128) * 32], F32, "ExternalOutput")

    banks = [b.ps([128, 512], F32, "bank%d" % i) for i in range(8)]
    Tbank = [Tok("bank%d" % i) for i in range(8)]
    ident, ones_bf, ones_f, Tc = load_consts(b, ident_d)
    cs_col, Tcs = silu_col(b, c_d, ident, Tc, banks[7][:, 0:16], Tbank[7])
    modb, Tmod = ada_rows(b, adaw_d, adab_d, 6144, cs_col, Tcs, ones_f, Tc, banks, Tbank, "modb")
    g1b = modb[:, 0:2048]
    sh2b = modb[:, 2048:4096]
    sc2b = modb[:, 4096:6144]
    n2gb, Tn2 = bcast_row(b, n2g_d, D, ones_f, Tc, banks[2], Tbank[2], "n2gb")
    Tg2 = Tok()
    b.stt(n2gb[:], sc2b, 1.0, n2gb[:], ALU.add, ALU.mult, [Tmod, Tn2], [Tg2])
    brb, Tbr = bcast_row(b, br_d, 32, ones_f, Tc, banks[3], Tbank[3], "brb")
    wo = b.sb([128, KC, D], BF16, "wo")
    Two = Tok()
    wsrc = wo_d.rearrange("(kc p) n -> p kc n", p=128)
    for q4 in range(4):
        b.dma(wo[:, q4 * 4:(q4 + 1) * 4, :], wsrc[:, q4 * 4:(q4 + 1) * 4, :], (), [Two], eng="pool")
    wr = b.sb([128, KC, 32], F32, "wr")
    Twr = Tok()
    b.dma(wr[:], wr_d.rearrange("(kc p) n -> p kc n", p=128), (), [Twr])

    oTs = [b.sb([128, KC, 128], BF16, "oT%d" % i) for i in range(2)]
    ToTs = [Tok() for _ in range(2)]
    osrc = oT_d.rearrange("(kc p) n -> p kc n", p=128)

    hb = [b.sb([128, D], F32, "hb%d" % i) for i in range(2)]
    Thb = [Tok() for _ in range(2)]
    tmp = b.sb([128, D], F32, "tmp")
    Ttmp = Tok()
    u2 = b.sb([128, D], F32, "u2")
    Tu2 = Tok()
    uTf = b.sb([128, KC, 128], F32, "uTf")
    TuTf = Tok()
    uTb = [b.sb([128, KC, 128], BF16, "uTb%d" % i) for i in range(2)]
    TuTb = [Tok() for _ in range(2)]
    sm = [b.sb([128, 8], F32, "sm%d" % i) for i in range(2)]
    Tsm = [Tok() for _ in range(2)]
    rt = [b.sb([128, 4, 32], F32, "rt%d" % i) for i in range(2)]
    Trt = [Tok() for _ in range(2)]
    t8 = b.sb([128, 8], F32, "t8")
    Tt8 = Tok()
    gall = b.sb([128, NT, 32], F32, "gall")
    Tgall = Tok()
    outevs = []

    for t in range(NT if dbg >= 1 else 0):
        rows = slice(t * 128, (t + 1) * 128)
        h, Th = hb[t % 2], Thb[t % 2]
        s, Ts = sm[t % 2], Tsm[t % 2]
        r, Tr = rt[t % 2], Trt[t % 2]
        ub, Tub = uTb[t % 2], TuTb[t % 2]
        b.dma(h[:], h_d[rows, :], (), [Th])
        oT, ToT = oTs[t % 2], ToTs[t % 2]
        b.dma(oT[:], osrc[:, :, rows], (), [ToT])
        for n in range(4):
            pb, Tpb = banks[n], Tbank[n]
            for kc in range(KC):
                b.mm(pb[:, :], oT[:, kc, :], wo[:, kc, n * 512:(n + 1) * 512], kc == 0, kc == KC - 1, [ToT, Two], [Tpb])
            b.tt(tmp[:, n * 512:(n + 1) * 512], pb[:, :], g1b[:, n * 512:(n + 1) * 512], ALU.mult, [Tpb, Tmod], [Ttmp])
        b.tt(h[:], h[:], tmp[:], ALU.add, [Th, Ttmp], [Th])
        outevs.append(b.dma(h1_d[rows, :], h[:], [Th], [], semtok=Th).ev)
        if dbg < 2:
            continue
        b.tt(tmp[:], h[:], h[:], ALU.mult, [Th], [Ttmp])
        b.red(s[:, 0:1], tmp[:], ALU.add, [Ttmp], [Ts])
        b.act(s[:, 1:2], s[:, 0:1], AF.Ln, [Ts], [Ts], bias=RMS_EPS, scale=1.0 / D)
        b.act(s[:, 2:3], s[:, 1:2], AF.Exp, [Ts], [Ts], scale=-0.5)
        b.stt(u2[:], h[:], s[:, 2:3], n2gb[:], ALU.mult, ALU.mult, [Th, Ts, Tg2], [Tu2])
        b.tt(u2[:], u2[:], sh2b, ALU.add, [Tu2, Tmod], [Tu2])
        if dbg < 3:
            continue
        for q4 in range(4):
            pb, Tpb = banks[4 + q4 % 2], Tbank[4 + q4 % 2]
            for j in range(4):
                kc = q4 * 4 + j
                b.tr(pb[:, j * 128:(j + 1) * 128], u2[:, kc * 128:(kc + 1) * 128], ident[:], [Tu2, Tc], [Tpb])
            b.cp(uTf[:, q4 * 4:(q4 + 1) * 4, :], pb[:, :].rearrange("p (j q) -> p j q", j=4), [Tpb], [TuTf])
            b.cp(ub[:, q4 * 4:(q4 + 1) * 4, :], uTf[:, q4 * 4:(q4 + 1) * 4, :], [TuTf], [Tub], eng="act")
        outevs.append(b.dma(xT_d[t], ub[:].rearrange("p k n -> p (k n)"), [Tub], [], semtok=Tub).ev)
        if dbg < 4:
            continue
        pb, Tpb = banks[6], Tbank[6]
        for kc in range(KC):
            b.mm(pb[:, 0:32], uTf[:, kc, :], wr[:, kc, :], kc == 0, kc == KC - 1, [TuTf, Twr], [Tpb])
        b.tt(r[:, 0, :], pb[:, 0:32], brb[:], ALU.add, [Tpb, Tbr], [Tr])
        if dbg < 5:
            b.cp(gall[:, t, :], r[:, 0, :], [Tr], [Tgall])
            continue
        b.P.op("dve", lambda e, o_=t8[:], i_=r[:, 0, :]: e.max(out=o_, in_=i_), [Tr], [Tt8])
        b.ts(r[:, 1, :], r[:, 0, :], t8[:, 3:4], None, ALU.is_ge, None, [Tr, Tt8], [Tr])
        b.ts(s[:, 3:4], t8[:, 0:1], -1.0, None, ALU.mult, None, [Tt8], [Ts])
        b.act(r[:, 2, :], r[:, 0, :], AF.Exp, [Tr, Ts], [Tr], bias=s[:, 3:4])
        b.tt(r[:, 2, :], r[:, 2, :], r[:, 1, :], ALU.mult, [Tr], [Tr])
        b.red(s[:, 4:5], r[:, 2, :], ALU.add, [Tr], [Ts])
        b.recip(s[:, 5:6], s[:, 4:5], [Ts], [Ts])
        b.ts(gall[:, t, :], r[:, 2, :], s[:, 5:6], None, ALU.mult, None, [Tr, Ts], [Tgall])
    outevs.append(b.dma(gates_d, gall[:].rearrange("p t e -> p (t e)"), [Tgall], [], semtok=Tgall).ev)
    b.P.wait_end("sp", outevs)
    if own:
        return b.finish()
    b.end_phase()


G = 512


def build_c(ntok=S, NE=4, b=None):
    own = b is None
    if own:
        b = Bld()
    NG = ntok // G
    xT_d = b.dram("xT", [D, ntok], BF16, "ExternalInput")
    gs_d = b.dram("gsel", [128, ntok // 128, NE], F32, "ExternalInput")
    wgu_d = b.dram("wgu", [NE, D, 4096], F32, "ExternalInput")
    bgu_d = b.dram("bgu", [NE, 32, 128], F32, "ExternalInput")
    wd_d = b.dram("wd", [NE, D, D], F32, "ExternalInput")
    bd_d = b.dram("bd", [NE, D], F32, "ExternalInput")
    ident_d = b.dram("ident", [128, 128], F32, "ExternalInput")
    y_d = b.dram("ypart", [ntok, D], F32, "ExternalOutput")

    banks = [b.ps([128, 512], F32, "bank%d" % i) for i in range(8)]
    Tbank = [Tok("bank%d" % i) for i in range(8)]
    ident, ones_bf, ones_f, Tc = load_consts(b, ident_d)
    bguT = b.sb([128, NE, 32], F32, "bguT")
    Tbgu = Tok()
    for e in range(NE):
        raw = b.sb([32, 128], F32, "bgu_raw%d" % e)
        Traw = Tok()
        b.dma(raw[:], bgu_d[e], (), [Traw])
        b.tr(banks[7][:, 0:32], raw[:], ident[0:32, 0:32], [Traw, Tc], [Tbank[7]])
        b.cp(bguT[:, e, :], banks[7][:, 0:32], [Tbank[7]], [Tbgu])
    bd = b.sb([NE, D], F32, "bd")
    Tbd = Tok()
    b.dma(bd[:], bd_d, (), [Tbd])

    xT = [b.sb([128, KC, G], BF16, "xT%d" % i) for i in range(2)]
    TxT = [Tok() for _ in range(2)]
    gsel_all = b.sb([128, ntok // 128, NE], F32, "gsel_all")
    Tg = Tok()
    if "gates_all" in b.dr:
        NTT = ntok // 128
        gall = b.sb([128, NTT, 32], F32, "gates_all_sb")
        Tga = Tok()
        b.dma(gall[:].rearrange("p (r t) e -> p r (t e)", r=NCORES), b.dr["gates_all"].rearrange("r p f -> p r f"), (), [Tga])
        esel = b.sb([128, NE, 32], F32, "esel")
        b.dma(esel[:], b.dr["esel"], (), [Tga], semtok=Tok())
        gtmp = b.sb([128, NTT, 32], F32, "gtmp")
        Tgt = Tok()
        for j in range(NE):
            b.tt(gtmp[:], gall[:], esel[:, j, :].unsqueeze(1).to_broadcast([128, NTT, 32]), ALU.mult, [Tga], [Tgt])
            b.red(gsel_all[:, :, j], gtmp[:], ALU.add, [Tgt], [Tg])
    else:
        b.dma(gsel_all[:], gs_d, (), [Tg])
    gselT = [b.sb([NE, G], F32, "gselT%d" % i) for i in range(2)]
    TgsT = [Tok() for _ in range(2)]
    actT = b.sb([128, KC, G], BF16, "actT")
    Tact = [Tok("act%d" % i) for i in range(KC)]
    NF = 2
    wg = [b.sb([128, KC, NF * 128], BF16, "wg%d" % i) for i in range(3)]
    wu = [b.sb([128, KC, NF * 128], BF16, "wu%d" % i) for i in range(3)]
    Twgu = [Tok() for _ in range(3)]
    wdn = [b.sb([128, KC, 512], BF16, "wdn%d" % i) for i in range(2)]
    Twdn = [Tok() for _ in range(2)]
    yacc = b.sb([128, 4, D], F32, "yacc")
    Tyacc = [[Tok() for _ in range(4)] for _ in range(4)]
    gc = [b.sb([128, G], F32, "gc%d" % i) for i in range(2)]
    sg = [b.sb([128, G], F32, "sg%d" % i) for i in range(2)]
    u1 = [b.sb([128, G], F32, "u1%d" % i) for i in range(2)]
    Tew = [Tok() for _ in range(2)]
    cnt = {"gu": 0, "dn": 0, "ew": 0}
    outevs = []
    xsrc = xT_d.rearrange("(kc p) n -> p kc n", p=128)
    wgsrc = wgu_d.rearrange("e (kc p) n -> e p kc n", p=128)
    wdsrc = wd_d.rearrange("e (kc p) n -> e p kc n", p=128)
    ydst = y_d.rearrange("(t p) n -> p t n", p=128)

    for g in range(NG):
        x, Tx = xT[g % 2], TxT[g % 2]
        gs = gsel_all[:, g * 4:(g + 1) * 4, :]
        gT, TgT = gselT[g % 2], TgsT[g % 2]
        if "xT_tiles" in b.dr:
            for t in range(4):
                b.dma(x[:, :, t * 128:(t + 1) * 128], b.dr["xT_tiles"][g * 4 + t].rearrange("p (k n) -> p k n", k=KC), (), [Tx])
        else:
            b.dma(x[:], xsrc[:, :, g * G:(g + 1) * G], (), [Tx])
        for t in range(4):
            b.tr(banks[7][0:NE, t * 128:(t + 1) * 128], gs[:, t, :], ident[:], [Tg, Tc], [Tbank[7]])
        b.cp(gT[:], banks[7][0:NE, :], [Tbank[7]], [TgT])
        for e in range(NE):
            for pc in range(KC // NF):
                wi = cnt["gu"] % 3
                cnt["gu"] += 1
                c0 = pc * NF * 128
                b.dma(wg[wi][:], wgsrc[e, :, :, c0:c0 + NF * 128], (), [Twgu[wi]], eng="pool")
                b.dma(wu[wi][:], wgsrc[e, :, :, 2048 + c0:2048 + c0 + NF * 128], (), [Twgu[wi]], eng="pool")
                for f in range(NF):
                    fc = pc * NF + f
                    bi = cnt["ew"] % 2
                    cnt["ew"] += 1
                    pg, Tpg = banks[bi * 2], Tbank[bi * 2]
                    pu, Tpu = banks[bi * 2 + 1], Tbank[bi * 2 + 1]
                    for kc in range(KC):
                        b.mm(pg[:, :], wg[wi][:, kc, f * 128:(f + 1) * 128], x[:, kc, :], kc == 0, kc == KC - 1,
                             [Twgu[wi], Tx], [Tpg])
                    for kc in range(KC):
                        b.mm(pu[:, :], wu[wi][:, kc, f * 128:(f + 1) * 128], x[:, kc, :], kc == 0, kc == KC - 1,
                             [Twgu[wi], Tx], [Tpu])
                    Te = Tew[bi]
                    b.ts(gc[bi][:], pg[:, :], bguT[:, e, fc:fc + 1], 7.0, ALU.add, ALU.min, [Tpg, Tbgu], [Te])
                    b.act(sg[bi][:], gc[bi][:], AF.Sigmoid, [Te], [Te], scale=1.702)
                    b.ts(u1[bi][:], pu[:, :], bguT[:, e, 16 + fc:17 + fc], -7.0, ALU.add, ALU.max, [Tpu, Tbgu], [Te])
                    b.ts(u1[bi][:], u1[bi][:], 7.0, 1.0, ALU.min, ALU.add, [Te], [Te])
                    b.tt(gc[bi][:], gc[bi][:], sg[bi][:], ALU.mult, [Te], [Te])
                    b.tt(actT[:, fc, :], u1[bi][:], gc[bi][:], ALU.mult, [Te], [Tact[fc]])
            for n in range(4):
                di = cnt["dn"] % 2
                cnt["dn"] += 1
                b.dma(wdn[di][:], wdsrc[e, :, :, n * 512:(n + 1) * 512], (), [Twdn[di]], eng="pool")
                for t in range(4):
                    pb, Tpb = banks[4 + t], Tbank[4 + t]
                    for fc in range(KC):
                        b.mm(pb[:, :], actT[:, fc, t * 128:(t + 1) * 128], wdn[di][:, fc, :], fc == 0,
                             fc == KC - 1, [Tact[fc], Twdn[di]], [Tpb])
                    ya = yacc[:, t, n * 512:(n + 1) * 512]
                    if e == 0:
                        b.ts(ya, pb[:, :], gs[:, t, e:e + 1], None, ALU.mult, None, [Tpb, Tg], [Tyacc[t][n]])
                    else:
                        b.stt(ya, pb[:, :], gs[:, t, e:e + 1], ya, ALU.mult, ALU.add, [Tpb, Tg], [Tyacc[t][n]])
        for n in range(4):
            for t in range(4):
                pb, Tpb = banks[4 + t], Tbank[4 + t]
                b.mm(pb[:, :], gT[:, t * 128:(t + 1) * 128], bd[:, n * 512:(n + 1) * 512], True, True, [TgT, Tbd], [Tpb])
                ya = yacc[:, t, n * 512:(n + 1) * 512]
                b.tt(ya, ya, pb[:, :], ALU.add, [Tpb], [Tyacc[t][n]])
        alltok = [Tyacc[t][n] for t in range(4) for n in range(4)]
        outevs.append(b.dma(ydst[:, g * 4:(g + 1) * 4, :], yacc[:], alltok, [], semtok=Tyacc[0][0]).ev)
    b.P.wait_end("sp", outevs)
    if own:
        return b.finish()
    b.end_phase()


def build_d(ntok=1024, final=False, NP=NCORES, b=None):
    own = b is None
    if own:
        b = Bld()
    NT = ntok // 128
    h1_d = b.dram("h1", [ntok, D], F32, "ExternalInput")
    yp_d = b.dram("yparts", [NP, ntok, D], F32, "ExternalInput")
    c_d = b.dram("cvec", [16, 128], F32, "ExternalInput")
    adaw_d = b.dram("adaw", [D, 2048], F32, "ExternalInput")
    adab_d = b.dram("adab", [1, 2048], F32, "ExternalInput")
    ident_d = b.dram("ident", [128, 128], F32, "ExternalInput")
    if final:
        fg_d = b.dram("fg", [1, D], F32, "ExternalInput")
        out_d = b.dram("out", [ntok, D], F32, "ExternalOutput")
    else:
        h2_d = b.dram("h2", [ntok, D], F32, "ExternalOutput")
        h2T_d = b.dram("h2T", [D, ntok], F32, "ExternalOutput")
    banks = [b.ps([128, 512], F32, "bank%d" % i) for i in range(8)]
    Tbank = [Tok("bank%d" % i) for i in range(8)]
    ident, ones_bf, ones_f, Tc = load_consts(b, ident_d)
    cs_col, Tcs = silu_col(b, c_d, ident, Tc, banks[7][:, 0:16], Tbank[7])
    g2b, Tg2 = ada_rows(b, adaw_d, adab_d, 2048, cs_col, Tcs, ones_f, Tc, banks, Tbank, "g2b")
    if final:
        fgb, Tfg = bcast_row(b, fg_d, D, ones_f, Tc, banks[2], Tbank[2], "fgb")
    hb = [b.sb([128, D], F32, "hb%d" % i) for i in range(2)]
    Thb = [Tok() for _ in range(2)]
    yb = [b.sb([128, D], F32, "yb%d" % i) for i in range(4)]
    Tyb = [Tok() for _ in range(4)]
    ys = b.sb([128, D], F32, "ys")
    Tys = Tok()
    tmp = b.sb([128, D], F32, "tmp")
    Ttmp = Tok()
    sm = [b.sb([128, 4], F32, "sm%d" % i) for i in range(2)]
    Tsm = [Tok() for _ in range(2)]
    hT = [b.sb([128, KC, 128], F32, "hT%d" % i) for i in range(2)]
    ThT = [Tok() for _ in range(2)]
    outevs = []
    cnt = 0
    for t in range(NT):
        rows = slice(t * 128, (t + 1) * 128)
        h, Th = hb[t % 2], Thb[t % 2]
        s, Ts = sm[t % 2], Tsm[t % 2]
        b.dma(h[:], h1_d[rows, :], (), [Th])
        for p in range(NP):
            y, Ty = yb[cnt % 4], Tyb[cnt % 4]
            b.dma(y[:], yp_d[p, rows, :], (), [Ty], eng=("sp" if cnt % 2 == 0 else "pool"))
            cnt += 1
            eng = "dve" if p % 2 == 0 else "pool"
            if p == 0:
                b.cp(ys[:], y[:], [Ty], [Tys], eng=eng)
            else:
                b.tt(ys[:], ys[:], y[:], ALU.add, [Ty, Tys], [Tys], eng=eng)
        b.tt(ys[:], ys[:], g2b[:], ALU.mult, [Tys, Tg2], [Tys])
        b.tt(h[:], h[:], ys[:], ALU.add, [Th, Tys], [Th])
        if final:
            b.tt(tmp[:], h[:], h[:], ALU.mult, [Th], [Ttmp], eng="pool")
            b.red(s[:, 0:1], tmp[:], ALU.add, [Ttmp], [Ts])
            b.act(s[:, 1:2], s[:, 0:1], AF.Ln, [Ts], [Ts], bias=RMS_EPS, scale=1.0 / D)
            b.act(s[:, 2:3], s[:, 1:2], AF.Exp, [Ts], [Ts], scale=-0.5)
            b.stt(h[:], h[:], s[:, 2:3], fgb[:], ALU.mult, ALU.mult, [Th, Ts, Tfg], [Th])
            outevs.append(b.dma(out_d[rows, :], h[:], [Th], [], semtok=Th).ev)
        else:
            outevs.append(b.dma(h2_d[rows, :], h[:], [Th], [], semtok=Th).ev)
            ht, Tht = hT[t % 2], ThT[t % 2]
            for q4 in range(4):
                pb, Tpb = banks[4 + q4 % 2], Tbank[4 + q4 % 2]
                for j in range(4):
                    kc = q4 * 4 + j
                    b.tr(pb[:, j * 128:(j + 1) * 128], h[:, kc * 128:(kc + 1) * 128], ident[:], [Th, Tc], [Tpb])
                b.cp(ht[:, q4 * 4:(q4 + 1) * 4, :], pb[:, :].rearrange("p (j q) -> p j q", j=4), [Tpb], [Tht],
                     eng=("dve" if q4 % 2 == 0 else "act"))
            outevs.append(b.dma(h2T_d.rearrange("(kc p) n -> p kc n", p=128)[:, :, rows], ht[:], [Tht], [], semtok=Tht).ev)
    b.P.wait_end("sp", outevs)
    if own:
        return b.finish()
    b.end_phase()


def build_attn_mla(seq=S, b=None):
    NCH = seq // CH
    own = b is None
    if own:
        b = Bld()
    hT_d = b.dram("hT", [D, seq], F32, "ExternalInput")
    c_d = b.dram("cvec", [16, 128], F32, "ExternalInput")
    adaw_d = b.dram("adaw", [D, 4096], F32, "ExternalInput")
    adab_d = b.dram("adab", [32, 128], F32, "ExternalInput")
    n1g_d = b.dram("n1g", [16, 128], F32, "ExternalInput")
    win_d = b.dram("win", [D, 1088], F32, "ExternalInput")
    gq_d = b.dram("gq", [4, 128], F32, "ExternalInput")
    gkv_d = b.dram("gkv", [4, 128], F32, "ExternalInput")
    wuq_d = b.dram("wuq", [512, 384], F32, "ExternalInput")
    wukv_d = b.dram("wukv", [512, 512], F32, "ExternalInput")
    pos_d = b.dram("pos", [1, seq], mybir.dt.int32, "ExternalInput")
    invf_d = b.dram("invf", [64, 1], F32, "ExternalInput")
    idx_d = b.dram("idx", [128, 3, 256], F32, "ExternalInput")
    ident_d = b.dram("ident", [128, 128], F32, "ExternalInput")
    oT_d = b.dram("oT", [256, seq], BF16, "ExternalOutput")

    banks = [b.ps([128, 512], F32, "bank%d" % i) for i in range(8)]
    Tbank = [Tok("bank%d" % i) for i in range(8)]
    hs = [banks[6][:, 0:256], banks[7][:, 0:256], banks[6][:, 256:512], banks[7][:, 256:512]]
    Ths = [Tbank[6], Tbank[7], Tbank[6], Tbank[7]]
    hsi = [0]

    def next_hs():
        i = hsi[0] % 4
        hsi[0] += 1
        return hs[i], Ths[i]

    ident, ones_bf, ones_f, Tc = load_consts(b, ident_d)
    p, Tp = next_hs()
    cs_col, Tcs = silu_col(b, c_d, ident, Tc, p[:, 0:16], Tp)
    p, Tp = next_hs()
    abT, Tab = col_from_rows(b, adab_d, 32, ident, Tc, p[:, 0:32], Tp, "abT")
    p, Tp = next_hs()
    g1T, Tg1 = col_from_rows(b, n1g_d, 16, ident, Tc, p[:, 0:16], Tp, "g1T")
    p, Tp = next_hs()
    gqT, Tgq = col_from_rows(b, gq_d, 4, ident, Tc, p[:, 0:4], Tp, "gqT")
    p, Tp = next_hs()
    gkvT, Tgkv = col_from_rows(b, gkv_d, 4, ident, Tc, p[:, 0:4], Tp, "gkvT")
    p, Tp = next_hs()
    hbuf = b.sb([128, KC, CH], F32, "hbuf")
    modT, Tm = ada_cols(b, adaw_d, 4096, cs_col, Tcs, abT, Tab, p, Tp, "modT",
                        bufs=[hbuf[:, :, 0:128], hbuf[:, :, 128:256]])
    sh1 = modT[:, 0:16]
    sc1 = modT[:, 16:32]
    gsc = b.sb([128, 16], F32, "gsc")
    Tgsc = Tok()
    b.stt(gsc[:], sc1, 1.0, g1T[:], ALU.add, ALU.mult, [Tm, Tg1], [Tgsc])
    sh1_bf = b.sb([128, 16], BF16, "sh1_bf")
    Tsh = Tok()
    b.cp(sh1_bf[:], sh1, [Tm], [Tsh])

    SCL = 192.0 ** -0.25
    Win = b.sb([128, KC, 1088 + 64], BF16, "Win")
    TW = Tok("Win")
    wsrc = win_d.rearrange("(kc p) n -> p kc n", p=128)
    for q4 in range(4):
        b.dma(Win[:, q4 * 4:(q4 + 1) * 4, 0:1088], wsrc[:, q4 * 4:(q4 + 1) * 4, :], (), [TW], eng="pool")
    b.ts(Win[:, :, 1088:1120], Win[:, :, 1056:1088], -1.0, None, ALU.mult, None, [TW], [TW])
    b.cp(Win[:, :, 1120:1152], Win[:, :, 1024:1056], [TW], [TW])
    p, Tp = next_hs()
    for f in range(10):
        if f < 8:
            cols = slice(f * 128, (f + 1) * 128)
            m = 128
        else:
            cols = slice(1024 + (f - 8) * 64, 1088 + (f - 8) * 64)
            m = 64
        for kc in range(KC):
            b.mm(p[0:m, f:f + 1], Win[:, kc, cols], sh1_bf[:, kc:kc + 1], kc == 0, kc == KC - 1, [TW, Tsh], [Tp])
    bz = b.sb([128, 10], F32, "bz")
    Tbz = Tok()
    b.memset(bz[:], 0.0, [Tbz])
    b.cp(bz[:, 0:8], p[:, 0:8], [Tp], [Tbz])
    b.cp(bz[0:64, 8:10], p[0:64, 8:10], [Tp], [Tbz])
    TWj = Tok("Win_scaled")
    for kc in range(KC):
        b.ts(Win[:, kc, :], Win[:, kc, :], gsc[:, kc:kc + 1], None, ALU.mult, None, [Tgsc, TW, Tbz], [TWj])
    Wuq = b.sb([128, 4, 384 + 128], BF16, "Wuq")
    TWq = Tok()
    b.dma(Wuq[:, :, 0:384], wuq_d.rearrange("(cc p) n -> p cc n", p=128), (), [TWq], eng="pool")
    for h in range(2):
        r0 = h * 192 + 128
        b.ts(Wuq[:, :, 384 + h * 64:384 + h * 64 + 32], Wuq[:, :, r0 + 32:r0 + 64], -1.0, None, ALU.mult, None, [TWq], [TWq])
        b.cp(Wuq[:, :, 384 + h * 64 + 32:384 + h * 64 + 64], Wuq[:, :, r0:r0 + 32], [TWq], [TWq])
    for cc in range(4):
        b.ts(Wuq[:, cc, :], Wuq[:, cc, :], gqT[:, cc:cc + 1], None, ALU.mult, None, [TWq, Tgq], [TWq])
    Wukv = b.sb([128, 4, 512], BF16, "Wukv")
    TWkv = Tok()
    b.dma(Wukv[:], wukv_d.rearrange("(cc p) n -> p cc n", p=128), (), [TWkv], eng="pool")
    for cc in range(4):
        b.ts(Wukv[:, cc, :], Wukv[:, cc, :], gkvT[:, cc:cc + 1], None, ALU.mult, None, [TWkv, Tgkv], [TWkv])

    idx = b.sb([128, 3, 256], F32, "idx")
    Tidx = Tok()
    b.dma(idx[:], idx_d, (), [Tidx])
    MSK = idx
    TM = Tok()
    b.ts(MSK[:], idx[:], 0.0, NEG, ALU.is_lt, ALU.mult, [Tidx], [TM])
    invf = b.sb([64, 1], F32, "invf")
    Tinv = Tok()
    b.dma(invf[:], invf_d, (), [Tinv])
    negpi = b.sb([64, 1], F32, "negpi")
    b.memset(negpi[:], -math.pi, [Tinv])

    kTn = b.sb([128, 2, seq], BF16, "kTn")
    kTr = b.sb([64, seq], BF16, "kTr")
    Vx = b.sb([128, seq // 128, 2, 129], BF16, "Vx")
    Tk = [Tok("k%d" % c) for c in range(NCH)]
    Tv = [Tok("v%d" % c) for c in range(NCH)]
    TV1 = Tok("vones")
    b.P.op("pool", lambda e: e.memset(Vx[:, :, :, 128:129], 1.0), [TW, TWq, TWkv], [TV1])
    qn = [b.sb([128, 2, CH], BF16, "qn%d" % i) for i in range(2)]
    qr = [b.sb([64, 2, CH], BF16, "qr%d" % i) for i in range(2)]
    Tq = [Tok("q%d" % i) for i in range(2)]
    Thb = Tok()
    sq = b.sb([128, KC, CH], BF16, "sq")
    Tsq = Tok()
    xn = [b.sb([128, KC, CH], BF16, "xn%d" % i) for i in range(1)]
    Txn = [Tok("xn%d" % i) for i in range(1)]
    rstd = b.sb([128, CH], F32, "rstd")
    Trs = Tok()
    lnv = rstd
    Tln = Trs
    zc = b.sb([128, 8, CH], F32, "zc")
    Tzc = Tok()
    zn = b.sb([128, 8, CH], BF16, "zn")
    Tzn = Tok()
    zsq = sq[:, 0:8, :]
    Tzsq = Tsq
    rq = b.sb([128, 2, CH], F32, "rq")
    Trq = Tok()
    krb = b.sb([64, 2, CH], F32, "krb")
    Tkr = Tok()
    qrb = b.sb([64, 4, CH], F32, "qrb")
    Tqr = Tok()
    posi = b.sb([64, CH], mybir.dt.int32, "posi")
    Tpos = Tok()
    ang = b.sb([64, 3, CH], F32, "ang")
    Tang = Tok()
    cs_t = b.sb([64, 2, CH], F32, "cs_t")
    Tcst = Tok()
    rtmp = b.sb([64, 2, CH], F32, "rtmp")
    Trt = Tok()
    kint = b.sb([64, CH], mybir.dt.int32, "kint")
    Tki = Tok()
    st2n = b.sb([128, 2, CH], BF16, "st2n")
    st2r = b.sb([64, 2, CH], BF16, "st2r")
    Tst2 = Tok()
    qmax = b.sb([128, NCH], F32, "qmax")
    kmax = b.sb([128, NCH + 1], F32, "kmax")
    negB = b.sb([128, NCH], F32, "negB")
    Tqm = Tok()
    Tkm = Tok()
    TnB = [Tok("nB%d" % c) for c in range(NCH)]
    b.memset(kmax[:, 0:1], 0.0, [Tkm])
    tmpm = b.sb([128, 4], F32, "tmpm")
    Ttm = Tok()
    hsrc = hT_d.rearrange("(kc p) n -> p kc n", p=128)
    hsrc_fn = b.dr.get("hT_src", lambda c: hsrc[:, :, c * CH:(c + 1) * CH])

    def stage1(c):
        t0 = c * CH
        x, Tx = xn[0], Txn[0]
        qnb, qrb_, Tqb = qn[c % 2], qr[c % 2], Tq[c % 2]
        b.dma(hbuf[:, 0:8, :], hsrc_fn(c)[:, 0:8, :], [Tm], [Thb], eng="sp")
        b.dma(hbuf[:, 8:16, :], hsrc_fn(c)[:, 8:16, :], [Tm], [Thb], eng="sp")
        b.dma(posi[:], pos_d[:, t0:t0 + CH].partition_broadcast(64), (), [Tpos], eng="sp")
        b.cp(ang[:, 0, :], posi[:], [Tpos], [Tang])
        b.ts(ang[:, 0, :], ang[:, 0, :], invf[:, 0:1], None, ALU.mult, None, [Tang, Tinv], [Tang])
        C1 = 6.28125
        C2 = 2 * math.pi - C1
        b.ts(rtmp[:, 0, :], ang[:, 0, :], 1.0 / (2 * math.pi), None, ALU.mult, None, [Tang], [Trt])
        b.cp(kint[:], rtmp[:, 0, :], [Trt], [Tki])
        b.cp(rtmp[:, 1, :], kint[:], [Tki], [Trt])
        b.stt(ang[:, 1, :], rtmp[:, 1, :], -C1, ang[:, 0, :], ALU.mult, ALU.add, [Trt, Tang], [Tang])
        b.stt(ang[:, 1, :], rtmp[:, 1, :], -C2, ang[:, 1, :], ALU.mult, ALU.add, [Trt, Tang], [Tang])
        b.ts(rtmp[:, 0, :], ang[:, 1, :], math.pi, None, ALU.is_gt, None, [Tang], [Trt])
        b.stt(ang[:, 1, :], rtmp[:, 0, :], -2 * math.pi, ang[:, 1, :], ALU.mult, ALU.add, [Trt, Tang], [Tang])
        b.ts(rtmp[:, 0, :], ang[:, 1, :], -math.pi, None, ALU.is_lt, None, [Tang], [Trt])
        b.stt(ang[:, 1, :], rtmp[:, 0, :], 2 * math.pi, ang[:, 1, :], ALU.mult, ALU.add, [Trt, Tang], [Tang])
        b.ts(ang[:, 2, :], ang[:, 1, :], 0.5 * math.pi, None, ALU.add, None, [Tang], [Tang])
        b.ts(rtmp[:, 0, :], ang[:, 2, :], math.pi, None, ALU.is_gt, None, [Tang], [Trt])
        b.stt(ang[:, 2, :], rtmp[:, 0, :], -2 * math.pi, ang[:, 2, :], ALU.mult, ALU.add, [Trt, Tang], [Tang])
        b.act(cs_t[:].rearrange("p a q -> p (a q)"), ang[:, 1:3, :].rearrange("p a q -> p (a q)"), AF.Sin,
              [Tang], [Tcst])
        sin_t, cos_t = cs_t[:, 0, :], cs_t[:, 1, :]
        b.tt(sq[:], hbuf[:], hbuf[:], ALU.mult, [Thb], [Tsq], eng="pool")
        p, Tp = next_hs()
        for kc in range(KC):
            b.mm(p, ones_bf[:], sq[:, kc, :], kc == 0, kc == KC - 1, [Tsq, Tc], [Tp])
        b.act(lnv[:], p, AF.Ln, [Tp], [Tln], bias=RMS_EPS, scale=1.0 / D)
        b.act(rstd[:], lnv[:], AF.Exp, [Tln], [Trs], scale=-0.5)
        b.tt(x[:], hbuf[:], rstd[:].unsqueeze(1).to_broadcast([128, KC, CH]), ALU.mult, [Thb, Trs], [Tx])
        for f in range(8):
            p, Tp = next_hs()
            for kc in range(KC):
                b.mm(p, Win[:, kc, f * 128:(f + 1) * 128], x[:, kc, :], kc == 0, kc == KC - 1, [TWj, Tx], [Tp])
            b.act(zc[:, f, :], p, AF.Identity, [Tp, Tbz], [Tzc], bias=bz[:, f:f + 1])
        for f in range(2):
            p, Tp = next_hs()
            for kc in range(KC):
                b.mm(p[0:64, :], Win[:, kc, 1024 + f * 64:1088 + f * 64], x[:, kc, :], kc == 0, kc == KC - 1, [TWj, Tx], [Tp])
            b.act(krb[:, f, :], p[0:64, :], AF.Identity, [Tp, Tbz], [Tkr], bias=bz[0:64, 8 + f:9 + f])
        b.tt(zsq, zc[:], zc[:], ALU.mult, [Tzc], [Tzsq], eng="pool")
        for g in range(2):
            p, Tp = next_hs()
            for cc in range(4):
                b.mm(p, ones_bf[:], zsq[:, g * 4 + cc, :], cc == 0, cc == 3, [Tzsq, Tc], [Tp])
            b.act(rq[:, g, :], p, AF.Ln, [Tp], [Trq], bias=RMS_EPS, scale=1.0 / 512)
        b.act(rq[:].rearrange("p g q -> p (g q)"), rq[:].rearrange("p g q -> p (g q)"), AF.Exp, [Trq], [Trq], scale=-0.5)
        for g in range(2):
            b.tt(zn[:, g * 4:(g + 1) * 4, :], zc[:, g * 4:(g + 1) * 4, :],
                 rq[:, g, :].unsqueeze(1).to_broadcast([128, 4, CH]), ALU.mult, [Tzc, Trq], [Tzn])
        for h in range(2):
            p, Tp = next_hs()
            for cc in range(4):
                b.mm(p, Wuq[:, cc, h * 192:h * 192 + 128], zn[:, cc, :], cc == 0, cc == 3, [TWq, Tzn], [Tp])
            b.act(qnb[:, h, :], p, AF.Copy, [Tp], [Tqb], scale=SCL)
        for h in range(2):
            for r in range(2):
                p, Tp = next_hs()
                cols = slice(h * 192 + 128, h * 192 + 192) if r == 0 else slice(384 + h * 64, 448 + h * 64)
                for cc in range(4):
                    b.mm(p[0:64, :], Wuq[:, cc, cols], zn[:, cc, :], cc == 0, cc == 3, [TWq, Tzn], [Tp])
                b.act(qrb[:, h * 2 + r, :], p[0:64, :], AF.Copy, [Tp], [Tqr], scale=SCL)
        for h in range(2):
            b.tt(rtmp[:, 0, :], qrb[:, h * 2, :], cos_t, ALU.mult, [Tqr, Tcst], [Trt])
            b.tt(rtmp[:, 1, :], qrb[:, h * 2 + 1, :], sin_t, ALU.mult, [Tqr, Tcst], [Trt])
            b.tt(qrb_[:, h, :], rtmp[:, 0, :], rtmp[:, 1, :], ALU.add, [Trt], [Tqb])
        b.tt(rtmp[:, 0, :], krb[:, 0, :], cos_t, ALU.mult, [Tkr, Tcst], [Trt])
        b.tt(rtmp[:, 1, :], krb[:, 1, :], sin_t, ALU.mult, [Tkr, Tcst], [Trt])
        b.stt(kTr[:, t0:t0 + CH], rtmp[:, 0, :], 1.0, rtmp[:, 1, :], ALU.mult, ALU.add, [Trt], [Tk[c]])
        b.ts(kTr[:, t0:t0 + CH], kTr[:, t0:t0 + CH], SCL, None, ALU.mult, None, [Tk[c]], [Tk[c]])
        for h in range(2):
            p, Tp = next_hs()
            for cc in range(4):
                b.mm(p, Wukv[:, cc, h * 128:(h + 1) * 128], zn[:, 4 + cc, :], cc == 0, cc == 3, [TWkv, Tzn], [Tp])
            b.act(kTn[:, h, t0:t0 + CH], p, AF.Copy, [Tp], [Tk[c]], scale=SCL)
        for tb in range(CH // 128):
            p, Tp = next_hs()
            for cc in range(4):
                b.mm(p, zn[:, 4 + cc, tb * 128:(tb + 1) * 128], Wukv[:, cc, 256:512], cc == 0, cc == 3, [TWkv, Tzn], [Tp])
            b.cp(Vx[:, c * 2 + tb, :, 0:128], p.rearrange("p (h v) -> p h v", h=2), [Tp, TV1], [Tv[c]])
        b.tt(st2n[:], qnb[:], qnb[:], ALU.mult, [Tqb], [Tst2], eng="pool")
        b.tt(st2r[:], qrb_[:], qrb_[:], ALU.mult, [Tqb], [Tst2], eng="pool")
        for h in range(2):
            p, Tp = next_hs()
            b.mm(p, ones_bf[:], st2n[:, h, :], True, False, [Tst2, Tc], [Tp])
            b.mm(p, ones_bf[0:64, :], st2r[:, h, :], False, True, [Tst2, Tc], [Tp])
            b.red(tmpm[:, h:h + 1], p, ALU.max, [Tp], [Ttm])
        b.tt(qmax[:, c:c + 1], tmpm[:, 0:1], tmpm[:, 1:2], ALU.max, [Ttm], [Tqm])
        b.tt(st2n[:], kTn[:, :, t0:t0 + CH], kTn[:, :, t0:t0 + CH], ALU.mult, [Tk[c]], [Tst2], eng="pool")
        b.tt(st2r[:, 0, :], kTr[:, t0:t0 + CH], kTr[:, t0:t0 + CH], ALU.mult, [Tk[c]], [Tst2], eng="pool")
        for h in range(2):
            p, Tp = next_hs()
            b.mm(p, ones_bf[:], st2n[:, h, :], True, False, [Tst2, Tc], [Tp])
            b.mm(p, ones_bf[0:64, :], st2r[:, 0, :], False, True, [Tst2, Tc], [Tp])
            b.red(tmpm[:, 2 + h:3 + h], p, ALU.max, [Tp], [Ttm])
        b.tt(tmpm[:, 2:3], tmpm[:, 2:3], tmpm[:, 3:4], ALU.max, [Ttm], [Ttm])
        b.tt(kmax[:, c + 1:c + 2], kmax[:, c:c + 1], tmpm[:, 2:3], ALU.max, [Ttm, Tkm], [Tkm])
        b.stt(negB[:, c:c + 1], qmax[:, c:c + 1], 1.0, kmax[:, c + 1:c + 2], ALU.mult, ALU.add, [Tqm, Tkm], [TnB[c]])
        b.ts(negB[:, c:c + 1], negB[:, c:c + 1], -0.5, None, ALU.mult, None, [TnB[c]], [TnB[c]])

    Sb = [banks[0], banks[1]]
    TS = [Tbank[0], Tbank[1]]
    ACC = [[banks[2], banks[3]], [banks[4], banks[5]]]
    TACC = [[Tbank[2], Tbank[3]], [Tbank[4], Tbank[5]]]
    Ssb = [b.sb([128, 2, CH], F32, "Ssb%d" % i) for i in range(1)]
    TSsb = [Tok() for _ in range(1)]
    PT = [b.sb([128, 2, CH], BF16, "PT%d" % i) for i in range(2)]
    TPT = [Tok() for _ in range(2)]
    ep = b.sb([128, 4], F32, "ep")
    Tep = Tok()
    ob = b.sb([128, 2, 128], F32, "ob")
    Tob = Tok()
    oTb = [b.sb([128, 2, CH], BF16, "oTb%d" % i) for i in range(2)]
    ToT = [Tok() for _ in range(2)]
    odst = oT_d.rearrange("(j p) n -> p j n", p=128)
    odst_fn = b.dr.get("oT_dst", lambda c: odst[:, :, c * CH:(c + 1) * CH])
    cnt = {"s": 0, "p": 0, "n": 0}
    outevs = []

    def scores(c, j):
        sbi = cnt["s"] % 2
        cnt["s"] += 1
        qnb, qrb_, Tqb = qn[c % 2], qr[c % 2], Tq[c % 2]
        for h in range(2):
            b.mm(Sb[sbi][:, h * CH:(h + 1) * CH], kTn[:, h, j * 128:(j + 1) * 128], qnb[:, h, :], True, False,
                 [Tk[j // 2], Tqb], [TS[sbi]])
            b.mm(Sb[sbi][:, h * CH:(h + 1) * CH], kTr[:, j * 128:(j + 1) * 128], qrb_[:, h, :], False, True,
                 [Tk[j // 2], Tqb], [TS[sbi]])
        return sbi

    def stage3(c):
        nj = 2 * c + 2
        sbi_next = scores(c, 0)
        for j in range(nj):
            sbi = sbi_next
            if j + 1 < nj:
                sbi_next = scores(c, j + 1)
            pi = cnt["p"] % 2
            cnt["p"] += 1
            t = j - (2 * c - 1)
            if t >= 1:
                ni = 0
                cnt["n"] += 1
                for h in range(2):
                    b.tt(Ssb[ni][:, h, :], Sb[sbi][:, h * CH:(h + 1) * CH], MSK[:, t, :], ALU.add,
                         [TS[sbi], TM], [TSsb[ni]])
                b.act(PT[pi][:].rearrange("p m q -> p (m q)"), Ssb[ni][:].rearrange("p m q -> p (m q)"), AF.Exp,
                      [TSsb[ni], TnB[c]], [TPT[pi]], bias=negB[:, c:c + 1])
            else:
                b.act(PT[pi][:].rearrange("p m q -> p (m q)"), Sb[sbi][:, :], AF.Exp,
                      [TS[sbi], TnB[c]], [TPT[pi]], bias=negB[:, c:c + 1])
            for h in range(2):
                for qb_ in range(2):
                    last = (2 * c) if qb_ == 0 else (2 * c + 1)
                    if j > last:
                        continue
                    b.mm(ACC[h][qb_][:, 0:129], PT[pi][:, h, qb_ * 128:(qb_ + 1) * 128], Vx[:, j, h, :], j == 0, j == last,
                         [TPT[pi], Tv[j // 2], TV1], [TACC[h][qb_]])
        obuf, Tobuf = oTb[c % 2], ToT[c % 2]
        for qb_ in range(2):
            for h in range(2):
                A = ACC[h][qb_]
                b.recip(ep[:, h:h + 1], A[:, 128:129], [TACC[h][qb_]], [Tep])
                b.ts(ob[:, h, :], A[:, 0:128], ep[:, h:h + 1], None, ALU.mult, None, [TACC[h][qb_], Tep], [Tob])
            p, Tp = next_hs()
            for h in range(2):
                b.tr(p[:, h * 128:(h + 1) * 128], ob[:, h, :], ident[:], [Tob, Tc], [Tp])
            b.cp(obuf[:, :, qb_ * 128:(qb_ + 1) * 128], p.rearrange("p (j q) -> p j q", j=2), [Tp], [Tobuf])
        outevs.append(b.dma(odst_fn(c), obuf[:], [Tobuf], [], eng="sp", semtok=Tobuf).ev)

    stage1(0)
    for c in range(NCH):
        if c + 1 < NCH:
            stage1(c + 1)
        stage3(c)
    b.P.wait_end("sp", outevs)
    if own:
        return b.finish()
    b.end_phase()


_IDENT = np.eye(128, dtype=np.float32)


def _idx_const():
    qi = np.arange(256)[None, :]
    ki = np.arange(128)[:, None]
    idx = np.zeros((128, 3, 256), np.float32)
    for t in range(3):
        rel = (1 - t) * 128 + qi - ki
        idx[:, t, :] = np.where(rel >= 0, t5_bucket_np(rel), -1)
    return idx


def _invf_const():
    half = 32
    inv = (np.float32(10000.0) ** (-np.arange(half, dtype=np.float32) / np.float32(half))).astype(np.float32)
    return np.concatenate([inv, inv])[:, None].astype(np.float32)


def mla_inputs(core, hT, c, ada_w_i, ada_b_i, n1g_i, w_in, gq, gkv, w_uq, w_ukv, pos):
    h0 = 2 * core
    wuq = np.ascontiguousarray(w_uq[:, h0 * 192:(h0 + 2) * 192])
    kv = w_ukv.reshape(512, 16, 256)
    wukv = np.concatenate([kv[:, h0, 0:128], kv[:, h0 + 1, 0:128], kv[:, h0, 128:256], kv[:, h0 + 1, 128:256]], axis=1)
    return {"hT": hT, "cvec": c.reshape(16, 128), "adaw": np.ascontiguousarray(ada_w_i[:, :4096]),
            "adab": ada_b_i[:4096].reshape(32, 128), "n1g": n1g_i.reshape(16, 128), "win": w_in,
            "gq": gq.reshape(4, 128), "gkv": gkv.reshape(4, 128), "wuq": wuq, "wukv": np.ascontiguousarray(wukv),
            "pos": pos.astype(np.int32), "invf": _invf_const(), "idx": _idx_const(), "ident": _IDENT}


_PROGS = {}


def _prog(name, fn):
    if name not in _PROGS:
        _PROGS[name] = fn()
    return _PROGS[name]


def _run(nc, in_maps):
    res = run_bass_kernel_spmd(nc, in_maps, core_ids=list(range(NCORES)))
    return res.results


def _attn_common(i, hT, c, ada_w, ada_b, norm1_g):
    return {"hT": hT, "cvec": c.reshape(16, 128), "adaw": np.ascontiguousarray(ada_w[i][:, :4096]),
            "adab": ada_b[i][:4096].reshape(32, 128), "n1g": norm1_g[i].reshape(16, 128), "ident": _IDENT}


def _ffn_layer(i, final, oT_all, h_tok, c, ada_w, ada_b, norm2_g, final_g, w_o, router_w, router_b,
               exp_w_gu, exp_b_gu, exp_w_down, exp_b_down):
    TPC = S // NCORES
    cvec = c.reshape(16, 128)
    adaw_b1 = np.ascontiguousarray(ada_w[i][:, 4096:10240])
    adab_b1 = np.ascontiguousarray(ada_b[i][None, 4096:10240])
    maps = []
    for k in range(NCORES):
        tok = slice(k * TPC, (k + 1) * TPC)
        maps.append({"oT": np.ascontiguousarray(oT_all[:, tok]), "h": np.ascontiguousarray(h_tok[tok]), "cvec": cvec,
                     "adaw": adaw_b1, "adab": adab_b1, "n2g": norm2_g[i][None, :], "wo": w_o, "wr": router_w[i],
                     "br": router_b[i][None, :], "ident": _IDENT})
    r = _run(_prog("b1", build_b1), maps)
    h1 = np.concatenate([np.asarray(x["h1"]) for x in r], axis=0)
    xT_all = np.concatenate([np.asarray(x["xT"]) for x in r], axis=0)
    xT_all = np.ascontiguousarray(xT_all.reshape(S // 128, 128, KC, 128).transpose(2, 1, 0, 3).reshape(D, S))
    gates = np.concatenate([np.asarray(x["gates"]).reshape(128, TPC // 128, 32).transpose(1, 0, 2).reshape(TPC, 32)
                            for x in r], axis=0)
    NE = 32 // NCORES
    maps = []
    for k in range(NCORES):
        es = slice(k * NE, (k + 1) * NE)
        maps.append({"xT": xT_all, "gsel": np.ascontiguousarray(gates[:, es].reshape(S // 128, 128, NE).transpose(1, 0, 2)), "wgu": exp_w_gu[i][es],
                     "bgu": exp_b_gu[i][es].reshape(NE, 32, 128), "wd": exp_w_down[i][es], "bd": exp_b_down[i][es],
                     "ident": _IDENT})
    r = _run(_prog("c", build_c), maps)
    yparts = [np.asarray(x["ypart"]) for x in r]
    adaw_d = np.ascontiguousarray(ada_w[i][:, 10240:12288])
    adab_d = np.ascontiguousarray(ada_b[i][None, 10240:12288])
    maps = []
    for k in range(NCORES):
        tok = slice(k * TPC, (k + 1) * TPC)
        m = {"h1": np.ascontiguousarray(h1[tok]), "yparts": np.stack([y[tok] for y in yparts], axis=0), "cvec": cvec,
             "adaw": adaw_d, "adab": adab_d, "ident": _IDENT}
        if final:
            m["fg"] = final_g[None, :]
        maps.append(m)
    if final:
        r = _run(_prog("d_final", lambda: build_d(final=True)), maps)
        return np.concatenate([np.asarray(x["out"]) for x in r], axis=0), None
    r = _run(_prog("d", lambda: build_d(final=False)), maps)
    h2 = np.concatenate([np.asarray(x["h2"]) for x in r], axis=0)
    h2T = np.concatenate([np.asarray(x["h2T"]) for x in r], axis=1)
    return h2, h2T


def kernel(x, c, positions, ada_w, ada_b, norm1_g, norm2_g, final_g, rel_bias,
           diff_w_qkv, diff_lq1, diff_lk1, diff_lq2, diff_lk2, diff_sub_g, diff_w_o,
           mla_w_in, mla_q_norm_g, mla_kv_norm_g, mla_w_uq, mla_w_ukv, mla_w_o,
           router_w, router_b, exp_w_gu, exp_b_gu, exp_w_down, exp_b_down):
    f = lambda a: np.asarray(a, dtype=np.float32)
    x, c, ada_w, ada_b, norm1_g, norm2_g, final_g, rel_bias = map(f, (x, c, ada_w, ada_b, norm1_g, norm2_g, final_g, rel_bias))
    diff_w_qkv, diff_lq1, diff_lk1, diff_lq2, diff_lk2, diff_sub_g, diff_w_o = map(
        f, (diff_w_qkv, diff_lq1, diff_lk1, diff_lq2, diff_lk2, diff_sub_g, diff_w_o))
    mla_w_in, mla_q_norm_g, mla_kv_norm_g, mla_w_uq, mla_w_ukv, mla_w_o = map(
        f, (mla_w_in, mla_q_norm_g, mla_kv_norm_g, mla_w_uq, mla_w_ukv, mla_w_o))
    router_w, router_b, exp_w_gu, exp_b_gu, exp_w_down, exp_b_down = map(
        f, (router_w, router_b, exp_w_gu, exp_b_gu, exp_w_down, exp_b_down))
    positions = np.asarray(positions).astype(np.int32)
    h_tok = x[0]
    hT = np.ascontiguousarray(h_tok.T)
    idx = _idx_const()
    base = _attn_common(0, hT, c, ada_w, ada_b, norm1_g)
    wq = diff_w_qkv[0]
    maps = []
    for k in range(NCORES):
        wslice = np.concatenate([wq[:, k * 256:(k + 1) * 256], wq[:, 2048 + k * 256:2048 + (k + 1) * 256],
                                 wq[:, 4096 + k * 256:4096 + (k + 1) * 256]], axis=1)
        rows = np.concatenate([rel_bias[:, k], diff_lq1[0], diff_lk1[0], diff_lq2[0], diff_lk2[0], diff_sub_g[0]])[None, :]
        m = dict(base)
        m.update({"wqkv": np.ascontiguousarray(wslice), "rows": np.ascontiguousarray(rows.astype(np.float32)), "idx": idx})
        maps.append(m)
    r = _run(_prog("a_diff", build_attn_diff), maps)
    oT_all = np.concatenate([np.asarray(t["oT"]) for t in r], axis=0)
    h_tok, hT = _ffn_layer(0, False, oT_all, h_tok, c, ada_w, ada_b, norm2_g, final_g, diff_w_o[0], router_w, router_b,
                           exp_w_gu, exp_b_gu, exp_w_down, exp_b_down)
    maps = [mla_inputs(k, hT, c, ada_w[1], ada_b[1], norm1_g[1], mla_w_in[0], mla_q_norm_g[0], mla_kv_norm_g[0],
                       mla_w_uq[0], mla_w_ukv[0], positions) for k in range(NCORES)]
    r = _run(_prog("a_mla", build_attn_mla), maps)
    oT_all = np.concatenate([np.asarray(t["oT"]) for t in r], axis=0)
    out, _ = _ffn_layer(1, True, oT_all, h_tok, c, ada_w, ada_b, norm2_g, final_g, mla_w_o[0], router_w, router_b,
                        exp_w_gu, exp_b_gu, exp_w_down, exp_b_down)
    return out.reshape(1, S, D).astype(np.float32)


def build_fused(seq=S):
    TPC = seq // NCORES
    NTL = TPC // 128
    cps = TPC // CH
    b = Bld()
    nc = b.nc
    I32 = mybir.dt.int32
    ext = lambda n, sh, dt=F32: nc.dram_tensor(n, list(sh), dt, kind="ExternalInput").ap()
    loc = lambda n, sh, dt=F32: nc.dram_tensor(n, list(sh), dt).ap()
    hT0 = ext("hT0", [D, seq]); xtok = ext("xtok", [TPC, D]); cvec = ext("cvec", [16, 128]); ident = ext("ident", [128, 128])
    idx = ext("idx", [128, 3, 256]); invf = ext("invf", [64, 1]); pos = ext("pos", [1, seq], I32)
    adaw = ext("adaw", [2, D, 12288]); adab = ext("adab", [2, 12288]); n1g = ext("n1g", [2, 16, 128]); n2g = ext("n2g", [2, D])
    fg = ext("fg", [1, D])
    wqkv = ext("wqkv", [D, 768]); rows = ext("rows", [1, 800]); wo0 = ext("wo0", [D, D])
    win = ext("win", [D, 1088]); gq = ext("gq", [4, 128]); gkv = ext("gkv", [4, 128]); wuq = ext("wuq", [512, 384])
    wukv = ext("wukv", [512, 512]); wo1 = ext("wo1", [D, D])
    wr = ext("wr", [2, D, 32]); br = ext("br", [2, 32]); wgu = ext("wgu", [2, 4, D, 4096]); bgu = ext("bgu", [2, 4, 32, 128])
    wd = ext("wd", [2, 4, D, D]); bd = ext("bd", [2, 4, D]); esel = ext("esel", [128, 4, 32])
    out = nc.dram_tensor("out", [TPC, D], F32, kind="ExternalOutput").ap()
    oT_loc = loc("oT_loc", [NCORES * 256, TPC], BF16); oT_x = loc("oT_x", [NCORES * 256, TPC], BF16)
    h1_loc = loc("h1_loc", [TPC, D]); xT_loc = loc("xT_loc", [NTL * 128, KC * 128], BF16); gates_loc = loc("gates_loc", [128, NTL * 32])
    xT_all = loc("xT_all", [NCORES * NTL * 128, KC * 128], BF16); gates_all = loc("gates_all", [NCORES * 128, NTL * 32])
    ypart_loc = loc("ypart_loc", [seq, D]); yparts = loc("yparts", [seq, D])
    h2_loc = loc("h2_loc", [TPC, D]); h2T_loc = loc("h2T_loc", [D, TPC]); hT_all = loc("hT_all", [NCORES * D, TPC])
    oT_loc3 = oT_loc.rearrange("(s e) n -> s e n", s=NCORES)
    hT_all3 = hT_all.rearrange("(r d) n -> r d n", r=NCORES)

    def oT_dst(c):
        return oT_loc3[c // cps].rearrange("(j p) n -> p j n", p=128)[:, :, (c % cps) * CH:(c % cps + 1) * CH]

    def exchange(kind, pairs):
        for a, o in pairs:
            b.coll(kind, a, o, (), [Tok()])
        b.end_phase()

    common = {"cvec": cvec, "ident": ident}
    for i in range(2):
        d = dict(common)
        d.update({"adaw": adaw[i][:, 0:4096], "adab": adab[i, 0:4096].rearrange("(a c) -> a c", c=128), "n1g": n1g[i],
                  "idx": idx, "oT": oT_loc3[0], "oT_dst": oT_dst})
        if i == 0:
            d.update({"hT": hT0, "wqkv": wqkv, "rows": rows})
            b.dr = d
            build_attn_diff(seq, b=b)
        else:
            d.update({"hT": hT_all3[0], "hT_src": lambda c: hT_all3[c // cps].rearrange("(kc p) n -> p kc n", p=128)[:, :, (c % cps) * CH:(c % cps + 1) * CH],
                      "win": win, "gq": gq, "gkv": gkv, "wuq": wuq, "wukv": wukv, "pos": pos, "invf": invf})
            b.dr = d
            build_attn_mla(seq, b=b)
        exchange("AllToAll", [(oT_loc, oT_x)])
        d = dict(common)
        d.update({"oT": oT_x, "h": (xtok if i == 0 else h2_loc), "adaw": adaw[i][:, 4096:10240], "adab": adab[i:i + 1, 4096:10240],
                  "n2g": n2g[i:i + 1, :], "wo": (wo0 if i == 0 else wo1), "wr": wr[i], "br": br[i:i + 1, :],
                  "h1": h1_loc, "xT": xT_loc.rearrange("(t p) f -> t p f", p=128), "gates": gates_loc})
        b.dr = d
        build_b1(TPC, b=b)
        exchange("AllGather", [(xT_loc, xT_all), (gates_loc, gates_all)])
        d = dict(common)
        d.update({"xT": xT_all, "gsel": gates_all, "xT_tiles": xT_all.rearrange("(t p) f -> t p f", p=128),
                  "gates_all": gates_all.rearrange("(r p) f -> r p f", p=128), "esel": esel,
                  "wgu": wgu[i], "bgu": bgu[i], "wd": wd[i], "bd": bd[i], "ypart": ypart_loc})
        b.dr = d
        build_c(seq, 4, b=b)
        exchange("AllToAll", [(ypart_loc, yparts)])
        d = dict(common)
        d.update({"h1": h1_loc, "yparts": yparts.rearrange("(r t) f -> r t f", r=NCORES), "adaw": adaw[i][:, 10240:12288],
                  "adab": adab[i:i + 1, 10240:12288], "fg": fg, "out": out, "h2": h2_loc, "h2T": h2T_loc})
        b.dr = d
        build_d(TPC, final=(i == 1), NP=NCORES, b=b)
        if i == 0:
            exchange("AllGather", [(h2T_loc, hT_all)])
    b.P.close()
    return nc
```

```python
import math
import contextlib
import numpy as np
import ml_dtypes
import concourse.bass as bass
import concourse.mybir as mybir
from concourse.bass_utils import run_bass_kernel_spmd

F32 = mybir.dt.float32
BF16 = mybir.dt.bfloat16
ALU = mybir.AluOpType
AF = mybir.ActivationFunctionType
AX = mybir.AxisListType

NCORES = 8
D = 2048
S = 8192
KC = D // 128
RMS_EPS = 1e-6
NEG = -30000.0

ENGS = ("pe", "act", "dve", "pool", "sp")


class Tok:
    __slots__ = ("name", "w", "r", "dsem")

    def __init__(self, name=""):
        self.name = name
        self.w = None
        self.r = []
        self.dsem = None


class Ev:
    __slots__ = ("sem", "val", "op")

    def __init__(self, sem, val=None, op=None):
        self.sem = sem
        self.val = val
        self.op = op


class Op:
    __slots__ = ("eng", "fn", "needs", "ev", "signal", "dma")

    def __init__(self, eng, fn, needs, dma):
        self.eng = eng
        self.fn = fn
        self.needs = needs
        self.ev = None
        self.signal = False
        self.dma = dma


class Prog:
    N_HW = 36
    N_SW = 20

    def __init__(self, nc):
        self.nc = nc
        self.stack = contextlib.ExitStack()
        self.sems = {}
        for e in ENGS:
            self.sems[e] = self.stack.enter_context(nc.semaphore("s_" + e))
        for i in range(self.N_HW):
            self.sems[("h", i)] = self.stack.enter_context(nc.semaphore("dh%d" % i))
        for i in range(self.N_SW):
            self.sems[("s", i)] = self.stack.enter_context(nc.semaphore("ds%d" % i))
        self.count = {k: 0 for k in self.sems}
        self.waited = {e: {} for e in ENGS}
        self.first_phase = True
        self._reset_phase()

    def _reset_phase(self):
        self.ops = {e: [] for e in ENGS}
        self.used = {"h": 0, "s": 0}

    def _needs(self, reads, writes):
        needs = []
        for t in reads:
            if t.w is not None:
                needs.append(t.w)
        for t in writes:
            if t.w is not None:
                needs.append(t.w)
            needs.extend(t.r)
        return needs

    def _finish(self, o, ev, needs, reads, writes):
        o.ev = ev
        for n in needs:
            if n.op is not None and not (n.op.eng == "pe" and o.eng == "pe"):
                n.op.signal = True
        for t in reads:
            if ev.op is not None:
                t.r = [x for x in t.r if x.sem != ev.sem]
            t.r.append(ev)
        for t in writes:
            t.w = ev
            t.r = []
        self.ops[o.eng].append(o)

    def op(self, eng, fn, reads=(), writes=()):
        needs = self._needs(reads, writes)
        o = Op(eng, fn, needs, None)
        self._finish(o, Ev(eng, None, o), needs, reads, writes)
        return o

    def dma(self, eng, fn, reads=(), writes=(), semtok=None):
        needs = self._needs(reads, writes)
        if semtok is None:
            semtok = writes[0] if writes else reads[0]
        kind = "h" if eng == "sp" else "s"
        if semtok.dsem is None or semtok.dsem[0] != kind or semtok.dsem[2] != id(self.ops):
            lim = self.N_HW if kind == "h" else self.N_SW
            idx = self.used[kind]
            assert idx < lim, "out of DMA semaphores (%s)" % kind
            self.used[kind] += 1
            semtok.dsem = (kind, idx, id(self.ops))
        key = semtok.dsem[:2]
        self.count[key] += 16
        o = Op(eng, fn, needs, key)
        self._finish(o, Ev(key, self.count[key], None), needs, reads, writes)
        return o

    def wait_end(self, eng, evs):
        needs = []
        for ev in evs:
            needs.append(ev)
            if ev.op is not None:
                ev.op.signal = True
        o = Op(eng, None, needs, None)
        o.ev = Ev(eng, None, o)
        self.ops[eng].append(o)

    def emit(self):
        nc = self.nc
        sems = self.sems
        for e in ENGS:
            for o in reversed(self.ops[e]):
                if o.dma is None and o.fn is not None:
                    o.signal = True
                    break
        for e in ENGS:
            c = self.count[e]
            for o in self.ops[e]:
                if o.dma is None and o.signal and o.fn is not None:
                    c += 1
                    o.ev.val = c
            self.count[e] = c
        barrier = None
        if not self.first_phase:
            barrier = dict(self.prev_totals)
        self.first_phase = False
        with nc.Block() as block:
            def run(engname):
                def body(engobj):
                    waited = self.waited[engname]
                    if barrier is not None:
                        for s_, v in barrier.items():
                            if v > 0 and waited.get(s_, 0) < v:
                                engobj.wait_ge(sems[s_], v)
                                waited[s_] = v
                    for o in self.ops[engname]:
                        mx = {}
                        for n in o.needs:
                            if n.val is None:
                                assert engname == "pe" and n.sem == "pe"
                                continue
                            if mx.get(n.sem, 0) < n.val:
                                mx[n.sem] = n.val
                        for s_, v in mx.items():
                            if waited.get(s_, 0) >= v:
                                continue
                            if s_ == engname and engname in ("pe", "sp"):
                                continue
                            engobj.wait_ge(sems[s_], v)
                            waited[s_] = v
                        if o.fn is None:
                            continue
                        ins = o.fn(engobj)
                        if o.dma is not None:
                            ins.then_inc(sems[o.dma], 16)
                        elif o.signal:
                            ins.then_inc(sems[engname], 1)
                return body

            block.tensor(run("pe"))
            block.scalar(run("act"))
            block.vector(run("dve"))
            block.gpsimd(run("pool"))
            block.sync(run("sp"))
        self.prev_totals = dict(self.count)
        self._reset_phase()

    def close(self):
        self.stack.close()


class Bld:
    def __init__(self):
        self.nc = bass.Bass("TRN2", target_bir_lowering=False)
        self.P = Prog(self.nc)
        self.st = contextlib.ExitStack()
        self.nt = 0
        self.phase = 0
        self.dr = {}

    def dram(self, name, shape, dt, kind):
        if name in self.dr:
            return self.dr[name]
        return self.nc.dram_tensor(name, list(shape), dt, kind=kind).ap()

    def sb(self, shape, dt, name=None):
        self.nt += 1
        nm = "sb%d_%s" % (self.phase, name or ("t%d" % self.nt))
        return self.st.enter_context(self.nc.sbuf_tensor(nm, list(shape), dt))

    def ps(self, shape, dt, name=None):
        self.nt += 1
        nm = "ps%d_%s" % (self.phase, name or ("p%d" % self.nt))
        return self.st.enter_context(self.nc.psum_tensor(nm, list(shape), dt))

    def end_phase(self):
        self.P.emit()
        self.st.close()
        self.st = contextlib.ExitStack()
        self.phase += 1
        self.dr = {}

    def finish(self):
        self.end_phase()
        self.P.close()
        return self.nc

    def mm(self, out, lhsT, rhs, start, stop, r, w):
        self.P.op("pe", lambda e: e.matmul(out, lhsT, rhs, start=start, stop=stop), r, w)

    def tr(self, out, in_, ident, r, w):
        self.P.op("pe", lambda e: e.transpose(out, in_, ident), r, w)

    def act(self, out, in_, func, r, w, bias=0.0, scale=1.0, accum=None, eng="act"):
        if accum is None:
            self.P.op(eng, lambda e: e.activation(out=out, in_=in_, func=func, bias=bias, scale=scale), r, w)
        else:
            self.P.op(eng, lambda e: e.activation(out=out, in_=in_, func=func, bias=bias, scale=scale, accum_out=accum), r, w)

    def tt(self, out, in0, in1, op, r, w, eng="dve"):
        self.P.op(eng, lambda e: e.tensor_tensor(out=out, in0=in0, in1=in1, op=op), r, w)

    def ts(self, out, in0, s1, s2, op0, op1, r, w, eng="dve"):
        if op1 is None:
            self.P.op(eng, lambda e: e.tensor_single_scalar(out=out, in_=in0, scalar=s1, op=op0), r, w)
        else:
            self.P.op(eng, lambda e: e.tensor_scalar(out=out, in0=in0, scalar1=s1, scalar2=s2, op0=op0, op1=op1), r, w)

    def stt(self, out, in0, scalar, in1, op0, op1, r, w, eng="dve"):
        self.P.op(eng, lambda e: e.scalar_tensor_tensor(out=out, in0=in0, scalar=scalar, in1=in1, op0=op0, op1=op1), r, w)

    def cp(self, out, in_, r, w, eng="dve"):
        if eng == "act":
            self.P.op(eng, lambda e: e.activation(out=out, in_=in_, func=AF.Copy), r, w)
        else:
            self.P.op(eng, lambda e: e.tensor_copy(out=out, in_=in_), r, w)

    def red(self, out, in_, op, r, w, eng="dve"):
        self.P.op(eng, lambda e: e.tensor_reduce(out=out, in_=in_, axis=AX.X, op=op), r, w)

    def recip(self, out, in_, r, w):
        self.P.op("dve", lambda e: e.reciprocal(out=out, in_=in_), r, w)

    def memset(self, ap, val, w, eng="dve"):
        self.P.op(eng, lambda e: e.memset(ap, val), (), w)

    def dma(self, out, in_, r, w, eng=None, semtok=None):
        if eng is None:
            eng = "sp"
        return self.P.dma(eng, lambda e: e.dma_start(out=out, in_=in_), r, w, semtok)

    def coll(self, kind, in_, out, r, w):
        rg = [list(range(NCORES))]
        return self.P.dma("pool", lambda e: e.collective_compute(kind, ALU.bypass, replica_groups=rg, ins=[in_], outs=[out]), r, w)


def t5_bucket_np(rel):
    n = np.maximum(rel, 0)
    nf = np.maximum(n, 1).astype(np.float32)
    large = 16 + (np.log(nf / np.float32(16)) / np.float32(math.log(128 / 16)) * np.float32(16)).astype(np.int32)
    large = np.minimum(large, 31)
    return np.where(n < 16, n, large)


def load_consts(b, ident_d):
    T = Tok("consts")
    ident = b.sb([128, 128], F32, "ident")
    b.dma(ident[:], ident_d, (), [T])
    ones_bf = b.sb([128, 128], BF16, "ones_bf")
    b.memset(ones_bf[:], 1.0, [T])
    ones_f = b.sb([128, 128], F32, "ones_f")
    b.memset(ones_f[:], 1.0, [T])
    return ident, ones_bf, ones_f, T


def col_from_rows(b, src_d, nrows, ident, Tc, psum_ap, Tps, name):
    raw = b.sb([nrows, 128], F32, name + "_raw")
    Traw = Tok(name + "_raw")
    b.dma(raw[:], src_d, (), [Traw])
    b.tr(psum_ap, raw[:], ident[0:nrows, 0:nrows], [Traw, Tc], [Tps])
    out = b.sb([128, nrows], F32, name)
    To = Tok(name)
    b.cp(out[:], psum_ap, [Tps], [To])
    return out, To


def ada_cols(b, adaw_d, ncols, cs_col, Tcs, abT, Tab, psum_ap, Tps, name, bufs=None):
    npc = ncols // 128
    if bufs is None:
        bufs = [b.sb([128, KC, 128], F32, "%s_w%d" % (name, i))[:] for i in range(2)]
    Tb = [Tok("adaw%d" % i) for i in range(2)]
    src = adaw_d.rearrange("(kc p) n -> p kc n", p=128)
    for p in range(npc):
        bb = p % 2
        b.dma(bufs[bb], src[:, :, p * 128:(p + 1) * 128], (), [Tb[bb]], eng=("sp" if p % 2 == 0 else "pool"))
        for kc in range(KC):
            b.mm(psum_ap[:, p:p + 1], bufs[bb][:, kc, :], cs_col[:, kc:kc + 1], kc == 0, kc == KC - 1,
                 [Tb[bb], Tcs], [Tps])
    modT = b.sb([128, npc], F32, name)
    Tm = Tok(name)
    b.tt(modT[:], psum_ap[:, 0:npc], abT[:, 0:npc], ALU.add, [Tps, Tab], [Tm])
    return modT, Tm


CH = 256
NCH = S // CH
DIFF_LAM_INIT = 0.8 - 0.6 * math.exp(-0.3 * 0.0)


def build_attn_diff(seq=S, b=None):
    NCH = seq // CH
    own = b is None
    if own:
        b = Bld()
    nc = b.nc
    hT_d = b.dram("hT", [D, seq], F32, "ExternalInput")
    c_d = b.dram("cvec", [16, 128], F32, "ExternalInput")
    adaw_d = b.dram("adaw", [D, 4096], F32, "ExternalInput")
    adab_d = b.dram("adab", [32, 128], F32, "ExternalInput")
    n1g_d = b.dram("n1g", [16, 128], F32, "ExternalInput")
    wqkv_d = b.dram("wqkv", [D, 768], F32, "ExternalInput")
    rows_d = b.dram("rows", [1, 800], F32, "ExternalInput")
    idx_d = b.dram("idx", [128, 3, 256], F32, "ExternalInput")
    ident_d = b.dram("ident", [128, 128], F32, "ExternalInput")
    oT_d = b.dram("oT", [256, seq], BF16, "ExternalOutput")

    banks = [b.ps([128, 512], F32, "bank%d" % i) for i in range(8)]
    Tbank = [Tok("bank%d" % i) for i in range(8)]
    hs = [banks[6][:, 0:256], banks[7][:, 0:256], banks[6][:, 256:512], banks[7][:, 256:512]]
    Ths = [Tbank[6], Tbank[7], Tbank[6], Tbank[7]]
    hsi = [0]

    def next_hs():
        i = hsi[0] % 4
        hsi[0] += 1
        return hs[i], Ths[i]

    ident, ones_bf, ones_f, Tc = load_consts(b, ident_d)

    p, Tp = next_hs()
    c_col, Tcc = col_from_rows(b, c_d, 16, ident, Tc, p[:, 0:16], Tp, "c_col")
    sig = b.sb([128, 16], F32, "sig")
    Tsig = Tok()
    b.act(sig[:], c_col[:], AF.Sigmoid, [Tcc], [Tsig])
    cs_col = b.sb([128, 16], F32, "cs_col")
    Tcs = Tok()
    b.tt(cs_col[:], c_col[:], sig[:], ALU.mult, [Tcc, Tsig], [Tcs])
    p, Tp = next_hs()
    abT, Tab = col_from_rows(b, adab_d, 32, ident, Tc, p[:, 0:32], Tp, "abT")
    p, Tp = next_hs()
    g1T, Tg1 = col_from_rows(b, n1g_d, 16, ident, Tc, p[:, 0:16], Tp, "g1T")
    p, Tp = next_hs()
    hbuf0 = b.sb([128, KC, CH], F32, "hbuf0")
    modT, Tm = ada_cols(b, adaw_d, 4096, cs_col, Tcs, abT, Tab, p, Tp, "modT",
                        bufs=[hbuf0[:, :, 0:128], hbuf0[:, :, 128:256]])
    sh1 = modT[:, 0:16]
    sc1 = modT[:, 16:32]
    gsc = b.sb([128, 16], F32, "gsc")
    Tgsc = Tok()
    b.stt(gsc[:], sc1, 1.0, g1T[:], ALU.add, ALU.mult, [Tm, Tg1], [Tgsc])
    sh1_bf = b.sb([128, 16], BF16, "sh1_bf")
    Tsh = Tok()
    b.cp(sh1_bf[:], sh1, [Tm], [Tsh])

    Wb = b.sb([128, KC, 768], BF16, "Wb")
    TW = Tok("Wb")
    wsrc = wqkv_d.rearrange("(kc p) n -> p kc n", p=128)
    for q4 in range(4):
        b.dma(Wb[:, q4 * 4:(q4 + 1) * 4, :], wsrc[:, q4 * 4:(q4 + 1) * 4, :], (), [TW], eng="pool")
    SCL = 128.0 ** -0.25
    p, Tp = next_hs()
    for f in range(4):
        for kc in range(KC):
            b.mm(p[:, f:f + 1], Wb[:, kc, f * 128:(f + 1) * 128], sh1_bf[:, kc:kc + 1], kc == 0, kc == KC - 1,
                 [TW, Tsh], [Tp])
    bqk = b.sb([128, 4], F32, "bqk")
    Tbqk = Tok()
    b.ts(bqk[:], p[:, 0:4], SCL, None, ALU.mult, None, [Tp], [Tbqk])
    p, Tp = next_hs()
    for kc in range(KC):
        b.mm(p[0:1, 0:256], sh1_bf[:, kc:kc + 1], Wb[:, kc, 512:768], kc == 0, kc == KC - 1, [TW, Tsh], [Tp])
    bv_row = b.sb([1, 256], BF16, "bv_row")
    Tbv = Tok()
    b.cp(bv_row[:], p[0:1, 0:256], [Tp], [Tbv])
    ones_row_bf = b.sb([1, 128], BF16, "ones_row_bf")
    b.memset(ones_row_bf[:], 1.0, [Tbv])
    TWj = Tok("Wb_scaled")
    for kc in range(KC):
        b.ts(Wb[:, kc, :], Wb[:, kc, :], gsc[:, kc:kc + 1], None, ALU.mult, None, [Tgsc, TW, Tbqk, Tbv], [TWj])

    rows = b.sb([1, 800], F32, "rows")
    Trows = Tok()
    b.dma(rows[:], rows_d, (), [Trows])
    bc = b.sb([128, 800], F32, "bc")
    Tbc = Tok()
    p0, Tp0 = banks[0], Tbank[0]
    p1, Tp1 = banks[1], Tbank[1]
    b.mm(p0[:, 0:512], ones_f[0:1, :], rows[:, 0:512], True, True, [Trows, Tc], [Tp0])
    b.mm(p1[:, 0:288], ones_f[0:1, :], rows[:, 512:800], True, True, [Trows, Tc], [Tp1])
    b.cp(bc[:, 0:512], p0[:, 0:512], [Tp0], [Tbc])
    b.cp(bc[:, 512:800], p1[:, 0:288], [Tp1], [Tbc])
    rb_b = bc[:, 0:32]
    small = b.sb([128, 16], F32, "small")
    Tsm = Tok()
    lt = b.sb([128, 256], F32, "lt")
    Tlt = Tok()
    b.tt(lt[:, 0:128], bc[:, 32:160], bc[:, 160:288], ALU.mult, [Tbc], [Tlt])
    b.tt(lt[:, 128:256], bc[:, 288:416], bc[:, 416:544], ALU.mult, [Tbc], [Tlt])
    b.red(small[:, 0:1], lt[:, 0:128], ALU.add, [Tlt], [Tsm])
    b.red(small[:, 1:2], lt[:, 128:256], ALU.add, [Tlt], [Tsm])
    b.act(small[:, 2:4], small[:, 0:2], AF.Exp, [Tsm], [Tsm])
    b.stt(small[:, 4:5], small[:, 3:4], -DIFF_LAM_INIT, small[:, 2:3], ALU.add, ALU.subtract, [Tsm], [Tsm])
    neg_lam = small[:, 4:5]
    b.red(small[:, 5:6], rb_b, ALU.max, [Tbc], [Tsm])
    maxbias = small[:, 5:6]
    b.tt(small[:, 6:7], bc[:, 31:32], small[:, 5:6], ALU.subtract, [Tbc, Tsm], [Tsm])
    c31mb = small[:, 6:7]
    subg_s = b.sb([128, 256], F32, "subg_s")
    Tsg = Tok()
    b.ts(subg_s[:], bc[:, 544:800], 1.0 - DIFF_LAM_INIT, None, ALU.mult, None, [Tbc], [Tsg])

    idx = b.sb([128, 3, 256], F32, "idx")
    Tidx = Tok()
    b.dma(idx[:], idx_d, (), [Tidx])
    BTN = b.sb([128, 3, 256], F32, "BTN")
    TBTN = Tok()
    eqt = b.sb([128, 3, 256], F32, "eqt")
    Teq = Tok()
    b.ts(BTN[:], idx[:], 0.0, NEG, ALU.is_lt, ALU.mult, [Tidx], [TBTN])
    for bk in range(32):
        b.ts(eqt[:], idx[:], float(bk), None, ALU.is_equal, None, [Tidx], [Teq])
        b.stt(BTN[:], eqt[:], rb_b[:, bk:bk + 1], BTN[:], ALU.mult, ALU.add, [Teq, Tbc], [TBTN])

    kT = b.sb([128, 2, seq], BF16, "kT")
    Vx = b.sb([128, seq // 128, 257], BF16, "Vx")
    Tk = [Tok("k%d" % c) for c in range(NCH)]
    Tv = [Tok("v%d" % c) for c in range(NCH)]
    TV1 = Tok("vones")
    b.P.op("pool", lambda e: e.memset(Vx[:, :, 256:257], 1.0), [TW], [TV1])
    qbuf = [b.sb([128, 2, CH], BF16, "qbuf%d" % i) for i in range(2)]
    Tq = [Tok("q%d" % i) for i in range(2)]
    hbuf = [hbuf0]
    Th = [Tok("h%d" % i) for i in range(1)]
    sq = b.sb([128, KC, CH], BF16, "sq")
    Tsq = Tok()
    xn = [b.sb([128, KC, CH], BF16, "xn%d" % i) for i in range(2)]
    Txn = [Tok("xn%d" % i) for i in range(2)]
    lnv = b.sb([128, CH], F32, "lnv")
    Tln = Tok()
    rstd = b.sb([128, CH], F32, "rstd")
    Trs = Tok()
    st2 = b.sb([128, 2, CH], BF16, "st2")
    Tst2 = Tok()
    qmax = b.sb([128, NCH], F32, "qmax")
    kmax = b.sb([128, NCH + 1], F32, "kmax")
    negB = b.sb([128, NCH, 2], F32, "negB")
    Tqm = Tok()
    Tkm = Tok()
    TnB = [Tok("nB%d" % c) for c in range(NCH)]
    b.memset(kmax[:, 0:1], 0.0, [Tkm])
    tmpm = b.sb([128, 4], F32, "tmpm")
    Ttm = Tok()

    hsrc = hT_d.rearrange("(kc p) n -> p kc n", p=128)
    hsrc_fn = b.dr.get("hT_src", lambda c: hsrc[:, :, c * CH:(c + 1) * CH])

    def stage1(c):
        t0 = c * CH
        hb, Thb = hbuf[0], Th[0]
        x, Tx = xn[c % 2], Txn[c % 2]
        qb, Tqb = qbuf[c % 2], Tq[c % 2]
        b.dma(hb[:, 0:8, :], hsrc_fn(c)[:, 0:8, :], [Tm], [Thb], eng="sp")
        b.dma(hb[:, 8:16, :], hsrc_fn(c)[:, 8:16, :], [Tm], [Thb], eng="sp")
        b.tt(sq[:], hb[:], hb[:], ALU.mult, [Thb], [Tsq], eng="pool")
        p, Tp = next_hs()
        for kc in range(KC):
            b.mm(p, ones_bf[:], sq[:, kc, :], kc == 0, kc == KC - 1, [Tsq, Tc], [Tp])
        b.act(lnv[:], p, AF.Ln, [Tp], [Tln], bias=RMS_EPS, scale=1.0 / D)
        b.act(rstd[:], lnv[:], AF.Exp, [Tln], [Trs], scale=-0.5)
        b.tt(x[:], hb[:], rstd[:].unsqueeze(1).to_broadcast([128, KC, CH]), ALU.mult, [Thb, Trs], [Tx])
        for f in range(4):
            p, Tp = next_hs()
            for kc in range(KC):
                b.mm(p, Wb[:, kc, f * 128:(f + 1) * 128], x[:, kc, :], kc == 0, kc == KC - 1, [TWj, Tx], [Tp])
            if f < 2:
                dst, Td = qb[:, f, :], Tqb
            else:
                dst, Td = kT[:, f - 2, t0:t0 + CH], Tk[c]
            b.act(dst, p, AF.Identity, [Tp, Tbqk], [Td], bias=bqk[:, f:f + 1], scale=SCL)
        for tb in range(CH // 128):
            p, Tp = next_hs()
            for kc in range(KC):
                b.mm(p, x[:, kc, tb * 128:(tb + 1) * 128], Wb[:, kc, 512:768], kc == 0, False, [TWj, Tx], [Tp])
            b.mm(p, ones_row_bf[:], bv_row[:], False, True, [Tbv], [Tp])
            b.cp(Vx[:, c * 2 + tb, 0:256], p, [Tp, TV1], [Tv[c]])
        b.tt(st2[:], qb[:], qb[:], ALU.mult, [Tqb], [Tst2], eng="pool")
        for m in range(2):
            p, Tp = next_hs()
            b.mm(p, ones_bf[:], st2[:, m, :], True, True, [Tst2, Tc], [Tp])
            b.red(tmpm[:, m:m + 1], p, ALU.max, [Tp], [Ttm])
        b.tt(qmax[:, c:c + 1], tmpm[:, 0:1], tmpm[:, 1:2], ALU.max, [Ttm], [Tqm])
        b.tt(st2[:], kT[:, :, t0:t0 + CH], kT[:, :, t0:t0 + CH], ALU.mult, [Tk[c]], [Tst2], eng="pool")
        for m in range(2):
            p, Tp = next_hs()
            b.mm(p, ones_bf[:], st2[:, m, :], True, True, [Tst2, Tc], [Tp])
            b.red(tmpm[:, 2 + m:3 + m], p, ALU.max, [Tp], [Ttm])
        b.tt(tmpm[:, 2:3], tmpm[:, 2:3], tmpm[:, 3:4], ALU.max, [Ttm], [Ttm])
        b.tt(kmax[:, c + 1:c + 2], kmax[:, c:c + 1], tmpm[:, 2:3], ALU.max, [Ttm, Tkm], [Tkm])
        b.stt(negB[:, c, 0:1], qmax[:, c:c + 1], 1.0, kmax[:, c + 1:c + 2], ALU.mult, ALU.add, [Tqm, Tkm], [TnB[c]])
        b.ts(negB[:, c, 0:1], negB[:, c, 0:1], -0.5, maxbias, ALU.mult, ALU.subtract, [Tsm, TnB[c]], [TnB[c]])
        b.tt(negB[:, c, 1:2], negB[:, c, 0:1], bc[:, 31:32], ALU.add, [TnB[c], Tbc], [TnB[c]])

    Sb = [banks[0], banks[1]]
    TS = [Tbank[0], Tbank[1]]
    ACC = [[banks[2], banks[3]], [banks[4], banks[5]]]
    TACC = [[Tbank[2], Tbank[3]], [Tbank[4], Tbank[5]]]
    Ssb = [b.sb([128, 2, CH], F32, "Ssb%d" % i) for i in range(2)]
    TSsb = [Tok() for _ in range(2)]
    PT = [b.sb([128, 2, CH], BF16, "PT%d" % i) for i in range(3)]
    TPT = [Tok() for _ in range(3)]
    ep = b.sb([128, 8], F32, "ep")
    Tep = Tok()
    t0buf = b.sb([128, 256], F32, "t0buf")
    Tt0 = Tok()
    obuf = b.sb([128, 256], F32, "obuf")
    Tob = Tok()
    osq = b.sb([128, 256], F32, "osq")
    Tosq = Tok()
    onb = b.sb([128, 256], F32, "onb")
    Ton = Tok()
    oTb = [b.sb([128, 2, CH], BF16, "oTb%d" % i) for i in range(2)]
    ToT = [Tok() for _ in range(2)]
    Tout = Tok("out")
    odst = oT_d.rearrange("(j p) n -> p j n", p=128)
    odst_fn = b.dr.get("oT_dst", lambda c: odst[:, :, c * CH:(c + 1) * CH])
    cnt = {"s": 0, "p": 0, "n": 0}
    outevs = []

    def scores(c, j):
        sbi = cnt["s"] % 2
        cnt["s"] += 1
        qb, Tqb = qbuf[c % 2], Tq[c % 2]
        for m in range(2):
            b.mm(Sb[sbi][:, m * CH:(m + 1) * CH], kT[:, m, j * 128:(j + 1) * 128], qb[:, m, :], True, True,
                 [Tk[j // 2], Tqb], [TS[sbi]])
        return sbi

    def stage3(c):
        nj = 2 * c + 2
        sbi_next = scores(c, 0)
        for j in range(nj):
            sbi = sbi_next
            if j + 1 < nj:
                sbi_next = scores(c, j + 1)
            pi = cnt["p"] % 3
            cnt["p"] += 1
            t = j - (2 * c - 1)
            if t >= 0:
                ni = cnt["n"] % 2
                cnt["n"] += 1
                for m in range(2):
                    b.tt(Ssb[ni][:, m, :], Sb[sbi][:, m * CH:(m + 1) * CH], BTN[:, t, :], ALU.add,
                         [TS[sbi], TBTN], [TSsb[ni]])
                b.act(PT[pi][:].rearrange("p m q -> p (m q)"), Ssb[ni][:].rearrange("p m q -> p (m q)"), AF.Exp,
                      [TSsb[ni], TnB[c]], [TPT[pi]], bias=negB[:, c, 0:1])
            else:
                b.act(PT[pi][:].rearrange("p m q -> p (m q)"), Sb[sbi][:, :], AF.Exp,
                      [TS[sbi], TnB[c]], [TPT[pi]], bias=negB[:, c, 1:2])
            for m in range(2):
                for qb_ in range(2):
                    last = (2 * c) if qb_ == 0 else (2 * c + 1)
                    if j > last:
                        continue
                    b.mm(ACC[m][qb_][:, 0:257], PT[pi][:, m, qb_ * 128:(qb_ + 1) * 128], Vx[:, j, :], j == 0, j == last,
                         [TPT[pi], Tv[j // 2], TV1], [TACC[m][qb_]])
        ob, Tobuf = oTb[c % 2], ToT[c % 2]
        for qb_ in range(2):
            A0, A1 = ACC[0][qb_], ACC[1][qb_]
            b.recip(ep[:, 0:1], A0[:, 256:257], [TACC[0][qb_]], [Tep])
            b.recip(ep[:, 1:2], A1[:, 256:257], [TACC[1][qb_]], [Tep])
            b.tt(ep[:, 2:3], ep[:, 1:2], neg_lam, ALU.mult, [Tep, Tsm], [Tep])
            b.ts(t0buf[:], A0[:, 0:256], ep[:, 0:1], None, ALU.mult, None, [TACC[0][qb_], Tep], [Tt0])
            b.stt(obuf[:], A1[:, 0:256], ep[:, 2:3], t0buf[:], ALU.mult, ALU.add, [TACC[1][qb_], Tep, Tt0], [Tob])
            b.tt(osq[:], obuf[:], obuf[:], ALU.mult, [Tob], [Tosq], eng="pool")
            b.red(ep[:, 3:4], osq[:], ALU.add, [Tosq], [Tep])
            b.act(ep[:, 4:5], ep[:, 3:4], AF.Ln, [Tep], [Tep], bias=RMS_EPS, scale=1.0 / 256)
            b.act(ep[:, 5:6], ep[:, 4:5], AF.Exp, [Tep], [Tep], scale=-0.5)
            b.stt(onb[:], obuf[:], ep[:, 5:6], subg_s[:], ALU.mult, ALU.mult, [Tob, Tep, Tsg], [Ton])
            p, Tp = next_hs()
            for jj in range(2):
                b.tr(p[:, jj * 128:(jj + 1) * 128], onb[:, jj * 128:(jj + 1) * 128], ident[:], [Ton, Tc], [Tp])
            b.cp(ob[:, :, qb_ * 128:(qb_ + 1) * 128], p.rearrange("p (j q) -> p j q", j=2), [Tp], [Tobuf])
        outevs.append(b.dma(odst_fn(c), ob[:], [Tobuf], [Tout], eng="sp", semtok=Tobuf).ev)

    stage1(0)
    for c in range(NCH):
        if c + 1 < NCH:
            stage1(c + 1)
        stage3(c)
    b.P.wait_end("sp", outevs)
    if own:
        return b.finish()
    b.end_phase()


def ada_rows(b, adaw_d, adab_d, ncols, cs_col, Tcs, ones_f, Tc, banks, Tbank, name):
    W = 128
    csb = b.sb([128, KC, 128], F32, name + "_csb")
    Tcsb = Tok()
    for kc in range(KC):
        b.ts(csb[:, kc, :], ones_f[:], cs_col[:, kc:kc + 1], None, ALU.mult, None, [Tcs, Tc], [Tcsb])
    abrow = [b.sb([1, W], F32, "%s_abrow%d" % (name, i)) for i in range(2)]
    Tabr = [Tok() for _ in range(2)]
    out = b.sb([128, ncols], F32, name)
    To = Tok(name)
    bufs = [b.sb([128, KC, W], F32, "%s_w%d" % (name, i)) for i in range(2)]
    Tb = [Tok() for _ in range(2)]
    src = adaw_d.rearrange("(kc p) n -> p kc n", p=128)
    for n in range(ncols // W):
        bb = n % 2
        b.dma(bufs[bb][:], src[:, :, n * W:(n + 1) * W], (), [Tb[bb]], eng="sp")
        b.dma(abrow[bb][:], adab_d[:, n * W:(n + 1) * W], (), [Tabr[bb]], eng="sp")
        pb, Tpb = banks[n % 2], Tbank[n % 2]
        for kc in range(KC):
            b.mm(pb[:, 0:W], csb[:, kc, :], bufs[bb][:, kc, :], kc == 0, False, [Tcsb, Tb[bb]], [Tpb])
        b.mm(pb[:, 0:W], ones_f[0:1, :], abrow[bb][:], False, True, [Tabr[bb], Tc], [Tpb])
        b.cp(out[:, n * W:(n + 1) * W], pb[:, 0:W], [Tpb], [To], eng=("dve" if n % 2 == 0 else "act"))
    return out, To


def silu_col(b, c_d, ident, Tc, psum_ap, Tps):
    c_col, Tcc = col_from_rows(b, c_d, 16, ident, Tc, psum_ap, Tps, "c_col")
    sig = b.sb([128, 16], F32, "sig")
    Tsig = Tok()
    b.act(sig[:], c_col[:], AF.Sigmoid, [Tcc], [Tsig])
    cs_col = b.sb([128, 16], F32, "cs_col")
    Tcs = Tok()
    b.tt(cs_col[:], c_col[:], sig[:], ALU.mult, [Tcc, Tsig], [Tcs])
    return cs_col, Tcs


def bcast_row(b, row_d, n, ones_f, Tc, bank, Tb_, name, eng="dve"):
    row = b.sb([1, n], F32, name + "_row")
    Tr = Tok()
    b.dma(row[:], row_d, (), [Tr])
    out = b.sb([128, n], F32, name)
    To = Tok()
    for i in range(0, n, 512):
        w = min(512, n - i)
        b.mm(bank[:, 0:w], ones_f[0:1, :], row[:, i:i + w], True, True, [Tr, Tc], [Tb_])
        b.cp(out[:, i:i + w], bank[:, 0:w], [Tb_], [To], eng=eng)
    return out, To


def build_b1(ntok=1024, dbg=9, b=None):
    own = b is None
    if own:
        b = Bld()
    NT = ntok // 128
    oT_d = b.dram("oT", [D, ntok], BF16, "ExternalInput")
    h_d = b.dram("h", [ntok, D], F32, "ExternalInput")
    c_d = b.dram("cvec", [16, 128], F32, "ExternalInput")
    adaw_d = b.dram("adaw", [D, 6144], F32, "ExternalInput")
    adab_d = b.dram("adab", [1, 6144], F32, "ExternalInput")
    n2g_d = b.dram("n2g", [1, D], F32, "ExternalInput")
    wo_d = b.dram("wo", [D, D], F32, "ExternalInput")
    wr_d = b.dram("wr", [D, 32], F32, "ExternalInput")
    br_d = b.dram("br", [1, 32], F32, "ExternalInput")
    ident_d = b.dram("ident", [128, 128], F32, "ExternalInput")
    h1_d = b.dram("h1", [ntok, D], F32, "ExternalOutput")
    xT_d = b.dram("xT", [ntok // 128, 128, KC * 128], BF16, "ExternalOutput")
    gates_d = b.dram("gates", [128, (ntok // 128) * 32], F32, "ExternalOutput")
    u2_d = b.dram("u2", [ntok, D], F32, "ExternalOutput")

    banks = [b.ps([128, 512], F32, "bank%d" % i) for i in range(8)]
    Tbank = [Tok("bank%d" % i) for i in range(8)]
    ident, ones_bf, ones_f, Tc = load_consts(b, ident_d)
    cs_col, Tcs = silu_col(b, c_d, ident, Tc, banks[7][:, 0:16], Tbank[7])
    modb, Tmod = ada_rows(b, adaw_d, adab_d, 6144, cs_col, Tcs, ones_f, Tc, banks, Tbank, "modb")
    g1b = modb[:, 0:2048]
    sh2b = modb[:, 2048:4096]
    sc2b = modb[:, 4096:6144]
    n2gb, Tn2 = bcast_row(b, n2g_d, D, ones_f, Tc, banks[2], Tbank[2], "n2gb")
    Tg2 = Tok()
    b.stt(n2gb[:], sc2b, 1.0, n2gb[:], ALU.add, ALU.mult, [Tmod, Tn2], [Tg2])
    brb, Tbr = bcast_row(b, br_d, 32, ones_f, Tc, banks[3], Tbank[3], "brb")
    wo = b.sb([128, KC, D], BF16, "wo")
    Two = Tok()
    wsrc = wo_d.rearrange("(kc p) n -> p kc n", p=128)
    for q4 in range(4):
        b.dma(wo[:, q4 * 4:(q4 + 1) * 4, :], wsrc[:, q4 * 4:(q4 + 1) * 4, :], (), [Two], eng="pool")
    wr = b.sb([128, KC, 32], F32, "wr")
    Twr = Tok()
    b.dma(wr[:], wr_d.rearrange("(kc p) n -> p kc n", p=128), (), [Twr])

    oTs = [b.sb([128, KC, 128], BF16, "oT%d" % i) for i in range(2)]
    ToTs = [Tok() for _ in range(2)]
    osrc = oT_d.rearrange("(kc p) n -> p kc n", p=128)

    hb = [b.sb([128, D], F32, "hb%d" % i) for i in range(2)]
    Thb = [Tok() for _ in range(2)]
    tmp = b.sb([128, D], F32, "tmp")
    Ttmp = Tok()
    u2 = b.sb([128, D], F32, "u2")
    Tu2 = Tok()
    uTf = b.sb([128, KC, 128], F32, "uTf")
    TuTf = Tok()
    uTb = [b.sb([128, KC, 128], BF16, "uTb%d" % i) for i in range(2)]
    TuTb = [Tok() for _ in range(2)]
    sm = [b.sb([128, 8], F32, "sm%d" % i) for i in range(2)]
    Tsm = [Tok() for _ in range(2)]
    rt = [b.sb([128, 4, 32], F32, "rt%d" % i) for i in range(2)]
    Trt = [Tok() for _ in range(2)]
    t8 = b.sb([128, 8], F32, "t8")
    Tt8 = Tok()
    gall = b.sb([128, NT, 32], F32, "gall")
    Tgall = Tok()
    outevs = []

    for t in range(NT if dbg >= 1 else 0):
        rows = slice(t * 128, (t + 1) * 128)
        h, Th = hb[t % 2], Thb[t % 2]
        s, Ts = sm[t % 2], Tsm[t % 2]
        r, Tr = rt[t % 2], Trt[t % 2]
        ub, Tub = uTb[t % 2], TuTb[t % 2]
        b.dma(h[:], h_d[rows, :], (), [Th])
        oT, ToT = oTs[t % 2], ToTs[t % 2]
        b.dma(oT[:], osrc[:, :, rows], (), [ToT])
        for n in range(4):
            pb, Tpb = banks[n], Tbank[n]
            for kc in range(KC):
                b.mm(pb[:, :], oT[:, kc, :], wo[:, kc, n * 512:(n + 1) * 512], kc == 0, kc == KC - 1, [ToT, Two], [Tpb])
            b.tt(tmp[:, n * 512:(n + 1) * 512], pb[:, :], g1b[:, n * 512:(n + 1) * 512], ALU.mult, [Tpb, Tmod], [Ttmp])
        b.tt(h[:], h[:], tmp[:], ALU.add, [Th, Ttmp], [Th])
        outevs.append(b.dma(h1_d[rows, :], h[:], [Th], [], semtok=Th).ev)
        if dbg < 2:
            continue
        b.tt(tmp[:], h[:], h[:], ALU.mult, [Th], [Ttmp])
        b.red(s[:, 0:1], tmp[:], ALU.add, [Ttmp], [Ts])
        b.act(s[:, 1:2], s[:, 0:1], AF.Ln, [Ts], [Ts], bias=RMS_EPS, scale=1.0 / D)
        b.act(s[:, 2:3], s[:, 1:2], AF.Exp, [Ts], [Ts], scale=-0.5)
        b.stt(u2[:], h[:], s[:, 2:3], n2gb[:], ALU.mult, ALU.mult, [Th, Ts, Tg2], [Tu2])
        b.tt(u2[:], u2[:], sh2b, ALU.add, [Tu2, Tmod], [Tu2])
        outevs.append(b.dma(u2_d[rows, :], u2[:], [Tu2], [], semtok=Tu2).ev)
        if dbg < 3:
            continue
        for q4 in range(4):
            pb, Tpb = banks[4 + q4 % 2], Tbank[4 + q4 % 2]
            for j in range(4):
                kc = q4 * 4 + j
                b.tr(pb[:, j * 128:(j + 1) * 128], u2[:, kc * 128:(kc + 1) * 128], ident[:], [Tu2, Tc], [Tpb])
            b.cp(uTf[:, q4 * 4:(q4 + 1) * 4, :], pb[:, :].rearrange("p (j q) -> p j q", j=4), [Tpb], [TuTf])
            b.cp(ub[:, q4 * 4:(q4 + 1) * 4, :], uTf[:, q4 * 4:(q4 + 1) * 4, :], [TuTf], [Tub], eng="act")
        outevs.append(b.dma(xT_d[t], ub[:].rearrange("p k n -> p (k n)"), [Tub], [], semtok=Tub).ev)
        if dbg < 4:
            continue
        pb, Tpb = banks[6], Tbank[6]
        for kc in range(KC):
            b.mm(pb[:, 0:32], uTf[:, kc, :], wr[:, kc, :], kc == 0, kc == KC - 1, [TuTf, Twr], [Tpb])
        b.tt(r[:, 0, :], pb[:, 0:32], brb[:], ALU.add, [Tpb, Tbr], [Tr])
        if dbg < 5:
            b.cp(gall[:, t, :], r[:, 0, :], [Tr], [Tgall])
            continue
        b.P.op("dve", lambda e, o_=t8[:], i_=r[:, 0, :]: e.max(out=o_, in_=i_), [Tr], [Tt8])
        b.ts(r[:, 1, :], r[:, 0, :], t8[:, 3:4], None, ALU.is_ge, None, [Tr, Tt8], [Tr])
        b.ts(s[:, 3:4], t8[:, 0:1], -1.0, None, ALU.mult, None, [Tt8], [Ts])
        b.act(r[:, 2, :], r[:, 0, :], AF.Exp, [Tr, Ts], [Tr], bias=s[:, 3:4])
        b.tt(r[:, 2, :], r[:, 2, :], r[:, 1, :], ALU.mult, [Tr], [Tr])
        b.red(s[:, 4:5], r[:, 2, :], ALU.add, [Tr], [Ts])
        b.recip(s[:, 5:6], s[:, 4:5], [Ts], [Ts])
        b.ts(gall[:, t, :], r[:, 2, :], s[:, 5:6], None, ALU.mult, None, [Tr, Ts], [Tgall])
    outevs.append(b.dma(gates_d, gall[:].rearrange("p t e -> p (t e)"), [Tgall], [], semtok=Tgall).ev)
    b.P.wait_end("sp", outevs)
    if own:
        return b.finish()
    b.end_phase()


G = 512


def build_c(ntok=S, NE=4, b=None):
    own = b is None
    if own:
        b = Bld()
    NG = ntok // G
    xT_d = b.dram("xT", [D, ntok], BF16, "ExternalInput")
    gs_d = b.dram("gsel", [128, ntok // 128, NE], F32, "ExternalInput")
    wgu_d = b.dram("wgu", [NE, D, 4096], F32, "ExternalInput")
    bgu_d = b.dram("bgu", [NE, 32, 128], F32, "ExternalInput")
    wd_d = b.dram("wd", [NE, D, D], F32, "ExternalInput")
    bd_d = b.dram("bd", [NE, D], F32, "ExternalInput")
    ident_d = b.dram("ident", [128, 128], F32, "ExternalInput")
    y_d = b.dram("ypart", [ntok, D], F32, "ExternalOutput")

    banks = [b.ps([128, 512], F32, "bank%d" % i) for i in range(8)]
    Tbank = [Tok("bank%d" % i) for i in range(8)]
    ident, ones_bf, ones_f, Tc = load_consts(b, ident_d)
    bguT = b.sb([128, NE, 32], F32, "bguT")
    Tbgu = Tok()
    for e in range(NE):
        raw = b.sb([32, 128], F32, "bgu_raw%d" % e)
        Traw = Tok()
        b.dma(raw[:], bgu_d[e], (), [Traw])
        b.tr(banks[7][:, 0:32], raw[:], ident[0:32, 0:32], [Traw, Tc], [Tbank[7]])
        b.cp(bguT[:, e, :], banks[7][:, 0:32], [Tbank[7]], [Tbgu])
    bd = b.sb([NE, D], F32, "bd")
    Tbd = Tok()
    b.dma(bd[:], bd_d, (), [Tbd])

    xT = [b.sb([128, KC, G], BF16, "xT%d" % i) for i in range(2)]
    TxT = [Tok() for _ in range(2)]
    gsel_all = b.sb([128, ntok // 128, NE], F32, "gsel_all")
    Tg = Tok()
    if "gates_all" in b.dr:
        NTT = ntok // 128
        gall = b.sb([128, NTT, 32], F32, "gates_all_sb")
        Tga = Tok()
        b.dma(gall[:].rearrange("p (r t) e -> p r (t e)", r=NCORES), b.dr["gates_all"].rearrange("r p f -> p r f"), (), [Tga])
        esel = b.sb([128, NE, 32], F32, "esel")
        b.dma(esel[:], b.dr["esel"], (), [Tga], semtok=Tok())
        gtmp = b.sb([128, NTT, 32], F32, "gtmp")
        Tgt = Tok()
        for j in range(NE):
            b.tt(gtmp[:], gall[:], esel[:, j, :].unsqueeze(1).to_broadcast([128, NTT, 32]), ALU.mult, [Tga], [Tgt])
            b.red(gsel_all[:, :, j], gtmp[:], ALU.add, [Tgt], [Tg])
    else:
        b.dma(gsel_all[:], gs_d, (), [Tg])
    gselT = [b.sb([NE, G], F32, "gselT%d" % i) for i in range(2)]
    TgsT = [Tok() for _ in range(2)]
    actT = b.sb([128, KC, G], BF16, "actT")
    Tact = [Tok("act%d" % i) for i in range(KC)]
    NF = 2
    wg = [b.sb([128, KC, NF * 128], BF16, "wg%d" % i) for i in range(3)]
    wu = [b.sb([128, KC, NF * 128], BF16, "wu%d" % i) for i in range(3)]
    Twgu = [Tok() for _ in range(3)]
    wdn = [b.sb([128, KC, 512], BF16, "wdn%d" % i) for i in range(2)]
    Twdn = [Tok() for _ in range(2)]
    yacc = b.sb([128, 4, D], F32, "yacc")
    Tyacc = [[Tok() for _ in range(4)] for _ in range(4)]
    gc = [b.sb([128, G], F32, "gc%d" % i) for i in range(2)]
    sg = [b.sb([128, G], F32, "sg%d" % i) for i in range(2)]
    u1 = [b.sb([128, G], F32, "u1%d" % i) for i in range(2)]
    Tew = [Tok() for _ in range(2)]
    cnt = {"gu": 0, "dn": 0, "ew": 0}
    outevs = []
    xsrc = xT_d.rearrange("(kc p) n -> p kc n", p=128)
    wgsrc = wgu_d.rearrange("e (kc p) n -> e p kc n", p=128)
    wdsrc = wd_d.rearrange("e (kc p) n -> e p kc n", p=128)
    ydst = y_d.rearrange("(t p) n -> p t n", p=128)

    for g in range(NG):
        x, Tx = xT[g % 2], TxT[g % 2]
        gs = gsel_all[:, g * 4:(g + 1) * 4, :]
        gT, TgT = gselT[g % 2], TgsT[g % 2]
        if "xT_tiles" in b.dr:
            for t in range(4):
                b.dma(x[:, :, t * 128:(t + 1) * 128], b.dr["xT_tiles"][g * 4 + t].rearrange("p (k n) -> p k n", k=KC), (), [Tx])
        else:
            b.dma(x[:], xsrc[:, :, g * G:(g + 1) * G], (), [Tx])
        for t in range(4):
            b.tr(banks[7][0:NE, t * 128:(t + 1) * 128], gs[:, t, :], ident[:], [Tg, Tc], [Tbank[7]])
        b.cp(gT[:], banks[7][0:NE, :], [Tbank[7]], [TgT])
        for e in range(NE):
            for pc in range(KC // NF):
                wi = cnt["gu"] % 3
                cnt["gu"] += 1
                c0 = pc * NF * 128
                b.dma(wg[wi][:], wgsrc[e, :, :, c0:c0 + NF * 128], (), [Twgu[wi]], eng="pool")
                b.dma(wu[wi][:], wgsrc[e, :, :, 2048 + c0:2048 + c0 + NF * 128], (), [Twgu[wi]], eng="pool")
                for f in range(NF):
                    fc = pc * NF + f
                    bi = cnt["ew"] % 2
                    cnt["ew"] += 1
                    pg, Tpg = banks[bi * 2], Tbank[bi * 2]
                    pu, Tpu = banks[bi * 2 + 1], Tbank[bi * 2 + 1]
                    for kc in range(KC):
                        b.mm(pg[:, :], wg[wi][:, kc, f * 128:(f + 1) * 128], x[:, kc, :], kc == 0, kc == KC - 1,
                             [Twgu[wi], Tx], [Tpg])
                    for kc in range(KC):
                        b.mm(pu[:, :], wu[wi][:, kc, f * 128:(f + 1) * 128], x[:, kc, :], kc == 0, kc == KC - 1,
                             [Twgu[wi], Tx], [Tpu])
                    Te = Tew[bi]
                    b.ts(gc[bi][:], pg[:, :], bguT[:, e, fc:fc + 1], 7.0, ALU.add, ALU.min, [Tpg, Tbgu], [Te])
                    b.act(sg[bi][:], gc[bi][:], AF.Sigmoid, [Te], [Te], scale=1.702)
                    b.ts(u1[bi][:], pu[:, :], bguT[:, e, 16 + fc:17 + fc], -7.0, ALU.add, ALU.max, [Tpu, Tbgu], [Te])
                    b.ts(u1[bi][:], u1[bi][:], 7.0, 1.0, ALU.min, ALU.add, [Te], [Te])
                    b.tt(gc[bi][:], gc[bi][:], sg[bi][:], ALU.mult, [Te], [Te])
                    b.tt(actT[:, fc, :], u1[bi][:], gc[bi][:], ALU.mult, [Te], [Tact[fc]])
            for n in range(4):
                di = cnt["dn"] % 2
                cnt["dn"] += 1
                b.dma(wdn[di][:], wdsrc[e, :, :, n * 512:(n + 1) * 512], (), [Twdn[di]], eng="pool")
                for t in range(4):
                    pb, Tpb = banks[4 + t], Tbank[4 + t]
                    for fc in range(KC):
                        b.mm(pb[:, :], actT[:, fc, t * 128:(t + 1) * 128], wdn[di][:, fc, :], fc == 0,
                             fc == KC - 1, [Tact[fc], Twdn[di]], [Tpb])
                    ya = yacc[:, t, n * 512:(n + 1) * 512]
                    if e == 0:
                        b.ts(ya, pb[:, :], gs[:, t, e:e + 1], None, ALU.mult, None, [Tpb, Tg], [Tyacc[t][n]])
                    else:
                        b.stt(ya, pb[:, :], gs[:, t, e:e + 1], ya, ALU.mult, ALU.add, [Tpb, Tg], [Tyacc[t][n]])
        for n in range(4):
            for t in range(4):
                pb, Tpb = banks[4 + t], Tbank[4 + t]
                b.mm(pb[:, :], gT[:, t * 128:(t + 1) * 128], bd[:, n * 512:(n + 1) * 512], True, True, [TgT, Tbd], [Tpb])
                ya = yacc[:, t, n * 512:(n + 1) * 512]
                b.tt(ya, ya, pb[:, :], ALU.add, [Tpb], [Tyacc[t][n]])
        alltok = [Tyacc[t][n] for t in range(4) for n in range(4)]
        outevs.append(b.dma(ydst[:, g * 4:(g + 1) * 4, :], yacc[:], alltok, [], semtok=Tyacc[0][0]).ev)
    b.P.wait_end("sp", outevs)
    if own:
        return b.finish()
    b.end_phase()


def build_d(ntok=1024, final=False, NP=NCORES, b=None):
    own = b is None
    if own:
        b = Bld()
    NT = ntok // 128
    h1_d = b.dram("h1", [ntok, D], F32, "ExternalInput")
    yp_d = b.dram("yparts", [NP, ntok, D], F32, "ExternalInput")
    c_d = b.dram("cvec", [16, 128], F32, "ExternalInput")
    adaw_d = b.dram("adaw", [D, 2048], F32, "ExternalInput")
    adab_d = b.dram("adab", [1, 2048], F32, "ExternalInput")
    ident_d = b.dram("ident", [128, 128], F32, "ExternalInput")
    if final:
        fg_d = b.dram("fg", [1, D], F32, "ExternalInput")
        out_d = b.dram("out", [ntok, D], F32, "ExternalOutput")
    else:
        h2_d = b.dram("h2", [ntok, D], F32, "ExternalOutput")
        h2T_d = b.dram("h2T", [D, ntok], F32, "ExternalOutput")
    banks = [b.ps([128, 512], F32, "bank%d" % i) for i in range(8)]
    Tbank = [Tok("bank%d" % i) for i in range(8)]
    ident, ones_bf, ones_f, Tc = load_consts(b, ident_d)
    cs_col, Tcs = silu_col(b, c_d, ident, Tc, banks[7][:, 0:16], Tbank[7])
    g2b, Tg2 = ada_rows(b, adaw_d, adab_d, 2048, cs_col, Tcs, ones_f, Tc, banks, Tbank, "g2b")
    if final:
        fgb, Tfg = bcast_row(b, fg_d, D, ones_f, Tc, banks[2], Tbank[2], "fgb")
    hb = [b.sb([128, D], F32, "hb%d" % i) for i in range(2)]
    Thb = [Tok() for _ in range(2)]
    yb = [b.sb([128, D], F32, "yb%d" % i) for i in range(4)]
    Tyb = [Tok() for _ in range(4)]
    ys = b.sb([128, D], F32, "ys")
    Tys = Tok()
    tmp = b.sb([128, D], F32, "tmp")
    Ttmp = Tok()
    sm = [b.sb([128, 4], F32, "sm%d" % i) for i in range(2)]
    Tsm = [Tok() for _ in range(2)]
    hT = [b.sb([128, KC, 128], F32, "hT%d" % i) for i in range(2)]
    ThT = [Tok() for _ in range(2)]
    outevs = []
    cnt = 0
    for t in range(NT):
        rows = slice(t * 128, (t + 1) * 128)
        h, Th = hb[t % 2], Thb[t % 2]
        s, Ts = sm[t % 2], Tsm[t % 2]
        b.dma(h[:], h1_d[rows, :], (), [Th])
        for p in range(NP):
            y, Ty = yb[cnt % 4], Tyb[cnt % 4]
            b.dma(y[:], yp_d[p, rows, :], (), [Ty], eng=("sp" if cnt % 2 == 0 else "pool"))
            cnt += 1
            eng = "dve" if p % 2 == 0 else "pool"
            if p == 0:
                b.cp(ys[:], y[:], [Ty], [Tys], eng=eng)
            else:
                b.tt(ys[:], ys[:], y[:], ALU.add, [Ty, Tys], [Tys], eng=eng)
        b.tt(ys[:], ys[:], g2b[:], ALU.mult, [Tys, Tg2], [Tys])
        b.tt(h[:], h[:], ys[:], ALU.add, [Th, Tys], [Th])
        if final:
            b.tt(tmp[:], h[:], h[:], ALU.mult, [Th], [Ttmp], eng="pool")
            b.red(s[:, 0:1], tmp[:], ALU.add, [Ttmp], [Ts])
            b.act(s[:, 1:2], s[:, 0:1], AF.Ln, [Ts], [Ts], bias=RMS_EPS, scale=1.0 / D)
            b.act(s[:, 2:3], s[:, 1:2], AF.Exp, [Ts], [Ts], scale=-0.5)
            b.stt(h[:], h[:], s[:, 2:3], fgb[:], ALU.mult, ALU.mult, [Th, Ts, Tfg], [Th])
            outevs.append(b.dma(out_d[rows, :], h[:], [Th], [], semtok=Th).ev)
        else:
            outevs.append(b.dma(h2_d[rows, :], h[:], [Th], [], semtok=Th).ev)
            ht, Tht = hT[t % 2], ThT[t % 2]
            for q4 in range(4):
                pb, Tpb = banks[4 + q4 % 2], Tbank[4 + q4 % 2]
                for j in range(4):
                    kc = q4 * 4 + j
                    b.tr(pb[:, j * 128:(j + 1) * 128], h[:, kc * 128:(kc + 1) * 128], ident[:], [Th, Tc], [Tpb])
                b.cp(ht[:, q4 * 4:(q4 + 1) * 4, :], pb[:, :].rearrange("p (j q) -> p j q", j=4), [Tpb], [Tht],
                     eng=("dve" if q4 % 2 == 0 else "act"))
            outevs.append(b.dma(h2T_d.rearrange("(kc p) n -> p kc n", p=128)[:, :, rows], ht[:], [Tht], [], semtok=Tht).ev)
    b.P.wait_end("sp", outevs)
    if own:
        return b.finish()
    b.end_phase()


def build_attn_mla(seq=S, b=None):
    NCH = seq // CH
    own = b is None
    if own:
        b = Bld()
    hT_d = b.dram("hT", [D, seq], F32, "ExternalInput")
    c_d = b.dram("cvec", [16, 128], F32, "ExternalInput")
    adaw_d = b.dram("adaw", [D, 4096], F32, "ExternalInput")
    adab_d = b.dram("adab", [32, 128], F32, "ExternalInput")
    n1g_d = b.dram("n1g", [16, 128], F32, "ExternalInput")
    win_d = b.dram("win", [D, 1088], F32, "ExternalInput")
    gq_d = b.dram("gq", [4, 128], F32, "ExternalInput")
    gkv_d = b.dram("gkv", [4, 128], F32, "ExternalInput")
    wuq_d = b.dram("wuq", [512, 384], F32, "ExternalInput")
    wukv_d = b.dram("wukv", [512, 512], F32, "ExternalInput")
    pos_d = b.dram("pos", [1, seq], mybir.dt.int32, "ExternalInput")
    invf_d = b.dram("invf", [64, 1], F32, "ExternalInput")
    idx_d = b.dram("idx", [128, 3, 256], F32, "ExternalInput")
    ident_d = b.dram("ident", [128, 128], F32, "ExternalInput")
    oT_d = b.dram("oT", [256, seq], BF16, "ExternalOutput")

    banks = [b.ps([128, 512], F32, "bank%d" % i) for i in range(8)]
    Tbank = [Tok("bank%d" % i) for i in range(8)]
    hs = [banks[6][:, 0:256], banks[7][:, 0:256], banks[6][:, 256:512], banks[7][:, 256:512]]
    Ths = [Tbank[6], Tbank[7], Tbank[6], Tbank[7]]
    hsi = [0]

    def next_hs():
        i = hsi[0] % 4
        hsi[0] += 1
        return hs[i], Ths[i]

    ident, ones_bf, ones_f, Tc = load_consts(b, ident_d)
    p, Tp = next_hs()
    cs_col, Tcs = silu_col(b, c_d, ident, Tc, p[:, 0:16], Tp)
    p, Tp = next_hs()
    abT, Tab = col_from_rows(b, adab_d, 32, ident, Tc, p[:, 0:32], Tp, "abT")
    p, Tp = next_hs()
    g1T, Tg1 = col_from_rows(b, n1g_d, 16, ident, Tc, p[:, 0:16], Tp, "g1T")
    p, Tp = next_hs()
    gqT, Tgq = col_from_rows(b, gq_d, 4, ident, Tc, p[:, 0:4], Tp, "gqT")
    p, Tp = next_hs()
    gkvT, Tgkv = col_from_rows(b, gkv_d, 4, ident, Tc, p[:, 0:4], Tp, "gkvT")
    p, Tp = next_hs()
    hbuf = b.sb([128, KC, CH], F32, "hbuf")
    modT, Tm = ada_cols(b, adaw_d, 4096, cs_col, Tcs, abT, Tab, p, Tp, "modT",
                        bufs=[hbuf[:, :, 0:128], hbuf[:, :, 128:256]])
    sh1 = modT[:, 0:16]
    sc1 = modT[:, 16:32]
    gsc = b.sb([128, 16], F32, "gsc")
    Tgsc = Tok()
    b.stt(gsc[:], sc1, 1.0, g1T[:], ALU.add, ALU.mult, [Tm, Tg1], [Tgsc])
    sh1_bf = b.sb([128, 16], BF16, "sh1_bf")
    Tsh = Tok()
    b.cp(sh1_bf[:], sh1, [Tm], [Tsh])

    SCL = 192.0 ** -0.25
    Win = b.sb([128, KC, 1088 + 64], BF16, "Win")
    TW = Tok("Win")
    wsrc = win_d.rearrange("(kc p) n -> p kc n", p=128)
    for q4 in range(4):
        b.dma(Win[:, q4 * 4:(q4 + 1) * 4, 0:1088], wsrc[:, q4 * 4:(q4 + 1) * 4, :], (), [TW], eng="pool")
    b.ts(Win[:, :, 1088:1120], Win[:, :, 1056:1088], -1.0, None, ALU.mult, None, [TW], [TW])
    b.cp(Win[:, :, 1120:1152], Win[:, :, 1024:1056], [TW], [TW])
    p, Tp = next_hs()
    for f in range(10):
        if f < 8:
            cols = slice(f * 128, (f + 1) * 128)
            m = 128
        else:
            cols = slice(1024 + (f - 8) * 64, 1088 + (f - 8) * 64)
            m = 64
        for kc in range(KC):
            b.mm(p[0:m, f:f + 1], Win[:, kc, cols], sh1_bf[:, kc:kc + 1], kc == 0, kc == KC - 1, [TW, Tsh], [Tp])
    bz = b.sb([128, 10], F32, "bz")
    Tbz = Tok()
    b.memset(bz[:], 0.0, [Tbz])
    b.cp(bz[:, 0:8], p[:, 0:8], [Tp], [Tbz])
    b.cp(bz[0:64, 8:10], p[0:64, 8:10], [Tp], [Tbz])
    TWj = Tok("Win_scaled")
    for kc in range(KC):
        b.ts(Win[:, kc, :], Win[:, kc, :], gsc[:, kc:kc + 1], None, ALU.mult, None, [Tgsc, TW, Tbz], [TWj])
    Wuq = b.sb([128, 4, 384 + 128], BF16, "Wuq")
    TWq = Tok()
    b.dma(Wuq[:, :, 0:384], wuq_d.rearrange("(cc p) n -> p cc n", p=128), (), [TWq], eng="pool")
    for h in range(2):
        r0 = h * 192 + 128
        b.ts(Wuq[:, :, 384 + h * 64:384 + h * 64 + 32], Wuq[:, :, r0 + 32:r0 + 64], -1.0, None, ALU.mult, None, [TWq], [TWq])
        b.cp(Wuq[:, :, 384 + h * 64 + 32:384 + h * 64 + 64], Wuq[:, :, r0:r0 + 32], [TWq], [TWq])
    for cc in range(4):
        b.ts(Wuq[:, cc, :], Wuq[:, cc, :], gqT[:, cc:cc + 1], None, ALU.mult, None, [TWq, Tgq], [TWq])
    Wukv = b.sb([128, 4, 512], BF16, "Wukv")
    TWkv = Tok()
    b.dma(Wukv[:], wukv_d.rearrange("(cc p) n -> p cc n", p=128), (), [TWkv], eng="pool")
    for cc in range(4):
        b.ts(Wukv[:, cc, :], Wukv[:, cc, :], gkvT[:, cc:cc + 1], None, ALU.mult, None, [TWkv, Tgkv], [TWkv])

    idx = b.sb([128, 3, 256], F32, "idx")
    Tidx = Tok()
    b.dma(idx[:], idx_d, (), [Tidx])
    MSK = idx
    TM = Tok()
    b.ts(MSK[:], idx[:], 0.0, NEG, ALU.is_lt, ALU.mult, [Tidx], [TM])
    invf = b.sb([64, 1], F32, "invf")
    Tinv = Tok()
    b.dma(invf[:], invf_d, (), [Tinv])
    negpi = b.sb([64, 1], F32, "negpi")
    b.memset(negpi[:], -math.pi, [Tinv])

    kTn = b.sb([128, 2, seq], BF16, "kTn")
    kTr = b.sb([64, seq], BF16, "kTr")
    Vx = b.sb([128, seq // 128, 2, 129], BF16, "Vx")
    Tk = [Tok("k%d" % c) for c in range(NCH)]
    Tv = [Tok("v%d" % c) for c in range(NCH)]
    TV1 = Tok("vones")
    b.P.op("pool", lambda e: e.memset(Vx[:, :, :, 128:129], 1.0), [TW, TWq, TWkv], [TV1])
    qn = [b.sb([128, 2, CH], BF16, "qn%d" % i) for i in range(2)]
    qr = [b.sb([64, 2, CH], BF16, "qr%d" % i) for i in range(2)]
    Tq = [Tok("q%d" % i) for i in range(2)]
    Thb = Tok()
    sq = b.sb([128, KC, CH], BF16, "sq")
    Tsq = Tok()
    xn = [b.sb([128, KC, CH], BF16, "xn%d" % i) for i in range(1)]
    Txn = [Tok("xn%d" % i) for i in range(1)]
    rstd = b.sb([128, CH], F32, "rstd")
    Trs = Tok()
    lnv = rstd
    Tln = Trs
    zc = b.sb([128, 8, CH], F32, "zc")
    Tzc = Tok()
    zn = b.sb([128, 8, CH], BF16, "zn")
    Tzn = Tok()
    zsq = sq[:, 0:8, :]
    Tzsq = Tsq
    rq = b.sb([128, 2, CH], F32, "rq")
    Trq = Tok()
    krb = b.sb([64, 2, CH], F32, "krb")
    Tkr = Tok()
    qrb = b.sb([64, 4, CH], F32, "qrb")
    Tqr = Tok()
    posi = b.sb([64, CH], mybir.dt.int32, "posi")
    Tpos = Tok()
    ang = b.sb([64, 3, CH], F32, "ang")
    Tang = Tok()
    cs_t = b.sb([64, 2, CH], F32, "cs_t")
    Tcst = Tok()
    rtmp = b.sb([64, 2, CH], F32, "rtmp")
    Trt = Tok()
    kint = b.sb([64, CH], mybir.dt.int32, "kint")
    Tki = Tok()
    st2n = b.sb([128, 2, CH], BF16, "st2n")
    st2r = b.sb([64, 2, CH], BF16, "st2r")
    Tst2 = Tok()
    qmax = b.sb([128, NCH], F32, "qmax")
    kmax = b.sb([128, NCH + 1], F32, "kmax")
    negB = b.sb([128, NCH], F32, "negB")
    Tqm = Tok()
    Tkm = Tok()
    TnB = [Tok("nB%d" % c) for c in range(NCH)]
    b.memset(kmax[:, 0:1], 0.0, [Tkm])
    tmpm = b.sb([128, 4], F32, "tmpm")
    Ttm = Tok()
    hsrc = hT_d.rearrange("(kc p) n -> p kc n", p=128)
    hsrc_fn = b.dr.get("hT_src", lambda c: hsrc[:, :, c * CH:(c + 1) * CH])

    def stage1(c):
        t0 = c * CH
        x, Tx = xn[0], Txn[0]
        qnb, qrb_, Tqb = qn[c % 2], qr[c % 2], Tq[c % 2]
        b.dma(hbuf[:, 0:8, :], hsrc_fn(c)[:, 0:8, :], [Tm], [Thb], eng="sp")
        b.dma(hbuf[:, 8:16, :], hsrc_fn(c)[:, 8:16, :], [Tm], [Thb], eng="sp")
        b.dma(posi[:], pos_d[:, t0:t0 + CH].partition_broadcast(64), (), [Tpos], eng="sp")
        b.cp(ang[:, 0, :], posi[:], [Tpos], [Tang])
        b.ts(ang[:, 0, :], ang[:, 0, :], invf[:, 0:1], None, ALU.mult, None, [Tang, Tinv], [Tang])
        C1 = 6.28125
        C2 = 2 * math.pi - C1
        b.ts(rtmp[:, 0, :], ang[:, 0, :], 1.0 / (2 * math.pi), None, ALU.mult, None, [Tang], [Trt])
        b.cp(kint[:], rtmp[:, 0, :], [Trt], [Tki])
        b.cp(rtmp[:, 1, :], kint[:], [Tki], [Trt])
        b.stt(ang[:, 1, :], rtmp[:, 1, :], -C1, ang[:, 0, :], ALU.mult, ALU.add, [Trt, Tang], [Tang])
        b.stt(ang[:, 1, :], rtmp[:, 1, :], -C2, ang[:, 1, :], ALU.mult, ALU.add, [Trt, Tang], [Tang])
        b.ts(rtmp[:, 0, :], ang[:, 1, :], math.pi, None, ALU.is_gt, None, [Tang], [Trt])
        b.stt(ang[:, 1, :], rtmp[:, 0, :], -2 * math.pi, ang[:, 1, :], ALU.mult, ALU.add, [Trt, Tang], [Tang])
        b.ts(rtmp[:, 0, :], ang[:, 1, :], -math.pi, None, ALU.is_lt, None, [Tang], [Trt])
        b.stt(ang[:, 1, :], rtmp[:, 0, :], 2 * math.pi, ang[:, 1, :], ALU.mult, ALU.add, [Trt, Tang], [Tang])
        b.ts(ang[:, 2, :], ang[:, 1, :], 0.5 * math.pi, None, ALU.add, None, [Tang], [Tang])
        b.ts(rtmp[:, 0, :], ang[:, 2, :], math.pi, None, ALU.is_gt, None, [Tang], [Trt])
        b.stt(ang[:, 2, :], rtmp[:, 0, :], -2 * math.pi, ang[:, 2, :], ALU.mult, ALU.add, [Trt, Tang], [Tang])
        b.act(cs_t[:].rearrange("p a q -> p (a q)"), ang[:, 1:3, :].rearrange("p a q -> p (a q)"), AF.Sin,
              [Tang], [Tcst])
        sin_t, cos_t = cs_t[:, 0, :], cs_t[:, 1, :]
        b.tt(sq[:], hbuf[:], hbuf[:], ALU.mult, [Thb], [Tsq], eng="pool")
        p, Tp = next_hs()
        for kc in range(KC):
            b.mm(p, ones_bf[:], sq[:, kc, :], kc == 0, kc == KC - 1, [Tsq, Tc], [Tp])
        b.act(lnv[:], p, AF.Ln, [Tp], [Tln], bias=RMS_EPS, scale=1.0 / D)
        b.act(rstd[:], lnv[:], AF.Exp, [Tln], [Trs], scale=-0.5)
        b.tt(x[:], hbuf[:], rstd[:].unsqueeze(1).to_broadcast([128, KC, CH]), ALU.mult, [Thb, Trs], [Tx])
        for f in range(8):
            p, Tp = next_hs()
            for kc in range(KC):
                b.mm(p, Win[:, kc, f * 128:(f + 1) * 128], x[:, kc, :], kc == 0, kc == KC - 1, [TWj, Tx], [Tp])
            b.act(zc[:, f, :], p, AF.Identity, [Tp, Tbz], [Tzc], bias=bz[:, f:f + 1])
        for f in range(2):
            p, Tp = next_hs()
            for kc in range(KC):
                b.mm(p[0:64, :], Win[:, kc, 1024 + f * 64:1088 + f * 64], x[:, kc, :], kc == 0, kc == KC - 1, [TWj, Tx], [Tp])
            b.act(krb[:, f, :], p[0:64, :], AF.Identity, [Tp, Tbz], [Tkr], bias=bz[0:64, 8 + f:9 + f])
        b.tt(zsq, zc[:], zc[:], ALU.mult, [Tzc], [Tzsq], eng="pool")
        for g in range(2):
            p, Tp = next_hs()
            for cc in range(4):
                b.mm(p, ones_bf[:], zsq[:, g * 4 + cc, :], cc == 0, cc == 3, [Tzsq, Tc], [Tp])
            b.act(rq[:, g, :], p, AF.Ln, [Tp], [Trq], bias=RMS_EPS, scale=1.0 / 512)
        b.act(rq[:].rearrange("p g q -> p (g q)"), rq[:].rearrange("p g q -> p (g q)"), AF.Exp, [Trq], [Trq], scale=-0.5)
        for g in range(2):
            b.tt(zn[:, g * 4:(g + 1) * 4, :], zc[:, g * 4:(g + 1) * 4, :],
                 rq[:, g, :].unsqueeze(1).to_broadcast([128, 4, CH]), ALU.mult, [Tzc, Trq], [Tzn])
        for h in range(2):
            p, Tp = next_hs()
            for cc in range(4):
                b.mm(p, Wuq[:, cc, h * 192:h * 192 + 128], zn[:, cc, :], cc == 0, cc == 3, [TWq, Tzn], [Tp])
            b.act(qnb[:, h, :], p, AF.Copy, [Tp], [Tqb], scale=SCL)
        for h in range(2):
            for r in range(2):
                p, Tp = next_hs()
                cols = slice(h * 192 + 128, h * 192 + 192) if r == 0 else slice(384 + h * 64, 448 + h * 64)
                for cc in range(4):
                    b.mm(p[0:64, :], Wuq[:, cc, cols], zn[:, cc, :], cc == 0, cc == 3, [TWq, Tzn], [Tp])
                b.act(qrb[:, h * 2 + r, :], p[0:64, :], AF.Copy, [Tp], [Tqr], scale=SCL)
        for h in range(2):
            b.tt(rtmp[:, 0, :], qrb[:, h * 2, :], cos_t, ALU.mult, [Tqr, Tcst], [Trt])
            b.tt(rtmp[:, 1, :], qrb[:, h * 2 + 1, :], sin_t, ALU.mult, [Tqr, Tcst], [Trt])
            b.tt(qrb_[:, h, :], rtmp[:, 0, :], rtmp[:, 1, :], ALU.add, [Trt], [Tqb])
        b.tt(rtmp[:, 0, :], krb[:, 0, :], cos_t, ALU.mult, [Tkr, Tcst], [Trt])
        b.tt(rtmp[:, 1, :], krb[:, 1, :], sin_t, ALU.mult, [Tkr, Tcst], [Trt])
        b.stt(kTr[:, t0:t0 + CH], rtmp[:, 0, :], 1.0, rtmp[:, 1, :], ALU.mult, ALU.add, [Trt], [Tk[c]])
        b.ts(kTr[:, t0:t0 + CH], kTr[:, t0:t0 + CH], SCL, None, ALU.mult, None, [Tk[c]], [Tk[c]])
        for h in range(2):
            p, Tp = next_hs()
            for cc in range(4):
                b.mm(p, Wukv[:, cc, h * 128:(h + 1) * 128], zn[:, 4 + cc, :], cc == 0, cc == 3, [TWkv, Tzn], [Tp])
            b.act(kTn[:, h, t0:t0 + CH], p, AF.Copy, [Tp], [Tk[c]], scale=SCL)
        for tb in range(CH // 128):
            p, Tp = next_hs()
            for cc in range(4):
                b.mm(p, zn[:, 4 + cc, tb * 128:(tb + 1) * 128], Wukv[:, cc, 256:512], cc == 0, cc == 3, [TWkv, Tzn], [Tp])
            b.cp(Vx[:, c * 2 + tb, :, 0:128], p.rearrange("p (h v) -> p h v", h=2), [Tp, TV1], [Tv[c]])
        b.tt(st2n[:], qnb[:], qnb[:], ALU.mult, [Tqb], [Tst2], eng="pool")
        b.tt(st2r[:], qrb_[:], qrb_[:], ALU.mult, [Tqb], [Tst2], eng="pool")
        for h in range(2):
            p, Tp = next_hs()
            b.mm(p, ones_bf[:], st2n[:, h, :], True, False, [Tst2, Tc], [Tp])
            b.mm(p, ones_bf[0:64, :], st2r[:, h, :], False, True, [Tst2, Tc], [Tp])
            b.red(tmpm[:, h:h + 1], p, ALU.max, [Tp], [Ttm])
        b.tt(qmax[:, c:c + 1], tmpm[:, 0:1], tmpm[:, 1:2], ALU.max, [Ttm], [Tqm])
        b.tt(st2n[:], kTn[:, :, t0:t0 + CH], kTn[:, :, t0:t0 + CH], ALU.mult, [Tk[c]], [Tst2], eng="pool")
        b.tt(st2r[:, 0, :], kTr[:, t0:t0 + CH], kTr[:, t0:t0 + CH], ALU.mult, [Tk[c]], [Tst2], eng="pool")
        for h in range(2):
            p, Tp = next_hs()
            b.mm(p, ones_bf[:], st2n[:, h, :], True, False, [Tst2, Tc], [Tp])
            b.mm(p, ones_bf[0:64, :], st2r[:, 0, :], False, True, [Tst2, Tc], [Tp])
            b.red(tmpm[:, 2 + h:3 + h], p, ALU.max, [Tp], [Ttm])
        b.tt(tmpm[:, 2:3], tmpm[:, 2:3], tmpm[:, 3:4], ALU.max, [Ttm], [Ttm])
        b.tt(kmax[:, c + 1:c + 2], kmax[:, c:c + 1], tmpm[:, 2:3], ALU.max, [Ttm, Tkm], [Tkm])
        b.stt(negB[:, c:c + 1], qmax[:, c:c + 1], 1.0, kmax[:, c + 1:c + 2], ALU.mult, ALU.add, [Tqm, Tkm], [TnB[c]])
        b.ts(negB[:, c:c + 1], negB[:, c:c + 1], -0.5, None, ALU.mult, None, [TnB[c]], [TnB[c]])

    Sb = [banks[0], banks[1]]
    TS = [Tbank[0], Tbank[1]]
    ACC = [[banks[2], banks[3]], [banks[4], banks[5]]]
    TACC = [[Tbank[2], Tbank[3]], [Tbank[4], Tbank[5]]]
    Ssb = [b.sb([128, 2, CH], F32, "Ssb%d" % i) for i in range(1)]
    TSsb = [Tok() for _ in range(1)]
    PT = [b.sb([128, 2, CH], BF16, "PT%d" % i) for i in range(2)]
    TPT = [Tok() for _ in range(2)]
    ep = b.sb([128, 4], F32, "ep")
    Tep = Tok()
    ob = b.sb([128, 2, 128], F32, "ob")
    Tob = Tok()
    oTb = [b.sb([128, 2, CH], BF16, "oTb%d" % i) for i in range(2)]
    ToT = [Tok() for _ in range(2)]
    odst = oT_d.rearrange("(j p) n -> p j n", p=128)
    odst_fn = b.dr.get("oT_dst", lambda c: odst[:, :, c * CH:(c + 1) * CH])
    cnt = {"s": 0, "p": 0, "n": 0}
    outevs = []

    def scores(c, j):
        sbi = cnt["s"] % 2
        cnt["s"] += 1
        qnb, qrb_, Tqb = qn[c % 2], qr[c % 2], Tq[c % 2]
        for h in range(2):
            b.mm(Sb[sbi][:, h * CH:(h + 1) * CH], kTn[:, h, j * 128:(j + 1) * 128], qnb[:, h, :], True, False,
                 [Tk[j // 2], Tqb], [TS[sbi]])
            b.mm(Sb[sbi][:, h * CH:(h + 1) * CH], kTr[:, j * 128:(j + 1) * 128], qrb_[:, h, :], False, True,
                 [Tk[j // 2], Tqb], [TS[sbi]])
        return sbi

    def stage3(c):
        nj = 2 * c + 2
        sbi_next = scores(c, 0)
        for j in range(nj):
            sbi = sbi_next
            if j + 1 < nj:
                sbi_next = scores(c, j + 1)
            pi = cnt["p"] % 2
            cnt["p"] += 1
            t = j - (2 * c - 1)
            if t >= 1:
                ni = 0
                cnt["n"] += 1
                for h in range(2):
                    b.tt(Ssb[ni][:, h, :], Sb[sbi][:, h * CH:(h + 1) * CH], MSK[:, t, :], ALU.add,
                         [TS[sbi], TM], [TSsb[ni]])
                b.act(PT[pi][:].rearrange("p m q -> p (m q)"), Ssb[ni][:].rearrange("p m q -> p (m q)"), AF.Exp,
                      [TSsb[ni], TnB[c]], [TPT[pi]], bias=negB[:, c:c + 1])
            else:
                b.act(PT[pi][:].rearrange("p m q -> p (m q)"), Sb[sbi][:, :], AF.Exp,
                      [TS[sbi], TnB[c]], [TPT[pi]], bias=negB[:, c:c + 1])
            for h in range(2):
                for qb_ in range(2):
                    last = (2 * c) if qb_ == 0 else (2 * c + 1)
                    if j > last:
                        continue
                    b.mm(ACC[h][qb_][:, 0:129], PT[pi][:, h, qb_ * 128:(qb_ + 1) * 128], Vx[:, j, h, :], j == 0, j == last,
                         [TPT[pi], Tv[j // 2], TV1], [TACC[h][qb_]])
        obuf, Tobuf = oTb[c % 2], ToT[c % 2]
        for qb_ in range(2):
            for h in range(2):
                A = ACC[h][qb_]
                b.recip(ep[:, h:h + 1], A[:, 128:129], [TACC[h][qb_]], [Tep])
                b.ts(ob[:, h, :], A[:, 0:128], ep[:, h:h + 1], None, ALU.mult, None, [TACC[h][qb_], Tep], [Tob])
            p, Tp = next_hs()
            for h in range(2):
                b.tr(p[:, h * 128:(h + 1) * 128], ob[:, h, :], ident[:], [Tob, Tc], [Tp])
            b.cp(obuf[:, :, qb_ * 128:(qb_ + 1) * 128], p.rearrange("p (j q) -> p j q", j=2), [Tp], [Tobuf])
        outevs.append(b.dma(odst_fn(c), obuf[:], [Tobuf], [], eng="sp", semtok=Tobuf).ev)

    stage1(0)
    for c in range(NCH):
        if c + 1 < NCH:
            stage1(c + 1)
        stage3(c)
    b.P.wait_end("sp", outevs)
    if own:
        return b.finish()
    b.end_phase()


_IDENT = np.eye(128, dtype=np.float32)


def _idx_const():
    qi = np.arange(256)[None, :]
    ki = np.arange(128)[:, None]
    idx = np.zeros((128, 3, 256), np.float32)
    for t in range(3):
        rel = (1 - t) * 128 + qi - ki
        idx[:, t, :] = np.where(rel >= 0, t5_bucket_np(rel), -1)
    return idx


def _invf_const():
    half = 32
    inv = (np.float32(10000.0) ** (-np.arange(half, dtype=np.float32) / np.float32(half))).astype(np.float32)
    return np.concatenate([inv, inv])[:, None].astype(np.float32)


def mla_inputs(core, hT, c, ada_w_i, ada_b_i, n1g_i, w_in, gq, gkv, w_uq, w_ukv, pos):
    h0 = 2 * core
    wuq = np.ascontiguousarray(w_uq[:, h0 * 192:(h0 + 2) * 192])
    kv = w_ukv.reshape(512, 16, 256)
    wukv = np.concatenate([kv[:, h0, 0:128], kv[:, h0 + 1, 0:128], kv[:, h0, 128:256], kv[:, h0 + 1, 128:256]], axis=1)
    return {"hT": hT, "cvec": c.reshape(16, 128), "adaw": np.ascontiguousarray(ada_w_i[:, :4096]),
            "adab": ada_b_i[:4096].reshape(32, 128), "n1g": n1g_i.reshape(16, 128), "win": w_in,
            "gq": gq.reshape(4, 128), "gkv": gkv.reshape(4, 128), "wuq": wuq, "wukv": np.ascontiguousarray(wukv),
            "pos": pos.astype(np.int32), "invf": _invf_const(), "idx": _idx_const(), "ident": _IDENT}


_PROGS = {}


def _prog(name, fn):
    if name not in _PROGS:
        _PROGS[name] = fn()
    return _PROGS[name]


def _run(nc, in_maps):
    res = run_bass_kernel_spmd(nc, in_maps, core_ids=list(range(NCORES)))
    return res.results


def _attn_common(i, hT, c, ada_w, ada_b, norm1_g):
    return {"hT": hT, "cvec": c.reshape(16, 128), "adaw": np.ascontiguousarray(ada_w[i][:, :4096]),
            "adab": ada_b[i][:4096].reshape(32, 128), "n1g": norm1_g[i].reshape(16, 128), "ident": _IDENT}


def _ffn_layer(i, final, oT_all, h_tok, c, ada_w, ada_b, norm2_g, final_g, w_o, router_w, router_b,
               exp_w_gu, exp_b_gu, exp_w_down, exp_b_down):
    TPC = S // NCORES
    cvec = c.reshape(16, 128)
    adaw_b1 = np.ascontiguousarray(ada_w[i][:, 4096:10240])
    adab_b1 = np.ascontiguousarray(ada_b[i][None, 4096:10240])
    maps = []
    for k in range(NCORES):
        tok = slice(k * TPC, (k + 1) * TPC)
        maps.append({"oT": np.ascontiguousarray(oT_all[:, tok]), "h": np.ascontiguousarray(h_tok[tok]), "cvec": cvec,
                     "adaw": adaw_b1, "adab": adab_b1, "n2g": norm2_g[i][None, :], "wo": w_o, "wr": router_w[i],
                     "br": router_b[i][None, :], "ident": _IDENT})
    r = _run(_prog("b1", build_b1), maps)
    h1 = np.concatenate([np.asarray(x["h1"]) for x in r], axis=0)
    xT_all = np.concatenate([np.asarray(x["xT"]) for x in r], axis=0)
    xT_all = np.ascontiguousarray(xT_all.reshape(S // 128, 128, KC, 128).transpose(2, 1, 0, 3).reshape(D, S))
    gates = np.concatenate([np.asarray(x["gates"]).reshape(128, TPC // 128, 32).transpose(1, 0, 2).reshape(TPC, 32)
                            for x in r], axis=0)
    u2_all = np.concatenate([np.asarray(x["u2"]) for x in r], axis=0)
    NE = 32 // NCORES
    cst = cs_consts(S)
    maps = []
    for k in range(NCORES):
        es = slice(k * NE, (k + 1) * NE)
        maps.append({"xtok": u2_all, "gsel": np.ascontiguousarray(gates[:, es].reshape(S // 128, 128, NE).transpose(1, 0, 2)), "wgu": exp_w_gu[i][es],
                     "bgu": exp_b_gu[i][es].reshape(NE, 32, 128), "wd": exp_w_down[i][es], "bd": exp_b_down[i][es],
                     "ident": _IDENT, "cst": cst})
    r = _run(_prog("cs", lambda: build_cs(S, NE, 8)), maps)
    yparts = [np.asarray(x["ypart"]) for x in r]
    adaw_d = np.ascontiguousarray(ada_w[i][:, 10240:12288])
    adab_d = np.ascontiguousarray(ada_b[i][None, 10240:12288])
    maps = []
    for k in range(NCORES):
        tok = slice(k * TPC, (k + 1) * TPC)
        m = {"h1": np.ascontiguousarray(h1[tok]), "yparts": np.stack([y[tok] for y in yparts], axis=0), "cvec": cvec,
             "adaw": adaw_d, "adab": adab_d, "ident": _IDENT}
        if final:
            m["fg"] = final_g[None, :]
        maps.append(m)
    if final:
        r = _run(_prog("d_final", lambda: build_d(final=True)), maps)
        return np.concatenate([np.asarray(x["out"]) for x in r], axis=0), None
    r = _run(_prog("d", lambda: build_d(final=False)), maps)
    h2 = np.concatenate([np.asarray(x["h2"]) for x in r], axis=0)
    h2T = np.concatenate([np.asarray(x["h2T"]) for x in r], axis=1)
    return h2, h2T


def kernel(x, c, positions, ada_w, ada_b, norm1_g, norm2_g, final_g, rel_bias,
           diff_w_qkv, diff_lq1, diff_lk1, diff_lq2, diff_lk2, diff_sub_g, diff_w_o,
           mla_w_in, mla_q_norm_g, mla_kv_norm_g, mla_w_uq, mla_w_ukv, mla_w_o,
           router_w, router_b, exp_w_gu, exp_b_gu, exp_w_down, exp_b_down):
    f = lambda a: np.asarray(a, dtype=np.float32)
    x, c, ada_w, ada_b, norm1_g, norm2_g, final_g, rel_bias = map(f, (x, c, ada_w, ada_b, norm1_g, norm2_g, final_g, rel_bias))
    diff_w_qkv, diff_lq1, diff_lk1, diff_lq2, diff_lk2, diff_sub_g, diff_w_o = map(
        f, (diff_w_qkv, diff_lq1, diff_lk1, diff_lq2, diff_lk2, diff_sub_g, diff_w_o))
    mla_w_in, mla_q_norm_g, mla_kv_norm_g, mla_w_uq, mla_w_ukv, mla_w_o = map(
        f, (mla_w_in, mla_q_norm_g, mla_kv_norm_g, mla_w_uq, mla_w_ukv, mla_w_o))
    router_w, router_b, exp_w_gu, exp_b_gu, exp_w_down, exp_b_down = map(
        f, (router_w, router_b, exp_w_gu, exp_b_gu, exp_w_down, exp_b_down))
    positions = np.asarray(positions).astype(np.int32)
    h_tok = x[0]
    hT = np.ascontiguousarray(h_tok.T)
    idx = _idx_const()
    base = _attn_common(0, hT, c, ada_w, ada_b, norm1_g)
    wq = diff_w_qkv[0]
    maps = []
    for k in range(NCORES):
        wslice = np.concatenate([wq[:, k * 256:(k + 1) * 256], wq[:, 2048 + k * 256:2048 + (k + 1) * 256],
                                 wq[:, 4096 + k * 256:4096 + (k + 1) * 256]], axis=1)
        rows = np.concatenate([rel_bias[:, k], diff_lq1[0], diff_lk1[0], diff_lq2[0], diff_lk2[0], diff_sub_g[0]])[None, :]
        m = dict(base)
        m.update({"wqkv": np.ascontiguousarray(wslice), "rows": np.ascontiguousarray(rows.astype(np.float32)), "idx": idx})
        maps.append(m)
    r = _run(_prog("a_diff", build_attn_diff), maps)
    oT_all = np.concatenate([np.asarray(t["oT"]) for t in r], axis=0)
    h_tok, hT = _ffn_layer(0, False, oT_all, h_tok, c, ada_w, ada_b, norm2_g, final_g, diff_w_o[0], router_w, router_b,
                           exp_w_gu, exp_b_gu, exp_w_down, exp_b_down)
    maps = [mla_inputs(k, hT, c, ada_w[1], ada_b[1], norm1_g[1], mla_w_in[0], mla_q_norm_g[0], mla_kv_norm_g[0],
                       mla_w_uq[0], mla_w_ukv[0], positions) for k in range(NCORES)]
    r = _run(_prog("a_mla", build_attn_mla), maps)
    oT_all = np.concatenate([np.asarray(t["oT"]) for t in r], axis=0)
    out, _ = _ffn_layer(1, True, oT_all, h_tok, c, ada_w, ada_b, norm2_g, final_g, mla_w_o[0], router_w, router_b,
                        exp_w_gu, exp_b_gu, exp_w_down, exp_b_down)
    return out.reshape(1, S, D).astype(np.float32)


def build_fused(seq=S):
    TPC = seq // NCORES
    NTL = TPC // 128
    cps = TPC // CH
    b = Bld()
    nc = b.nc
    I32 = mybir.dt.int32
    ext = lambda n, sh, dt=F32: nc.dram_tensor(n, list(sh), dt, kind="ExternalInput").ap()
    loc = lambda n, sh, dt=F32: nc.dram_tensor(n, list(sh), dt).ap()
    hT0 = ext("hT0", [D, seq]); xtok = ext("xtok", [TPC, D]); cvec = ext("cvec", [16, 128]); ident = ext("ident", [128, 128])
    idx = ext("idx", [128, 3, 256]); invf = ext("invf", [64, 1]); pos = ext("pos", [1, seq], I32)
    adaw = ext("adaw", [2, D, 12288]); adab = ext("adab", [2, 12288]); n1g = ext("n1g", [2, 16, 128]); n2g = ext("n2g", [2, D])
    fg = ext("fg", [1, D])
    wqkv = ext("wqkv", [D, 768]); rows = ext("rows", [1, 800]); wo0 = ext("wo0", [D, D])
    win = ext("win", [D, 1088]); gq = ext("gq", [4, 128]); gkv = ext("gkv", [4, 128]); wuq = ext("wuq", [512, 384])
    wukv = ext("wukv", [512, 512]); wo1 = ext("wo1", [D, D])
    wr = ext("wr", [2, D, 32]); br = ext("br", [2, 32]); wgu = ext("wgu", [2, 4, D, 4096]); bgu = ext("bgu", [2, 4, 32, 128])
    wd = ext("wd", [2, 4, D, D]); bd = ext("bd", [2, 4, D]); esel = ext("esel", [128, 4, 32])
    out = nc.dram_tensor("out", [TPC, D], F32, kind="ExternalOutput").ap()
    oT_loc = loc("oT_loc", [NCORES * 256, TPC], BF16); oT_x = loc("oT_x", [NCORES * 256, TPC], BF16)
    h1_loc = loc("h1_loc", [TPC, D]); xT_loc = loc("xT_loc", [NTL * 128, KC * 128], BF16); gates_loc = loc("gates_loc", [128, NTL * 32])
    xT_all = loc("xT_all", [NCORES * NTL * 128, KC * 128], BF16); gates_all = loc("gates_all", [NCORES * 128, NTL * 32])
    ypart_loc = loc("ypart_loc", [seq, D]); yparts = loc("yparts", [seq, D])
    h2_loc = loc("h2_loc", [TPC, D]); h2T_loc = loc("h2T_loc", [D, TPC]); hT_all = loc("hT_all", [NCORES * D, TPC])
    oT_loc3 = oT_loc.rearrange("(s e) n -> s e n", s=NCORES)
    hT_all3 = hT_all.rearrange("(r d) n -> r d n", r=NCORES)

    def oT_dst(c):
        return oT_loc3[c // cps].rearrange("(j p) n -> p j n", p=128)[:, :, (c % cps) * CH:(c % cps + 1) * CH]

    def exchange(kind, pairs):
        for a, o in pairs:
            b.coll(kind, a, o, (), [Tok()])
        b.end_phase()

    common = {"cvec": cvec, "ident": ident}
    for i in range(2):
        d = dict(common)
        d.update({"adaw": adaw[i][:, 0:4096], "adab": adab[i, 0:4096].rearrange("(a c) -> a c", c=128), "n1g": n1g[i],
                  "idx": idx, "oT": oT_loc3[0], "oT_dst": oT_dst})
        if i == 0:
            d.update({"hT": hT0, "wqkv": wqkv, "rows": rows})
            b.dr = d
            build_attn_diff(seq, b=b)
        else:
            d.update({"hT": hT_all3[0], "hT_src": lambda c: hT_all3[c // cps].rearrange("(kc p) n -> p kc n", p=128)[:, :, (c % cps) * CH:(c % cps + 1) * CH],
                      "win": win, "gq": gq, "gkv": gkv, "wuq": wuq, "wukv": wukv, "pos": pos, "invf": invf})
            b.dr = d
            build_attn_mla(seq, b=b)
        exchange("AllToAll", [(oT_loc, oT_x)])
        d = dict(common)
        d.update({"oT": oT_x, "h": (xtok if i == 0 else h2_loc), "adaw": adaw[i][:, 4096:10240], "adab": adab[i:i + 1, 4096:10240],
                  "n2g": n2g[i:i + 1, :], "wo": (wo0 if i == 0 else wo1), "wr": wr[i], "br": br[i:i + 1, :],
                  "h1": h1_loc, "xT": xT_loc.rearrange("(t p) f -> t p f", p=128), "gates": gates_loc})
        b.dr = d
        build_b1(TPC, b=b)
        exchange("AllGather", [(xT_loc, xT_all), (gates_loc, gates_all)])
        d = dict(common)
        d.update({"xT": xT_all, "gsel": gates_all, "xT_tiles": xT_all.rearrange("(t p) f -> t p f", p=128),
                  "gates_all": gates_all.rearrange("(r p) f -> r p f", p=128), "esel": esel,
                  "wgu": wgu[i], "bgu": bgu[i], "wd": wd[i], "bd": bd[i], "ypart": ypart_loc})
        b.dr = d
        build_c(seq, 4, b=b)
        exchange("AllToAll", [(ypart_loc, yparts)])
        d = dict(common)
        d.update({"h1": h1_loc, "yparts": yparts.rearrange("(r t) f -> r t f", r=NCORES), "adaw": adaw[i][:, 10240:12288],
                  "adab": adab[i:i + 1, 10240:12288], "fg": fg, "out": out, "h2": h2_loc, "h2T": h2T_loc})
        b.dr = d
        build_d(TPC, final=(i == 1), NP=NCORES, b=b)
        if i == 0:
            exchange("AllGather", [(h2T_loc, hT_all)])
    b.P.close()
    return nc


def build_cs(ntok=S, NE=4, NB=8):
    b = Bld()
    nc = b.nc
    I32 = mybir.dt.int32
    NTT = ntok // 128
    xtok_d = b.dram("xtok", [ntok, D], F32, "ExternalInput")
    gs_d = b.dram("gsel", [128, NTT, NE], F32, "ExternalInput")
    wgu_d = b.dram("wgu", [NE, D, 4096], F32, "ExternalInput")
    bgu_d = b.dram("bgu", [NE, 32, 128], F32, "ExternalInput")
    wd_d = b.dram("wd", [NE, D, D], F32, "ExternalInput")
    bd_d = b.dram("bd", [NE, D], F32, "ExternalInput")
    ident_d = b.dram("ident", [128, 128], F32, "ExternalInput")
    cst_d = b.dram("cst", [128, 128 + 512 + 2 * NTT], F32, "ExternalInput")
    y_d = b.dram("ypart", [ntok, D], F32, "ExternalOutput")

    banks = [b.ps([128, 512], F32, "bank%d" % i) for i in range(8)]
    Tbank = [Tok("bank%d" % i) for i in range(8)]
    ident, ones_bf, ones_f, Tc = load_consts(b, ident_d)
    cst = b.sb([128, 128 + 512 + 2 * NTT], F32, "cst")
    Tcst = Tok()
    b.dma(cst[:], cst_d, (), [Tcst])
    U_bf = b.sb([128, 128], BF16, "U_bf")
    b.cp(U_bf[:], cst[:, 0:128], [Tcst], [Tcst])
    iota = cst[:, 128:640]
    bguT = b.sb([128, NE, 32], F32, "bguT")
    Tbgu = Tok()
    for e in range(NE):
        raw = b.sb([32, 128], F32, "bgu_raw%d" % e)
        Traw = Tok()
        b.dma(raw[:], bgu_d[e], (), [Traw])
        b.tr(banks[7][:, 0:32], raw[:], ident[0:32, 0:32], [Traw, Tc], [Tbank[7]])
        b.cp(bguT[:, e, :], banks[7][:, 0:32], [Tbank[7]], [Tbgu])
    bd = b.sb([NE, D], F32, "bd")
    Tbd = Tok()
    b.dma(bd[:], bd_d, (), [Tbd])
    gsel = b.sb([128, NTT, NE], F32, "gsel")
    Tg = Tok()
    b.dma(gsel[:], gs_d, (), [Tg])
    Ty = Tok("ypart")

    gT = b.sb([NE, 128], F32, "gT")
    TgT = Tok()
    yinit = [b.sb([128, D], F32, "yinit%d" % i) for i in range(1)]
    Tyi = [Tok() for _ in range(1)]
    for t in range(NTT):
        b.tr(banks[6][0:NE, 0:128], gsel[:, t, :], ident[:], [Tg, Tc], [Tbank[6]])
        b.cp(gT[:], banks[6][0:NE, 0:128], [Tbank[6]], [TgT])
        yi, Ti = yinit[0], Tyi[0]
        for n in range(4):
            b.mm(banks[n][:, :], gT[:], bd[:, n * 512:(n + 1) * 512], True, True, [TgT, Tbd], [Tbank[n]])
            b.cp(yi[:, n * 512:(n + 1) * 512], banks[n][:, :], [Tbank[n]], [Ti], eng=("dve" if n % 2 == 0 else "act"))
        b.dma(y_d[t * 128:(t + 1) * 128, :], yi[:], [Ti], [Ty], semtok=Ti)

    mask = b.sb([128, NE, NTT], BF16, "mask")
    maskf = b.sb([128, NE, NTT], F32, "maskf")
    Tmk = Tok()
    for j in range(NE):
        b.ts(maskf[:, j, :], gsel[:, :, j], 0.0, None, ALU.is_gt, None, [Tg], [Tmk])
    b.cp(mask[:], maskf[:], [Tmk], [Tmk])
    posm = b.sb([128, NE, NTT], F32, "posm")
    Tpos = Tok()
    sc = [b.sb([128, NTT], F32, "scan%d" % i) for i in range(2)]
    Tsc = Tok()
    for j in range(NE):
        b.mm(banks[4][:, 0:NTT], U_bf[:], mask[:, j, :], True, True, [Tmk, Tcst], [Tbank[4]])
        b.mm(banks[5][:, 0:NTT], ones_bf[:], mask[:, j, :], True, True, [Tmk, Tc], [Tbank[5]])
        b.cp(sc[0][:], banks[5][:, 0:NTT], [Tbank[5]], [Tsc])
        cur = 0
        d_ = 1
        while d_ < NTT:
            b.cp(sc[1 - cur][:, 0:d_], sc[cur][:, 0:d_], [Tsc], [Tsc])
            b.tt(sc[1 - cur][:, d_:NTT], sc[cur][:, d_:NTT], sc[cur][:, 0:NTT - d_], ALU.add, [Tsc], [Tsc])
            cur = 1 - cur
            d_ *= 2
        b.tt(sc[1 - cur][:], sc[cur][:], banks[5][:, 0:NTT], ALU.subtract, [Tsc, Tbank[5]], [Tsc])
        b.tt(posm[:, j, :], sc[1 - cur][:], banks[4][:, 0:NTT], ALU.add, [Tsc, Tbank[4]], [Tpos])
        b.stt(posm[:, j, :], posm[:, j, :], 1.0, maskf[:, j, :], ALU.add, ALU.mult, [Tpos, Tmk], [Tpos])
        b.ts(posm[:, j, :], posm[:, j, :], -1.0, None, ALU.add, None, [Tpos], [Tpos])
    info = b.sb([128, NE, NTT, 5], BF16, "info")
    Tinfo = Tok()
    ghi = b.sb([128, NTT, NE], BF16, "ghi")
    glo = b.sb([128, NTT, NE], F32, "glo")
    b.cp(ghi[:], gsel[:], [Tg], [Tinfo])
    b.tt(glo[:], gsel[:], ghi[:], ALU.subtract, [Tg, Tinfo], [Tinfo])
    for j in range(NE):
        b.cp(info[:, j, :, 0], cst[:, 640:640 + NTT], [Tcst], [Tinfo])
        b.cp(info[:, j, :, 1], cst[:, 640 + NTT:640 + 2 * NTT], [Tcst], [Tinfo])
        b.cp(info[:, j, :, 2], ghi[:, :, j], [Tinfo], [Tinfo])
        b.cp(info[:, j, :, 3], glo[:, :, j], [Tinfo], [Tinfo])
    b.memset(info[:, :, :, 4], 1.0, [Tinfo])

    oh = [b.sb([128, 512], BF16, "oh%d" % i) for i in range(4)]
    Toh = [Tok() for _ in range(4)]
    sinfo = [b.sb([128, 4, 8], F32, "sinfo%d" % i) for i in range(2)]
    sidx = [b.sb([128, 4], I32, "sidx%d" % i) for i in range(2)]
    sgate = [b.sb([128, 4], F32, "sgate%d" % i) for i in range(2)]
    Tsx = [Tok() for _ in range(2)]
    xg = [yinit[0], b.sb([128, D], F32, "xg1")]
    Txg = [Tyi[0], Tok()]
    for i in range(2):
        b.memset(xg[i][:], 0.0, [Txg[i]])
    x = b.sb([128, KC, G], BF16, "xblk")
    Tx = Tok()
    actT = b.sb([128, KC, G], BF16, "actT")
    Tact = [Tok("act%d" % i) for i in range(KC)]
    NF = 2
    wg = [b.sb([128, KC, NF * 128], BF16, "wg%d" % i) for i in range(3)]
    wu = [b.sb([128, KC, NF * 128], BF16, "wu%d" % i) for i in range(3)]
    Twgu = [Tok() for _ in range(3)]
    wdn = [b.sb([128, KC, 512], BF16, "wdn%d" % i) for i in range(2)]
    Twdn = [Tok() for _ in range(2)]
    ysb = [b.sb([128, D], F32, "ysb%d" % i) for i in range(4)]
    Tys = [Tok() for _ in range(4)]
    gc = [b.sb([128, G], F32, "gc%d" % i) for i in range(2)]
    sg_ = [b.sb([128, G], F32, "sg%d" % i) for i in range(2)]
    u1 = [b.sb([128, G], F32, "u1%d" % i) for i in range(2)]
    Tew = [Tok() for _ in range(2)]
    cnt = {"gu": 0, "dn": 0, "ew": 0, "oh": 0, "xg": 0}
    wgsrc = wgu_d.rearrange("e (kc p) n -> e p kc n", p=128)
    wdsrc = wd_d.rearrange("e (kc p) n -> e p kc n", p=128)
    outevs = []
    NPART = KC // NF
    TPP = (NTT + NPART - 1) // NPART

    def info_part(e, bk, part):
        for T in range(part * TPP, min(NTT, (part + 1) * TPP)):
            oi = cnt["oh"] % 4
            cnt["oh"] += 1
            b.ts(oh[oi][:], iota, posm[:, e, T:T + 1], float(-bk * 512), ALU.subtract, ALU.is_equal,
                 [Tpos, Tcst], [Toh[oi]])
            for s4 in range(4):
                b.mm(banks[4 + s4][:, 0:5], oh[oi][:, s4 * 128:(s4 + 1) * 128], info[:, e, T, :], T == 0, T == NTT - 1,
                     [Toh[oi], Tinfo], [Tbank[4 + s4]])

    def info_finish(st):
        si, Ts_ = sinfo[st], Tsx[st]
        for s4 in range(4):
            b.cp(si[:, s4, 0:5], banks[4 + s4][:, 0:5], [Tbank[4 + s4]], [Ts_])
        b.stt(si[:, :, 5], si[:, :, 0], 128.0, si[:, :, 1], ALU.mult, ALU.add, [Ts_], [Ts_])
        b.ts(si[:, :, 6], si[:, :, 4], -65536.0, 65536.0, ALU.mult, ALU.add, [Ts_], [Ts_])
        b.tt(si[:, :, 5], si[:, :, 5], si[:, :, 6], ALU.add, [Ts_], [Ts_])
        b.cp(sidx[st][:], si[:, :, 5], [Ts_], [Ts_])
        b.tt(sgate[st][:], si[:, :, 2], si[:, :, 3], ALU.add, [Ts_], [Ts_])

    _bcc = {}

    def _bc(eng):
        if "r" not in _bcc:
            _bcc["r"] = eng.to_reg(ntok - 1)
        return _bcc["r"]

    blocks = [(e, bk) for e in range(NE) for bk in range(NB)]
    for part in range(NPART):
        info_part(blocks[0][0], blocks[0][1], part)
    info_finish(0)
    for bi_, (e, bk) in enumerate(blocks):
        st = bi_ % 2
        nxt = blocks[bi_ + 1] if bi_ + 1 < len(blocks) else None
        for s4 in range(4):
            xi = cnt["xg"] % 2
            cnt["xg"] += 1
            b.P.dma("pool", lambda eng, o_=xg[xi][:], ix=sidx[st][:, s4:s4 + 1]: eng.indirect_dma_start(
                out=o_, out_offset=None, in_=xtok_d, in_offset=bass.IndirectOffsetOnAxis(ap=ix, axis=0),
                bounds_check=_bc(eng), oob_is_err=False), [Tsx[st]], [Txg[xi]])
            for q4 in range(4):
                pb, Tpb = banks[4 + q4], Tbank[4 + q4]
                for jj in range(4):
                    kc = q4 * 4 + jj
                    b.tr(pb[:, jj * 128:(jj + 1) * 128], xg[xi][:, kc * 128:(kc + 1) * 128], ident[:], [Txg[xi], Tc], [Tpb])
                b.cp(x[:, q4 * 4:(q4 + 1) * 4, s4 * 128:(s4 + 1) * 128], pb[:, :].rearrange("p (j q) -> p j q", j=4),
                     [Tpb], [Tx], eng=("dve" if q4 % 2 == 0 else "act"))
        for pc in range(NPART):
            wi = cnt["gu"] % 3
            cnt["gu"] += 1
            c0 = pc * NF * 128
            b.dma(wg[wi][:], wgsrc[e, :, :, c0:c0 + NF * 128], (), [Twgu[wi]], eng="pool")
            b.dma(wu[wi][:], wgsrc[e, :, :, 2048 + c0:2048 + c0 + NF * 128], (), [Twgu[wi]], eng="pool")
            for f in range(NF):
                fc = pc * NF + f
                bi = cnt["ew"] % 2
                cnt["ew"] += 1
                pg, Tpg = banks[bi * 2], Tbank[bi * 2]
                pu, Tpu = banks[bi * 2 + 1], Tbank[bi * 2 + 1]
                for kc in range(KC):
                    b.mm(pg[:, :], wg[wi][:, kc, f * 128:(f + 1) * 128], x[:, kc, :], kc == 0, kc == KC - 1,
                         [Twgu[wi], Tx], [Tpg])
                for kc in range(KC):
                    b.mm(pu[:, :], wu[wi][:, kc, f * 128:(f + 1) * 128], x[:, kc, :], kc == 0, kc == KC - 1,
                         [Twgu[wi], Tx], [Tpu])
                Te = Tew[bi]
                b.ts(gc[bi][:], pg[:, :], bguT[:, e, fc:fc + 1], 7.0, ALU.add, ALU.min, [Tpg, Tbgu], [Te])
                b.act(sg_[bi][:], gc[bi][:], AF.Sigmoid, [Te], [Te], scale=1.702)
                b.ts(u1[bi][:], pu[:, :], bguT[:, e, 16 + fc:17 + fc], -7.0, ALU.add, ALU.max, [Tpu, Tbgu], [Te])
                b.ts(u1[bi][:], u1[bi][:], 7.0, 1.0, ALU.min, ALU.add, [Te], [Te])
                b.tt(gc[bi][:], gc[bi][:], sg_[bi][:], ALU.mult, [Te], [Te])
                b.tt(actT[:, fc, :], u1[bi][:], gc[bi][:], ALU.mult, [Te], [Tact[fc]])
            if nxt is not None:
                info_part(nxt[0], nxt[1], pc)
        if nxt is not None:
            info_finish(1 - st)
        for n in range(4):
            di = cnt["dn"] % 2
            cnt["dn"] += 1
            b.dma(wdn[di][:], wdsrc[e, :, :, n * 512:(n + 1) * 512], (), [Twdn[di]], eng="pool")
            for s4 in range(4):
                pb, Tpb = banks[4 + s4], Tbank[4 + s4]
                for fc in range(KC):
                    b.mm(pb[:, :], actT[:, fc, s4 * 128:(s4 + 1) * 128], wdn[di][:, fc, :], fc == 0, fc == KC - 1,
                         [Tact[fc], Twdn[di]], [Tpb])
                if s4 % 2 == 0:
                    b.ts(ysb[s4][:, n * 512:(n + 1) * 512], pb[:, :], sgate[st][:, s4:s4 + 1], None, ALU.mult, None,
                         [Tpb, Tsx[st]], [Tys[s4]])
                else:
                    b.act(ysb[s4][:, n * 512:(n + 1) * 512], pb[:, :], AF.Copy, [Tpb, Tsx[st]], [Tys[s4]],
                          scale=sgate[st][:, s4:s4 + 1])
        for s4 in range(4):
            outevs.append(b.P.dma("pool", lambda eng, i_=ysb[s4][:], ix=sidx[st][:, s4:s4 + 1]: eng.indirect_dma_start(
                out=y_d, out_offset=bass.IndirectOffsetOnAxis(ap=ix, axis=0), in_=i_, in_offset=None,
                bounds_check=_bc(eng), oob_is_err=False, compute_op=ALU.add), [Tys[s4], Tsx[st], Ty], [Ty], semtok=Tys[s4]).ev)
    b.P.wait_end("sp", outevs)
    return b.finish()


def cs_consts(ntok):
    NTT = ntok // 128
    U = (np.arange(128)[:, None] < np.arange(128)[None, :]).astype(np.float32)
    iota = np.broadcast_to(np.arange(512, dtype=np.float32)[None, :], (128, 512))
    tidx = np.broadcast_to(np.arange(NTT, dtype=np.float32)[None, :], (128, NTT))
    pidx = np.broadcast_to(np.arange(128, dtype=np.float32)[:, None], (128, NTT))
    return np.ascontiguousarray(np.concatenate([U, iota, tidx, pidx], axis=1).astype(np.float32))
```
